# Optimizing a Trainium2 kernel written in Bass

```python
import jax
import jax.numpy as jnp
from jax import lax
import numpy as np

D_MODEL = 1024
BATCH = 16
SEQ = 2048
DEPTH = 2

N_EVEN = (DEPTH + 1) // 2
N_ODD = DEPTH // 2

A_WIDTH = 512
A_GROUPS = 4
A_CHUNK = 128
B_WIDTH = 512
CONV_WIDTH = 31
N_HEADS = 8
HEAD_DIM = D_MODEL // N_HEADS
MOBA_BLOCK = 256
MOBA_TOPK = 3
MOBA_QCHUNK = 64
N_GROUPS = 4
EXPERTS_PER_GROUP = 4
N_EXPERTS = N_GROUPS * EXPERTS_PER_GROUP
EXPERT_TOPK = 2
D_EXPERT = 512
DN_ALPHA = (2.0 * DEPTH) ** 0.25
DN_BETA = (8.0 * DEPTH) ** -0.25
LN_EPS = 1e-5

kernel_name = 'hybrid_sgu_conv_moba_hmoe_block'


def layer_norm(x, g, b):
    xf = x.astype(jnp.float32)
    mu = xf.mean(-1, keepdims=True)
    var = jnp.square(xf - mu).mean(-1, keepdims=True)
    return ((xf - mu) * lax.rsqrt(var + LN_EPS) * g.astype(jnp.float32) + b.astype(jnp.float32)).astype(x.dtype)


def spatial_gating(z, ln_g, ln_b, w_s, b_s):
    bsz, t, _ = z.shape
    u, v = jnp.split(z, 2, axis=-1)
    v = layer_norm(v, ln_g, ln_b)
    n_chunks = t // A_CHUNK
    v = v.reshape(bsz, n_chunks, A_CHUNK, A_GROUPS, A_WIDTH // A_GROUPS)
    causal = jnp.tril(jnp.ones((A_CHUNK, A_CHUNK), dtype=bool))
    w = jnp.where(causal[None], w_s, 0)
    s = jnp.einsum('gpq,bcqgd->bcpgd', w, v) + b_s.T[:, :, None]
    return u * s.reshape(bsz, t, A_WIDTH)


def conv_module(z, w_dw, b_dw, ln_g, ln_b):
    a, gate = jnp.split(z, 2, axis=-1)
    a = a * jax.nn.sigmoid(gate)
    y = lax.conv_general_dilated(
        a, w_dw[:, None, :], window_strides=(1,), padding=[(CONV_WIDTH - 1, 0)],
        dimension_numbers=('NWC', 'WIO', 'NWC'), feature_group_count=B_WIDTH) + b_dw
    return jax.nn.silu(layer_norm(y, ln_g, ln_b))


def even_mixer(h, w_in, sgu_ln_g, sgu_ln_b, w_s, b_s, w_dw, b_dw, conv_ln_g, conv_ln_b, w_out):
    z = h @ w_in
    z_a, z_b = jnp.split(z, [2 * A_WIDTH], axis=-1)
    y_a = spatial_gating(jax.nn.gelu(z_a, approximate=False), sgu_ln_g, sgu_ln_b, w_s, b_s)
    y_b = conv_module(z_b, w_dw, b_dw, conv_ln_g, conv_ln_b)
    return jnp.concatenate([y_a, y_b], axis=-1) @ w_out


def moba_attention(q, k, v):
    bsz, t, h, dh = q.shape
    n_blocks = -(-t // MOBA_BLOCK)
    tp = n_blocks * MOBA_BLOCK
    pad = ((0, 0), (0, tp - t), (0, 0), (0, 0))
    q, k, v = jnp.pad(q, pad), jnp.pad(k, pad), jnp.pad(v, pad)
    kb = k.reshape(bsz, n_blocks, MOBA_BLOCK, h, dh).transpose(0, 3, 1, 2, 4)
    vb = v.reshape(bsz, n_blocks, MOBA_BLOCK, h, dh).transpose(0, 3, 1, 2, 4)
    own = jnp.arange(tp) // MOBA_BLOCK
    own_idx = jnp.broadcast_to(own[None, :, None, None], (bsz, tp, h, 1))
    own_valid = jnp.ones((bsz, tp, h, 1), dtype=bool)
    n_sel = min(MOBA_TOPK, n_blocks - 1)
    if n_sel > 0:
        k_mean = kb.astype(jnp.float32).mean(axis=3)
        gate = jnp.einsum('bthd,bhnd->bthn', q.astype(jnp.float32), k_mean)
        past = jnp.arange(n_blocks)[None, :] < own[:, None]
        gate = jnp.where(past[None, :, None, :], gate, -jnp.inf)
        _, top_idx = lax.top_k(gate, n_sel)
        sel_idx = jnp.concatenate([top_idx, own_idx], axis=-1)
        sel_valid = jnp.concatenate([top_idx < own[None, :, None, None], own_valid], axis=-1)
    else:
        sel_idx, sel_valid = own_idx, own_valid
    n_s = sel_idx.shape[-1]
    n_q = tp // MOBA_QCHUNK
    qs = q.reshape(bsz * n_q, MOBA_QCHUNK, h, dh)
    idxs = sel_idx.reshape(bsz * n_q, MOBA_QCHUNK, h, n_s)
    valids = sel_valid.reshape(bsz * n_q, MOBA_QCHUNK, h, n_s)
    b_ids = jnp.repeat(jnp.arange(bsz), n_q)
    j_ids = jnp.tile(jnp.arange(n_q), bsz)
    heads = jnp.arange(h)[None, :, None]
    offs = jnp.arange(MOBA_BLOCK)
    scale = dh ** -0.5

    def attend_chunk(args):
        b, j, qc, idx, valid = args
        k_sel = kb[b][heads, idx]
        v_sel = vb[b][heads, idx]
        s = jnp.einsum('qhd,qhspd->qhsp', qc.astype(jnp.float32), k_sel.astype(jnp.float32)) * scale
        qpos = j * MOBA_QCHUNK + jnp.arange(MOBA_QCHUNK)
        kpos = idx[..., None] * MOBA_BLOCK + offs
        mask = valid[..., None] & (kpos <= qpos[:, None, None, None])
        s = jnp.where(mask, s, -jnp.inf).reshape(MOBA_QCHUNK, h, n_s * MOBA_BLOCK)
        p = jax.nn.softmax(s, axis=-1)
        o = jnp.einsum('qhk,qhkd->qhd', p,
                       v_sel.reshape(MOBA_QCHUNK, h, n_s * MOBA_BLOCK, dh).astype(jnp.float32))
        return o.astype(qc.dtype)

    out = lax.map(attend_chunk, (b_ids, j_ids, qs, idxs, valids))
    return out.reshape(bsz, tp, h, dh)[:, :t]


def odd_mixer(h, w_qkv, w_out):
    bsz, t, _ = h.shape
    qkv = (h @ w_qkv).reshape(bsz, t, 3, N_HEADS, HEAD_DIM)
    o = moba_attention(qkv[:, :, 0], qkv[:, :, 1], qkv[:, :, 2])
    return o.reshape(bsz, t, N_HEADS * HEAD_DIM) @ w_out


def hier_moe(h, w_grp, b_grp, w_er, b_er, w_gate, w_up, w_down):
    x = h.reshape(-1, h.shape[-1])
    n = x.shape[0]
    xf = x.astype(jnp.float32)
    grp_logits = xf @ w_grp.astype(jnp.float32) + b_grp.astype(jnp.float32)
    grp_prob = jax.nn.softmax(grp_logits, axis=-1)
    g_w, g_sel = lax.top_k(grp_prob, 1)
    exp_logits = jnp.einsum('nd,gde->nge', xf, w_er.astype(jnp.float32)) + b_er.astype(jnp.float32)
    in_grp = jnp.take_along_axis(exp_logits, g_sel[:, :, None], axis=1)[:, 0]
    top_v, top_i = lax.top_k(in_grp, EXPERT_TOPK)
    top_w = jax.nn.softmax(top_v, axis=-1) * g_w
    expert_id = g_sel * EXPERTS_PER_GROUP + top_i
    combine = jnp.einsum('nk,nke->ne', top_w, jax.nn.one_hot(expert_id, N_EXPERTS, dtype=jnp.float32))
    out = jnp.zeros((n, x.shape[-1]), jnp.float32)
    for e in range(N_EXPERTS):
        hid = jax.nn.silu(x @ w_gate[e]) * (x @ w_up[e])
        out = out + combine[:, e:e + 1] * (hid @ w_down[e]).astype(jnp.float32)
    return out.astype(h.dtype).reshape(h.shape)


def setup_inputs(seed: int = 0) -> dict:
    key = jax.random.key(seed)
    ks = iter(jax.random.split(key, 32))
    d = D_MODEL

    def nrm(shape, s):
        return jax.random.normal(next(ks), shape, jnp.float32) * s

    return {
        'x': nrm((BATCH, SEQ, d), 1.0),
        'c': nrm((BATCH, d), 1.0),
        'ada_w': nrm((DEPTH, d, 6 * d), 0.1 * d ** -0.5),
        'ada_b': nrm((DEPTH, 6 * d), 0.01),
        'ln_mix_g': 1.0 + nrm((DEPTH, d), 0.02),
        'ln_mix_b': nrm((DEPTH, d), 0.02),
        'ln_ffn_g': 1.0 + nrm((DEPTH, d), 0.02),
        'ln_ffn_b': nrm((DEPTH, d), 0.02),
        'ev_w_in': nrm((N_EVEN, d, 2 * A_WIDTH + 2 * B_WIDTH), d ** -0.5),
        'ev_sgu_ln_g': 1.0 + nrm((N_EVEN, A_WIDTH), 0.02),
        'ev_sgu_ln_b': nrm((N_EVEN, A_WIDTH), 0.02),
        'ev_w_s': nrm((N_EVEN, A_GROUPS, A_CHUNK, A_CHUNK), A_CHUNK ** -0.5),
        'ev_b_s': 1.0 + nrm((N_EVEN, A_GROUPS, A_CHUNK), 0.02),
        'ev_w_dw': nrm((N_EVEN, CONV_WIDTH, B_WIDTH), CONV_WIDTH ** -0.5),
        'ev_b_dw': nrm((N_EVEN, B_WIDTH), 0.02),
        'ev_conv_ln_g': 1.0 + nrm((N_EVEN, B_WIDTH), 0.02),
        'ev_conv_ln_b': nrm((N_EVEN, B_WIDTH), 0.02),
        'ev_w_out': nrm((N_EVEN, A_WIDTH + B_WIDTH, d), DN_BETA * (A_WIDTH + B_WIDTH) ** -0.5),
        'od_w_qkv': nrm((N_ODD, d, 3 * N_HEADS * HEAD_DIM), d ** -0.5),
        'od_w_out': nrm((N_ODD, N_HEADS * HEAD_DIM, d), DN_BETA * (N_HEADS * HEAD_DIM) ** -0.5),
        'moe_w_grp': nrm((DEPTH, d, N_GROUPS), d ** -0.5),
        'moe_b_grp': nrm((DEPTH, N_GROUPS), 0.01),
        'moe_w_er': nrm((DEPTH, N_GROUPS, d, EXPERTS_PER_GROUP), d ** -0.5),
        'moe_b_er': nrm((DEPTH, N_GROUPS, EXPERTS_PER_GROUP), 0.01),
        'moe_w_gate': nrm((DEPTH, N_EXPERTS, d, D_EXPERT), d ** -0.5),
        'moe_w_up': nrm((DEPTH, N_EXPERTS, d, D_EXPERT), d ** -0.5),
        'moe_w_down': nrm((DEPTH, N_EXPERTS, D_EXPERT, d), DN_BETA * D_EXPERT ** -0.5),
    }


def reference(x, c, ada_w, ada_b, ln_mix_g, ln_mix_b, ln_ffn_g, ln_ffn_b,
              ev_w_in, ev_sgu_ln_g, ev_sgu_ln_b, ev_w_s, ev_b_s, ev_w_dw, ev_b_dw,
              ev_conv_ln_g, ev_conv_ln_b, ev_w_out, od_w_qkv, od_w_out,
              moe_w_grp, moe_b_grp, moe_w_er, moe_b_er, moe_w_gate, moe_w_up, moe_w_down):
    c_act = jax.nn.silu(c)
    for i in range(DEPTH):
        cond = c_act @ ada_w[i] + ada_b[i]
        sh_m, sc_m, g_m, sh_f, sc_f, g_f = jnp.split(cond[:, None, :], 6, axis=-1)
        h = x * (1.0 + sc_m) + sh_m
        j = i // 2
        if i % 2 == 0:
            y = even_mixer(h, ev_w_in[j], ev_sgu_ln_g[j], ev_sgu_ln_b[j], ev_w_s[j], ev_b_s[j],
                           ev_w_dw[j], ev_b_dw[j], ev_conv_ln_g[j], ev_conv_ln_b[j], ev_w_out[j])
        else:
            y = odd_mixer(h, od_w_qkv[j], od_w_out[j])
        x = layer_norm(DN_ALPHA * x + (1.0 + g_m) * y, ln_mix_g[i], ln_mix_b[i])
        h = x * (1.0 + sc_f) + sh_f
        y = hier_moe(h, moe_w_grp[i], moe_b_grp[i], moe_w_er[i], moe_b_er[i],
                     moe_w_gate[i], moe_w_up[i], moe_w_down[i])
        x = layer_norm(DN_ALPHA * x + (1.0 + g_f) * y, ln_ffn_g[i], ln_ffn_b[i])
    return x
```

```python
import threading
import numpy as np
from contextlib import ExitStack
import concourse.bass as bass
import concourse.mybir as mybir
from concourse.bass_utils import run_bass_kernel_spmd

F32 = mybir.dt.float32
BF16 = mybir.dt.bfloat16
I32 = mybir.dt.int32
U32 = mybir.dt.uint32
AF = mybir.ActivationFunctionType
ALU = mybir.AluOpType
AX = mybir.AxisListType

NCORES = 8
D = 1024
SEQ = 2048
NSEQ = 2
TOK = NSEQ * SEQ
TT = 512
ALPHA = 4.0 ** 0.25
EPS = 1e-5
NEG = -1.0e30
ATTN_DEBUG = "ABC"
SPARSE = True
MOE_DEBUG = "ABC"
NTHR = 2
HEAD_M = 250
HEAD_A = 56
HEAD_B = 83
HEAD_C = 70
HEAD_AT = 300

ENGS = ("pe", "act", "dve", "pool", "sp")


class Buf:
    __slots__ = ("name", "writers", "readers", "dsem", "dcount", "excl", "dslot")

    def __init__(self, name):
        self.name = name
        self.writers = []
        self.readers = []
        self.dsem = None
        self.dcount = 0
        self.excl = False
        self.dslot = None


class Op:
    __slots__ = ("eng", "fn", "waits", "is_dma", "is_nop", "dbuf", "value", "needed", "eidx", "semval", "epoch")

    def __init__(self, eng, fn):
        self.eng = eng
        self.fn = fn
        self.waits = []
        self.is_dma = False
        self.is_nop = False
        self.dbuf = None
        self.value = 0
        self.needed = False
        self.eidx = 0
        self.semval = 0
        self.epoch = 0


class Turns:
    def __init__(self, n):
        self.n = n
        self.cur = 0
        self.alive = [True] * n
        self.cv = threading.Condition()
        self.local = threading.local()
        self.head = 0

    def _advance(self):
        for k in range(1, self.n + 1):
            c = (self.cur + k) % self.n
            if self.alive[c]:
                self.cur = c
                return

    def start(self, tid):
        self.local.tid = tid
        with self.cv:
            while self.cur != tid:
                self.cv.wait()

    def yield_turn(self):
        tid = self.local.tid
        if tid == 0 and self.head > 0:
            self.head -= 1
            return
        with self.cv:
            self._advance()
            self.cv.notify_all()
            while self.cur != tid:
                self.cv.wait()

    def finish(self):
        tid = self.local.tid
        with self.cv:
            self.alive[tid] = False
            if any(self.alive):
                self._advance()
            self.cv.notify_all()


class Prog:
    def __init__(self, nc, es):
        self.nc = nc
        self.es = es
        self.ops = {e: [] for e in ENGS}
        self.clock = {e: {} for e in ENGS}
        self.esem = {}
        self.bufs = []
        self.same_engine_sync = True
        self.epoch = 0
        self.turns = None
        self.slots = []
        self.free_slots = []

    def buf(self, name):
        b = Buf(name)
        self.bufs.append(b)
        return b

    def _dep(self, op, d):
        E = op.eng
        if d.is_dma:
            key = ("d", id(d.dbuf))
            if self.clock[E].get(key, 0) >= d.value:
                return
            self.clock[E][key] = d.value
            op.waits.append(("d", d.dbuf, d.value))
        else:
            if d.is_nop:
                return
            if d.eng == E and (E == "pe" or not self.same_engine_sync):
                return
            key = ("e", d.eng)
            if self.clock[E].get(key, 0) >= d.eidx:
                return
            self.clock[E][key] = d.eidx
            d.needed = True
            op.waits.append(("e", d.eng, d))

    def op(self, eng, fn, reads=(), writes=(), joins=(), dma=None):
        o = Op(eng, fn)
        o.epoch = self.epoch
        lst = self.ops[eng]
        lst.append(o)
        o.eidx = len(lst)
        if dma is not None:
            o.is_dma = True
            if dma.dslot is None:
                if self.free_slots:
                    dma.dslot = self.free_slots.pop()
                else:
                    dma.dslot = Buf("slot%d" % len(self.slots))
                    self.slots.append(dma.dslot)
            sl = dma.dslot
            o.dbuf = sl
            sl.dcount += 16
            o.value = sl.dcount
        deps = []
        for b in reads:
            deps.extend(b.writers)
            if b.excl:
                deps.extend(r for r in b.readers if r.eng != eng)
        for b in writes:
            deps.extend(b.writers)
            deps.extend(b.readers)
        for b in joins:
            if b.readers:
                deps.extend(b.writers)
                deps.extend(b.readers)
        best = {}
        for d in deps:
            if d.is_dma:
                key = ("d", id(d.dbuf))
                v = d.value
            else:
                if d.is_nop:
                    continue
                key = ("e", d.eng)
                v = d.eidx
            if key not in best or best[key][0] < v:
                best[key] = (v, d)
        for key in best:
            self._dep(o, best[key][1])
        for b in reads:
            b.readers.append(o)
        for b in writes:
            b.writers = [o]
            b.readers = []
        for b in joins:
            if b.readers:
                b.writers = [o]
                b.readers = []
            else:
                b.writers.append(o)
        if self.turns is not None:
            self.turns.yield_turn()
        return o

    def run_threads(self, fns, head=0):
        if len(fns) == 1:
            fns[0]()
            return
        turns = Turns(len(fns))
        turns.head = head
        self.turns = turns
        errs = []

        def wrap(tid, fn):
            turns.start(tid)
            try:
                fn()
            except BaseException as ex:
                errs.append(ex)
            finally:
                turns.finish()
        ths = [threading.Thread(target=wrap, args=(i, f)) for i, f in enumerate(fns)]
        for t in ths:
            t.start()
        for t in ths:
            t.join()
        self.turns = None
        if errs:
            raise errs[0]

    def barrier(self):
        lasts = {}
        for e in ENGS:
            for o in reversed(self.ops[e]):
                if not o.is_dma and not o.is_nop:
                    lasts[e] = o
                    break
        dm = [(b, b.dcount) for b in self.slots if b.dcount > 0]
        for e in ENGS:
            o = Op(e, lambda eng: eng.nop())
            o.is_nop = True
            self.ops[e].append(o)
            o.eidx = len(self.ops[e])
            for e2, l in lasts.items():
                if e2 == e:
                    continue
                key = ("e", e2)
                if self.clock[e].get(key, 0) >= l.eidx:
                    continue
                self.clock[e][key] = l.eidx
                l.needed = True
                o.waits.append(("e", e2, l))
            for b, v in dm:
                key = ("d", id(b))
                if self.clock[e].get(key, 0) >= v:
                    continue
                self.clock[e][key] = v
                o.waits.append(("d", b, v))
        for b in self.bufs:
            b.writers = []
            b.readers = []
            if b.dslot is not None:
                self.free_slots.append(b.dslot)
                b.dslot = None

    def emit(self):
        nc, es = self.nc, self.es
        for e in ENGS:
            eps_ = sorted(set(o.epoch for o in self.ops[e] if o.needed))
            for ep in eps_:
                self.esem[(e, ep)] = es.enter_context(nc.semaphore("es_%s_%d" % (e, ep)))
        n = 0
        for e in ENGS:
            for o in self.ops[e]:
                if o.is_dma and o.dbuf.dsem is None:
                    o.dbuf.dsem = es.enter_context(nc.semaphore("ds%d" % n))
                    n += 1
        self.ndsem = n
        for e in ENGS:
            c = {}
            for o in self.ops[e]:
                if o.needed:
                    c[o.epoch] = c.get(o.epoch, 0) + 1
                    o.semval = c[o.epoch]
        block = es.enter_context(nc.Block())

        def run(ename):
            def body(eng):
                for o in self.ops[ename]:
                    for (k, a, b) in o.waits:
                        if k == "d":
                            eng.wait_ge(a.dsem, b)
                        else:
                            eng.wait_ge(self.esem[(a, b.epoch)], b.semval)
                    ins = o.fn(eng)
                    if o.is_dma:
                        ins.then_inc(o.dbuf.dsem, 16)
                    elif o.needed:
                        ins.then_inc(self.esem[(ename, o.epoch)], 1)
            return body

        block.tensor(run("pe"))
        block.scalar(run("act"))
        block.vector(run("dve"))
        block.gpsimd(run("pool"))
        block.sync(run("sp"))


class TB:
    def __init__(self, p, t, name):
        self.t = t
        self.b = p.buf(name)

    def __getitem__(self, k):
        return self.t[k]


class Ring:
    def __init__(self, items):
        self.items = items
        self.i = 0

    def next(self):
        r = self.items[self.i % len(self.items)]
        self.i += 1
        return r


def build(stages=4, debug=False, only=None):
    nc = bass.Bass("TRN2", target_bir_lowering=False)

    def din(name, shape):
        return nc.dram_tensor(name, list(shape), F32, kind="ExternalInput").ap()

    xT = din("xT", [8, 128, TOK])
    cT = din("cT", [128, 8, NSEQ])
    ada_w = din("ada_w", [2, D, 6 * D])
    ada_bT = din("ada_bT", [128, 2, 48])
    lnp = din("lnp", [128, 2, 4, 8])
    ev_w_in = din("ev_w_in", [D, 2048])
    ev_w_out = din("ev_w_out", [D, D])
    sgp_d = din("sgp", [128, 2, 4])
    wsT_d = din("wsT", [128, 4, 128])
    bs_d = din("bs", [1, 512])
    wdw_d = din("wdw", [128, 4, 31])
    cvp_d = din("cvp", [128, 3, 4])
    od_w_qkv = din("od_w_qkv", [D, 3 * D])
    od_w_out = din("od_w_out", [D, D])
    wr_d = din("wr", [2, 128, 8, 20])
    br_d = din("br", [2, 1, 20])
    w_gate = din("moe_w_gate", [2, 16, 128, 8, 512])
    w_up = din("moe_w_up", [2, 16, 128, 8, 512])
    w_down = din("moe_w_down", [2, 16, 128, 4, D])
    consts_d = din("consts", [128, 2473])

    yT = nc.dram_tensor("yT", [8, 128, TOK], F32, kind="ExternalOutput").ap()
    XA = nc.dram_tensor("XA", [8, 128, TOK], F32, kind="Internal").ap()
    XB = nc.dram_tensor("XB", [8, 128, TOK], F32, kind="Internal").ap()
    dbg = {}

    es = ExitStack()
    p = Prog(nc, es)

    uniq = [0]

    def sb(st, name, shape, dt):
        uniq[0] += 1
        name = "%s_u%d" % (name, uniq[0])
        return TB(p, st.enter_context(nc.sbuf_tensor(name, list(shape), dt)), name)

    def MM(out, lhsT, rhs, first, last, reads, wb):
        p.op("pe", lambda e: e.matmul(out, lhsT, rhs, start=first, stop=last), reads=reads,
             writes=[wb] if first else [], joins=[] if first else [wb])

    def MMg(out, lhsT, rhs, reads, wb, newgroup):
        p.op("pe", lambda e: e.matmul(out, lhsT, rhs, start=True, stop=True), reads=reads,
             writes=[wb] if newgroup else [], joins=[] if newgroup else [wb])

    def ACT(out, in_, func, reads, writes, bias=None, scale=None):
        kw = {}
        if bias is not None:
            kw["bias"] = bias
        if scale is not None:
            kw["scale"] = scale
        p.op("act", lambda e: e.activation(out=out, in_=in_, func=func, **kw), reads=reads, writes=writes)

    def TTo(eng, out, in0, in1, op, reads, writes):
        p.op(eng, lambda e: e.tensor_tensor(out=out, in0=in0, in1=in1, op=op), reads=reads, writes=writes)

    def TS(eng, out, in0, s1, s2, op0, op1, reads, writes):
        if op1 is None:
            p.op(eng, lambda e: e.tensor_scalar(out=out, in0=in0, scalar1=s1, scalar2=None, op0=op0),
                 reads=reads, writes=writes)
        else:
            p.op(eng, lambda e: e.tensor_scalar(out=out, in0=in0, scalar1=s1, scalar2=s2, op0=op0, op1=op1),
                 reads=reads, writes=writes)

    def STT(out, in0, scalar, in1, op0, op1, reads, writes):
        p.op("dve", lambda e: e.scalar_tensor_tensor(out=out, in0=in0, scalar=scalar, in1=in1, op0=op0, op1=op1),
             reads=reads, writes=writes)

    def CP(eng, out, in_, reads, writes):
        p.op(eng, lambda e: e.tensor_copy(out=out, in_=in_), reads=reads, writes=writes)

    def RED(out, in_, op, reads, writes):
        p.op("dve", lambda e: e.tensor_reduce(out=out, in_=in_, axis=AX.X, op=op), reads=reads, writes=writes)

    def RECIP(out, in_, reads, writes):
        p.op("dve", lambda e: e.reciprocal(out=out, in_=in_), reads=reads, writes=writes)

    def MSET(eng, ap, val, writes):
        p.op(eng, lambda e: e.memset(ap, val), writes=writes)

    def DMA(q, out, in_, reads, writes, dbuf):
        p.op(q, lambda e: e.dma_start(out=out, in_=in_), reads=reads, writes=writes, dma=dbuf)

    G = ExitStack()
    es.enter_context(G)
    psb = [TB(p, G.enter_context(nc.psum_tensor("ps%d" % i, [128, 512], F32)), "ps%d" % i) for i in range(8)]
    for t_ in psb:
        t_.b.excl = True
    class TRing:
        def __init__(self, items):
            self.full = Ring(items)
            h = len(items) // 2
            self.sub = [Ring(items[:h]), Ring(items[h:])]

        def next(self):
            if p.turns is not None:
                return self.sub[p.turns.local.tid % 2].next()
            return self.full.next()
    PA = TRing(psb[0:4])
    PB = TRing(psb[4:6])
    PC = TRing(psb[6:8])

    cst = sb(G, "cst", [128, 2473], F32)
    DMA("sp", cst[:], consts_d[:, :], [], [cst.b], cst.b)
    ident = cst[:, 0:128]
    tri = cst[:, 128:256]
    ones = cst[:, 256:384]
    sel = cst[0:16, 384:2432]
    thr8 = cst[:, 2432:2440]
    jt32 = cst[:, 2440:2472]
    pcol = cst[:, 2472:2473]

    identb = sb(G, "identb", [128, 128], BF16)
    trib = sb(G, "trib", [128, 128], BF16)
    onesb = sb(G, "onesb", [128, 128], BF16)
    on1024 = sb(G, "on1024", [128, 128], BF16)
    on512 = sb(G, "on512", [128, 128], BF16)
    CP("dve", identb[:], ident, [cst.b], [identb.b])
    CP("dve", trib[:], tri, [cst.b], [trib.b])
    CP("dve", onesb[:], ones, [cst.b], [onesb.b])
    TS("dve", on1024[:], ones, 1.0 / 1024, None, ALU.mult, None, [cst.b], [on1024.b])
    TS("dve", on512[:], ones, 1.0 / 512, None, ALU.mult, None, [cst.b], [on512.b])
    ustrb = sb(G, "ustrb", [128, 128], BF16)
    TTo("dve", ustrb[:], tri, ident, ALU.subtract, [cst.b], [ustrb.b])
    epst = sb(G, "epst", [128, 1], F32)
    MSET("dve", epst[:], EPS, [epst.b])

    lnp_sb = sb(G, "lnp_sb", [128, 2, 4, 8], F32)
    DMA("sp", lnp_sb[:], lnp[:, :, :, :], [], [lnp_sb.b], lnp_sb.b)
    cond = sb(G, "cond", [128, 2, 48, NSEQ], F32)

    with ExitStack() as st:
        cT_sb = sb(st, "cT_sb", [128, 8, NSEQ], F32)
        csil = sb(st, "csil", [128, 8, NSEQ], BF16)
        adab = sb(st, "adab", [128, 2, 48], F32)
        DMA("sp", cT_sb[:], cT[:, :, :], [], [cT_sb.b], cT_sb.b)
        DMA("sp", adab[:], ada_bT[:, :, :], [], [adab.b], adab.b)
        ACT(csil[:], cT_sb[:], AF.Silu, [cT_sb.b], [csil.b])
        wb2 = [sb(st, "adaw%d" % i, [128, 8, 1024], BF16) for i in range(2)]
        k = 0
        for l in range(2):
            for s6 in range(6):
                w = wb2[k % 2]
                k += 1
                DMA("pool", w[:], ada_w[l, :, s6 * 1024:(s6 + 1) * 1024].rearrange("(kc p) n -> p kc n", p=128),
                    [], [w.b], w.b)
                ps = PC.next()
                for dc in range(8):
                    for kc in range(8):
                        first = (dc == 0 and kc == 0)
                        p.op("pe", (lambda e, o=ps[:, dc * 2:dc * 2 + 2], a=w[:, kc, dc * 128:(dc + 1) * 128],
                                    b=csil[:, kc, :], f=(kc == 0), la=(kc == 7): e.matmul(o, a, b, start=f, stop=la)),
                             reads=[w.b, csil.b], writes=[ps.b] if first else [], joins=[] if first else [ps.b])
                add1 = 1.0 if s6 in (1, 2, 4, 5) else 0.0
                for s in range(NSEQ):
                    STT(cond[:, l, s6 * 8:(s6 + 1) * 8, s], ps[:, s:16:2], add1, adab[:, l, s6 * 8:(s6 + 1) * 8],
                        ALU.add, ALU.add, [ps.b, adab.b], [cond.b])
        p.barrier()

    def cv(l, split, dc, s):
        return cond[:, l, split * 8 + dc, s:s + 1]

    def load_x(src, tok0, xt, W=TT):
        DMA("sp", xt[:], src[:, :, tok0:tok0 + W].rearrange("kc p t -> p kc t"), [], [xt.b], xt.b)

    def modulate(xt, hT, l, split_sh, split_sc, s):
        for dc in range(8):
            ACT(hT[:, dc, :], xt[:, dc, :], AF.Identity, [xt.b, cond.b], [hT.b] if dc == 0 else [],
                bias=cv(l, split_sh, dc, s), scale=cv(l, split_sc, dc, s)) if dc == 0 else \
                p.op("act", (lambda e, o=hT[:, dc, :], i=xt[:, dc, :], b=cv(l, split_sh, dc, s), sc=cv(l, split_sc, dc, s):
                             e.activation(out=o, in_=i, func=AF.Identity, bias=b, scale=sc)),
                     reads=[xt.b, cond.b], joins=[hT.b])

    def ln_fm(r, nch, onb, lnT, W=TT):
        rb, sd, mu = lnT["rb"], lnT["sd"], lnT["mu"]
        if "sqf" in lnT:
            sqf, sqb = lnT["sqf"], lnT["sqb"]
        else:
            sqf, sqb = (lambda c, t_=lnT["sq"]: t_[:, c, :]), [lnT["sq"].b]
        for c in range(nch):
            p.op("act", (lambda e, o=sqf(c), i=r[:, c, :]: e.activation(out=o, in_=i, func=AF.Square)),
                 reads=[r.b], writes=sqb if c == 0 else [], joins=[] if c == 0 else sqb)
            p.op("dve", (lambda e, o=rb[:, c, :], i=r[:, c, :]: e.tensor_copy(out=o, in_=i)),
                 reads=[r.b], writes=[rb.b] if c == 0 else [], joins=[] if c == 0 else [rb.b])
        mps = PB.next()
        for c in range(nch):
            MM(mps[:, 0:W], onb[:], rb[:, c, :], c == 0, c == nch - 1, [onb.b, rb.b], mps.b)
        vps = PC.next()
        for c in range(nch):
            MM(vps[:, 0:W], onb[:], sqf(c), c == 0, c == nch - 1, [onb.b] + sqb, vps.b)
        ACT(mu[:], mps[:, 0:W], AF.Identity, [mps.b], [mu.b])
        STT(sd[:], mu[:], -1.0, mu[:], ALU.mult, ALU.mult, [mu.b], [sd.b])
        STT(sd[:], vps[:, 0:W], EPS, sd[:], ALU.add, ALU.add, [vps.b, sd.b], [sd.b])
        ACT(sd[:], sd[:], AF.Sqrt, [sd.b], [sd.b])
        RECIP(sd[:], sd[:], [sd.b], [sd.b])
        for c in range(nch):
            p.op("dve", (lambda e, o=r[:, c, :], a=r[:, c, :], b=mps[:, 0:W]: e.tensor_tensor(out=o, in0=a, in1=b, op=ALU.subtract)),
                 reads=[mps.b, r.b] if c == 0 else [mps.b], writes=[r.b] if c == 0 else [], joins=[] if c == 0 else [r.b])
        for c in range(nch):
            p.op("pool", (lambda e, o=r[:, c, :], a=r[:, c, :], b=sd[:]: e.tensor_tensor(out=o, in0=a, in1=b, op=ALU.mult)),
                 reads=[sd.b, r.b] if c == 0 else [sd.b], writes=[r.b] if c == 0 else [], joins=[] if c == 0 else [r.b])

    def ln_out_store(r, gi, bi, l, dst, tok0, W=TT):
        xo = r
        for dc in range(8):
            p.op("act", (lambda e, o=xo[:, dc, :], i=r[:, dc, :], sc=lnp_sb[:, l, gi, dc:dc + 1], b=lnp_sb[:, l, bi, dc:dc + 1]:
                         e.activation(out=o, in_=i, func=AF.Identity, bias=b, scale=sc)),
                 reads=[r.b, lnp_sb.b] if dc == 0 else [lnp_sb.b], writes=[xo.b] if dc == 0 else [], joins=[] if dc == 0 else [xo.b])
        DMA("act", dst[:, :, tok0:tok0 + W].rearrange("kc p t -> p kc t"), xo[:], [xo.b], [], xo.b)

    def mixer0(src, dst, l):
        with ExitStack() as st:
            win = sb(st, "win", [128, 8, 2048], BF16)
            wout = sb(st, "wout", [128, 8, 1024], BF16)
            for j in range(4):
                p.op("pool", (lambda e, j=j: e.dma_start(out=win[:, :, j * 512:(j + 1) * 512],
                                                          in_=ev_w_in[:, j * 512:(j + 1) * 512].rearrange("(kc p) n -> p kc n", p=128))),
                     writes=[win.b] if j == 0 else [], joins=[] if j == 0 else [win.b], dma=win.b)
            for j in range(2):
                p.op("pool", (lambda e, j=j: e.dma_start(out=wout[:, :, j * 512:(j + 1) * 512],
                                                          in_=ev_w_out[:, j * 512:(j + 1) * 512].rearrange("(kc p) n -> p kc n", p=128))),
                     writes=[wout.b] if j == 0 else [], joins=[] if j == 0 else [wout.b], dma=wout.b)
            sgp = sb(st, "sgp_sb", [128, 2, 4], F32)
            DMA("sp", sgp[:], sgp_d[:, :, :], [], [sgp.b], sgp.b)
            cvp = sb(st, "cvp_sb", [128, 3, 4], F32)
            DMA("sp", cvp[:], cvp_d[:, :, :], [], [cvp.b], cvp.b)
            wsTm = sb(st, "wsTm", [128, 4, 128], BF16)
            C4 = sb(st, "C4", [128, 4, 128], F32)
            diag = sb(st, "diag", [128, 4, 31, 128], BF16)
            stmp = ExitStack()
            wsT = sb(stmp, "wsT_sb", [128, 4, 128], F32)
            DMA("sp", wsT[:], wsT_d[:, :, :], [], [wsT.b], wsT.b)
            bsB = sb(stmp, "bsB", [128, 4, 128], F32)
            DMA("sp", bsB[:].rearrange("p g q -> p (g q)"), bs_d[0:1, :].to_broadcast([128, 512]), [], [bsB.b], bsB.b)
            wdw = sb(stmp, "wdw_sb", [128, 4, 31], F32)
            DMA("sp", wdw[:], wdw_d[:, :, :], [], [wdw.b], wdw.b)
            for g in range(4):
                p.op("dve", (lambda e, g=g: e.tensor_tensor(out=wsTm[:, g, :], in0=wsT[:, g, :], in1=tri, op=ALU.mult)),
                     reads=[wsT.b, cst.b], writes=[wsTm.b] if g == 0 else [], joins=[] if g == 0 else [wsTm.b])
            rps = PC.next()
            for g in range(4):
                p.op("pe", (lambda e, g=g: e.matmul(rps[:, g * 128:(g + 1) * 128], onesb[:], wsTm[:, g, :], start=True, stop=True)),
                     reads=[onesb.b, wsTm.b], writes=[rps.b] if g == 0 else [], joins=[] if g == 0 else [rps.b])
            for g in range(4):
                p.op("dve", (lambda e, g=g: e.scalar_tensor_tensor(out=C4[:, g, :], in0=rps[:, g * 128:(g + 1) * 128],
                                                                    scalar=sgp[:, 1, g:g + 1], in1=bsB[:, g, :],
                                                                    op0=ALU.mult, op1=ALU.add)),
                     reads=[rps.b, sgp.b, bsB.b], writes=[C4.b] if g == 0 else [], joins=[] if g == 0 else [C4.b])
            for c in range(4):
                for k in range(31):
                    p.op("dve", (lambda e, c=c, k=k: e.tensor_scalar(out=diag[:, c, k, :], in0=ident, scalar1=wdw[:, c, k:k + 1],
                                                                      scalar2=None, op0=ALU.mult)),
                         reads=[cst.b, wdw.b], writes=[diag.b] if (c == 0 and k == 0) else [],
                         joins=[] if (c == 0 and k == 0) else [diag.b])

            p.barrier()
            stmp.close()
            WM = 256
            NSB = WM // 128

            def mkM(s):
                GL = sb(st, "GL", [128, 4, 30 + SEQ], BF16).t
                GLh = p.buf("GLh")
                GLb = [p.buf("GLb%d" % i) for i in range(SEQ // WM)]
                MSET("pool", GL[:, :, 0:30], 0.0, [GLh])
                xt = sb(st, "xt", [128, 8, WM], F32)
                hT = sb(st, "hT", [128, 8, WM], BF16)
                uT = sb(st, "uT", [128, 4, WM], F32)
                vg = Ring([sb(st, "vg%d" % i, [128, TT], F32) for i in range(1)])
                st6 = sb(st, "st6", [128, 6], F32)
                mv = sb(st, "mv", [128, 2], F32)
                rs = sb(st, "rs", [128, 1], F32)
                vn = sb(st, "vn", [128, NSB, TT], BF16)
                t1 = sb(st, "t1", [128, WM], F32)
                yab = sb(st, "yab", [128, 8, WM], BF16)

                class _V:
                    def __init__(self, lo, name):
                        self.lo = lo
                        self.b = p.buf(name)

                    def __getitem__(self, k):
                        a, c, d_ = k
                        return yab[a, self.lo + c, d_]
                ya = _V(0, "ya")
                yb = _V(4, "yb")
                sg = Ring([sb(st, "sg%d" % i, [128, WM], F32) for i in range(2)])
                yc = sb(st, "yc", [128, 4, WM], F32)
                tmp = Ring([sb(st, "tmp%d" % i, [128, WM], F32) for i in range(2)])
                _rb = sb(st, "lnr_rb", [128, 8, WM], BF16)
                _sd = sb(st, "lnr_sd", [128, WM], F32)
                lnr = {"rb": _rb, "sd": _sd, "mu": t1, "sqf": (lambda c: yab[:, c, :]), "sqb": [ya.b, yb.b]}
                lnc = {"rb": _rb, "sd": _sd, "mu": t1, "sqf": (lambda c: yab[:, 4 + c, :]), "sqb": [yb.b]}

                def run():
                    for t in range(SEQ // WM):
                        tok0 = s * SEQ + t * WM
                        load_x(src, tok0, xt, WM)
                        modulate(xt, hT, l, 0, 1, s)
                        for fo in range(4):
                            ps = PA.next()
                            for kc in range(8):
                                MM(ps[:, 0:WM], win[:, kc, fo * 128:(fo + 1) * 128], hT[:, kc, :], kc == 0, kc == 7, [win.b, hT.b], ps.b)
                            p.op("act", (lambda e, o=uT[:, fo, :], i=ps[:, 0:WM]: e.activation(out=o, in_=i, func=AF.Gelu)),
                                 reads=[ps.b], writes=[uT.b] if fo == 0 else [], joins=[] if fo == 0 else [uT.b])
                        for sub in range(NSB):
                            ps = PA.next()
                            for kc in range(8):
                                MM(ps[:], hT[:, kc, sub * 128:(sub + 1) * 128], win[:, kc, 512:1024], kc == 0, kc == 7, [win.b, hT.b], ps.b)
                            v = vg.next()
                            ACT(v[:], ps[:], AF.Gelu, [ps.b], [v.b])
                            p.op("dve", (lambda e, v=v: e.bn_stats(out=st6[:], in_=v[:])), reads=[v.b], writes=[st6.b])
                            p.op("dve", lambda e: e.bn_aggr(out=mv[:], in_=st6[:]), reads=[st6.b], writes=[mv.b])
                            ACT(rs[:], mv[:, 1:2], AF.Sqrt, [mv.b, epst.b], [rs.b], bias=epst[:, 0:1])
                            RECIP(rs[:], rs[:], [rs.b], [rs.b])
                            p.op("dve", (lambda e, v=v, sub=sub: e.tensor_scalar(out=vn[:, sub, :], in0=v[:], scalar1=mv[:, 0:1], scalar2=rs[:, 0:1],
                                                                                  op0=ALU.subtract, op1=ALU.mult)),
                                 reads=[v.b, mv.b, rs.b], writes=[vn.b] if sub == 0 else [], joins=[] if sub == 0 else [vn.b])
                        for g in range(4):
                            ps = PA.next()
                            for sub in range(NSB):
                                p.op("pe", (lambda e, ps=ps, g=g, sub=sub: e.matmul(ps[:, sub * 128:(sub + 1) * 128], vn[:, sub, g * 128:(g + 1) * 128],
                                                                                     wsTm[:, g, :], start=True, stop=True)),
                                     reads=[vn.b, wsTm.b], writes=[ps.b] if sub == 0 else [], joins=[] if sub == 0 else [ps.b])
                            STT(t1[:].rearrange("p (c q) -> p c q", c=NSB), ps[:, 0:WM].rearrange("p (c q) -> p c q", c=NSB), sgp[:, 0, g:g + 1],
                                C4[:, g:g + 1, :].to_broadcast([128, NSB, 128]), ALU.mult, ALU.add, [ps.b, sgp.b, C4.b], [t1.b])
                            p.op("dve", (lambda e, g=g: e.tensor_tensor(out=ya[:, g, :], in0=t1[:], in1=uT[:, g, :], op=ALU.mult)),
                                 reads=[t1.b, uT.b], writes=[ya.b] if g == 0 else [], joins=[] if g == 0 else [ya.b])
                        for fo in range(4):
                            pa = PA.next()
                            for kc in range(8):
                                MM(pa[:, 0:WM], win[:, kc, 1024 + fo * 128:1024 + (fo + 1) * 128], hT[:, kc, :], kc == 0, kc == 7, [win.b, hT.b], pa.b)
                            pg = PA.next()
                            for kc in range(8):
                                MM(pg[:, 0:WM], win[:, kc, 1536 + fo * 128:1536 + (fo + 1) * 128], hT[:, kc, :], kc == 0, kc == 7, [win.b, hT.b], pg.b)
                            sgt = sg.next()
                            ACT(sgt[:], pg[:, 0:WM], AF.Sigmoid, [pg.b], [sgt.b])
                            p.op("dve", (lambda e, fo=fo, pa=pa, sgt=sgt, t=t: e.tensor_tensor(out=GL[:, fo, 30 + t * WM:30 + (t + 1) * WM], in0=pa[:, 0:WM], in1=sgt[:], op=ALU.mult)),
                                 reads=[pa.b, sgt.b], writes=[GLb[t]] if fo == 0 else [], joins=[] if fo == 0 else [GLb[t]])
                        glreads = [diag.b, GLb[t], GLh] + ([GLb[t - 1]] if t > 0 else [])
                        for c in range(4):
                            ps = PA.next()
                            for k in range(31):
                                MM(ps[:, 0:WM], diag[:, c, k, :], GL[:, c, t * WM + k:t * WM + k + WM], k == 0, k == 30, glreads, ps.b)
                            p.op("act", (lambda e, c=c, ps=ps: e.activation(out=yc[:, c, :], in_=ps[:, 0:WM], func=AF.Identity, bias=cvp[:, 0, c:c + 1])),
                                 reads=[ps.b, cvp.b], writes=[yc.b] if c == 0 else [], joins=[] if c == 0 else [yc.b])
                        ln_fm(yc, 4, on512, lnc, WM)
                        for c in range(4):
                            p.op("act", (lambda e, c=c: e.activation(out=yb[:, c, :], in_=yc[:, c, :], func=AF.Silu,
                                                                      bias=cvp[:, 2, c:c + 1], scale=cvp[:, 1, c:c + 1])),
                                 reads=[yc.b, cvp.b], writes=[yb.b] if c == 0 else [], joins=[] if c == 0 else [yb.b])
                        for dc in range(8):
                            ps = PA.next()
                            for kc in range(8):
                                src_y = ya if kc < 4 else yb
                                MM(ps[:, 0:WM], wout[:, kc, dc * 128:(dc + 1) * 128], src_y[:, kc % 4, :], kc == 0, kc == 7, [wout.b, ya.b, yb.b], ps.b)
                            tm = tmp.next()
                            ACT(tm[:], ps[:, 0:WM], AF.Identity, [ps.b, cond.b], [tm.b], scale=cv(l, 2, dc, s))
                            p.op("dve", (lambda e, dc=dc, tm=tm: e.scalar_tensor_tensor(out=xt[:, dc, :], in0=xt[:, dc, :], scalar=ALPHA, in1=tm[:],
                                                                                  op0=ALU.mult, op1=ALU.add)),
                                 reads=[xt.b, tm.b] if dc == 0 else [tm.b], writes=[xt.b] if dc == 0 else [], joins=[] if dc == 0 else [xt.b])
                        ln_fm(xt, 8, on1024, lnr, WM)
                        ln_out_store(xt, 0, 1, l, dst, tok0, WM)
                return run
            p.run_threads([mkM(0), mkM(1)], head=HEAD_M)
            p.barrier()

    ST = 1024
    NTI = ST // 128

    def moe(src, dst, l):
        with ExitStack() as st:
            wr_sb = sb(st, "wr_sb", [128, 8, 20], F32)
            DMA("sp", wr_sb[:], wr_d[l, :, :, :], [], [wr_sb.b], wr_sb.b)
            brB = sb(st, "brB", [128, 20], F32)
            DMA("sp", brB[:], br_d[l, 0:1, :].to_broadcast([128, 20]), [], [brB.b], brB.b)
            hT = sb(st, "hTm", [128, 8, ST], BF16)
            yacc_t = sb(st, "yacc", [128, 2, 8, TT], F32).t
            yb_ = [[p.buf("yacc%d_%d" % (a, b)) for b in range(8)] for a in range(2)]
            wgs = Ring([sb(st, "wg%d" % i, [128, 8, 512], BF16) for i in range(2)])
            wus = Ring([sb(st, "wu%d" % i, [128, 8, 512], BF16) for i in range(2)])
            wds = Ring([sb(st, "wd%d" % i, [128, 4, 1024], BF16) for i in range(2)])
            xt = sb(st, "xtm", [128, 8, TT], F32)
            hf = sb(st, "hf", [128, 8, 128], F32)
            acts = Ring([sb(st, "act%d" % i, [128, 4, TT], BF16) for i in range(2)])
            sgs = Ring([sb(st, "sgm%d" % i, [128, TT], F32) for i in range(2)])
            t1s = Ring([sb(st, "t1m%d" % i, [128, TT], F32) for i in range(2)])
            lnr = {"rb": sb(st, "lnm_rb", [128, 8, TT], BF16), "sq": sb(st, "lnm_sq", [128, 8, TT], BF16), "sd": sb(st, "lnm_sd", [128, TT], F32), "mu": sb(st, "lnm_mu", [128, TT], F32)}
            L = sb(st, "Lrt", [128, NTI, 20], F32)
            cwT = sb(st, "cwT", [16, ST], F32)

            def rt(name, shape):
                return sb(st, "rt_" + name, shape, F32)
            gmax = rt("gmax", [128, NTI]); gsel = rt("gsel", [128, NTI, 4]); gd = rt("gd", [128, NTI, 4])
            gsum = rt("gsum", [128, NTI]); gw = rt("gw", [128, NTI]); tmp4 = rt("tmp4", [128, NTI, 4, 4])
            ig = rt("ig", [128, NTI, 4]); m1 = rt("m1", [128, NTI]); oh1 = rt("oh1", [128, NTI, 4])
            ig2 = rt("ig2", [128, NTI, 4]); m2 = rt("m2", [128, NTI]); oh2 = rt("oh2", [128, NTI, 4])
            dd = rt("dd", [128, NTI]); w1 = rt("w1", [128, NTI]); w2 = rt("w2", [128, NTI])
            a1 = rt("a1", [128, NTI, 4]); a2 = rt("a2", [128, NTI, 4]); cw = rt("cw", [128, NTI, 4, 4])

            def bc3(t):
                return t[:].unsqueeze(2).to_broadcast([128, NTI, 4])

            for sti in range(TOK // ST):
                s = (sti * ST) // SEQ
                base = sti * ST
                for tt in range(ST // TT):
                    load_x(src, base + tt * TT, xt)
                    for dc in range(8):
                        p.op("act", (lambda e, o=hT[:, dc, tt * TT:(tt + 1) * TT], i=xt[:, dc, :], b=cv(l, 3, dc, s), sc=cv(l, 4, dc, s):
                                     e.activation(out=o, in_=i, func=AF.Identity, bias=b, scale=sc)),
                             reads=[xt.b, cond.b], writes=[hT.b] if (dc == 0 and tt == 0) else [],
                             joins=[] if (dc == 0 and tt == 0) else [hT.b])
                    for sub in range(4):
                        for dc in range(8):
                            p.op("dve", (lambda e, o=hf[:, dc, :], i=xt[:, dc, sub * 128:(sub + 1) * 128], b=cv(l, 3, dc, s), sc=cv(l, 4, dc, s):
                                         e.tensor_scalar(out=o, in0=i, scalar1=sc, scalar2=b, op0=ALU.mult, op1=ALU.add)),
                                 reads=[xt.b, cond.b], writes=[hf.b] if dc == 0 else [], joins=[] if dc == 0 else [hf.b])
                        lps = PC.next()
                        for kc in range(8):
                            MM(lps[:, 0:20], hf[:, kc, :], wr_sb[:, kc, :], kc == 0, kc == 7, [hf.b, wr_sb.b], lps.b)
                        i16 = tt * 4 + sub
                        p.op("dve", (lambda e, o=L[:, i16, :], a=lps[:, 0:20]: e.tensor_tensor(out=o, in0=a, in1=brB[:], op=ALU.add)),
                             reads=[lps.b, brB.b], writes=[L.b] if i16 == 0 else [], joins=[] if i16 == 0 else [L.b])
                Lg = L[:, :, 0:4]
                Le = L[:, :, 4:20].rearrange("p t (g j) -> p t g j", g=4)
                RED(gmax[:], Lg, ALU.max, [L.b], [gmax.b])
                TTo("dve", gsel[:], Lg, bc3(gmax), ALU.is_equal, [L.b, gmax.b], [gsel.b])
                TTo("dve", gd[:], Lg, bc3(gmax), ALU.subtract, [L.b, gmax.b], [gd.b])
                ACT(gd[:], gd[:], AF.Exp, [gd.b], [gd.b])
                RED(gsum[:], gd[:], ALU.add, [gd.b], [gsum.b])
                RECIP(gw[:], gsum[:], [gsum.b], [gw.b])
                TTo("dve", tmp4[:], Le, gsel[:].unsqueeze(3).to_broadcast([128, NTI, 4, 4]), ALU.mult, [L.b, gsel.b], [tmp4.b])
                RED(ig[:], tmp4[:].rearrange("p t g j -> p t j g"), ALU.add, [tmp4.b], [ig.b])
                RED(m1[:], ig[:], ALU.max, [ig.b], [m1.b])
                TTo("dve", oh1[:], ig[:], bc3(m1), ALU.is_equal, [ig.b, m1.b], [oh1.b])
                STT(ig2[:], oh1[:], NEG, ig[:], ALU.mult, ALU.add, [oh1.b, ig.b], [ig2.b])
                RED(m2[:], ig2[:], ALU.max, [ig2.b], [m2.b])
                TTo("dve", oh2[:], ig2[:], bc3(m2), ALU.is_equal, [ig2.b, m2.b], [oh2.b])
                TTo("dve", dd[:], m2[:], m1[:], ALU.subtract, [m1.b, m2.b], [dd.b])
                ACT(dd[:], dd[:], AF.Exp, [dd.b], [dd.b])
                TS("dve", w1[:], dd[:], 1.0, None, ALU.add, None, [dd.b], [w1.b])
                RECIP(w1[:], w1[:], [w1.b], [w1.b])
                TTo("dve", w2[:], dd[:], w1[:], ALU.mult, [dd.b, w1.b], [w2.b])
                TTo("dve", w1[:], w1[:], gw[:], ALU.mult, [w1.b, gw.b], [w1.b])
                TTo("dve", w2[:], w2[:], gw[:], ALU.mult, [w2.b, gw.b], [w2.b])
                TTo("dve", a1[:], oh1[:], bc3(w1), ALU.mult, [oh1.b, w1.b], [a1.b])
                TTo("dve", a2[:], oh2[:], bc3(w2), ALU.mult, [oh2.b, w2.b], [a2.b])
                TTo("dve", a1[:], a1[:], a2[:], ALU.add, [a1.b, a2.b], [a1.b])
                TTo("dve", cw[:], gsel[:].unsqueeze(3).to_broadcast([128, NTI, 4, 4]),
                    a1[:].unsqueeze(2).to_broadcast([128, NTI, 4, 4]), ALU.mult, [gsel.b, a1.b], [cw.b])
                for i in range(NTI):
                    tp = PC.next()
                    p.op("pe", (lambda e, tp=tp, i=i: e.transpose(tp[0:16, 0:128], cw[:, i, :, :].rearrange("p g j -> p (g j)"), ident)),
                         reads=[cw.b, cst.b], writes=[tp.b])
                    p.op("act", (lambda e, tp=tp, i=i: e.activation(out=cwT[0:16, i * 128:(i + 1) * 128], in_=tp[0:16, 0:128], func=AF.Identity)),
                         reads=[tp.b], writes=[cwT.b] if i == 0 else [], joins=[] if i == 0 else [cwT.b])
                for ex in range(16):
                    wg, wu, wd = wgs.next(), wus.next(), wds.next()
                    DMA("pool", wg[:], w_gate[l, ex], [], [wg.b], wg.b)
                    DMA("pool", wu[:], w_up[l, ex], [], [wu.b], wu.b)
                    DMA("pool", wd[:], w_down[l, ex], [], [wd.b], wd.b)
                    for tt in range(ST // TT):
                        cps = PC.next()
                        MM(cps[:], sel[:, ex * 128:(ex + 1) * 128], cwT[0:16, tt * TT:(tt + 1) * TT], True, True, [cst.b, cwT.b], cps.b)
                        act = acts.next()
                        for fc in range(4):
                            gps = PA.next()
                            for kc in range(8):
                                MM(gps[:], wg[:, kc, fc * 128:(fc + 1) * 128], hT[:, kc, tt * TT:(tt + 1) * TT], kc == 0, kc == 7, [wg.b, hT.b], gps.b)
                            ups = PA.next()
                            for kc in range(8):
                                MM(ups[:], wu[:, kc, fc * 128:(fc + 1) * 128], hT[:, kc, tt * TT:(tt + 1) * TT], kc == 0, kc == 7, [wu.b, hT.b], ups.b)
                            sg = sgs.next()
                            t1 = t1s.next()
                            ACT(sg[:], gps[:], AF.Silu, [gps.b], [sg.b])
                            TTo("dve", t1[:], sg[:], ups[:], ALU.mult, [sg.b, ups.b], [t1.b])
                            p.op("dve", (lambda e, o=act[:, fc, :], a=t1[:], b=cps[:]: e.tensor_tensor(out=o, in0=a, in1=b, op=ALU.mult)),
                                 reads=[t1.b, cps.b], writes=[act.b] if fc == 0 else [], joins=[] if fc == 0 else [act.b])
                        for dc in range(8):
                            dps = PB.next()
                            for fc in range(4):
                                MM(dps[:], wd[:, fc, dc * 128:(dc + 1) * 128], act[:, fc, :], fc == 0, fc == 3, [wd.b, act.b], dps.b)
                            if ex == 0:
                                p.op("act", (lambda e, o=yacc_t[:, tt, dc, :], i=dps[:]: e.activation(out=o, in_=i, func=AF.Identity)),
                                     reads=[dps.b], writes=[yb_[tt][dc]])
                            else:
                                p.op("dve", (lambda e, o=yacc_t[:, tt, dc, :], i=dps[:]: e.tensor_tensor(out=o, in0=o, in1=i, op=ALU.add)),
                                     reads=[dps.b], writes=[yb_[tt][dc]])
                for tt in range(ST // TT):
                    tok0 = base + tt * TT
                    load_x(src, tok0, xt)
                    for dc in range(8):
                        tm = t1s.next()
                        ACT(tm[:], yacc_t[:, tt, dc, :], AF.Identity, [yb_[tt][dc], cond.b], [tm.b], scale=cv(l, 5, dc, s))
                        p.op("dve", (lambda e, dc=dc, tm=tm: e.scalar_tensor_tensor(out=xt[:, dc, :], in0=xt[:, dc, :], scalar=ALPHA, in1=tm[:],
                                                                              op0=ALU.mult, op1=ALU.add)),
                             reads=[xt.b, tm.b] if dc == 0 else [tm.b], writes=[xt.b] if dc == 0 else [], joins=[] if dc == 0 else [xt.b])
                    ln_fm(xt, 8, on1024, lnr)
                    ln_out_store(xt, 2, 3, l, dst, tok0)
            p.barrier()

    T_S = 512
    NTL = 31
    S_ROWS = NTL * T_S
    Hs = nc.dram_tensor("Hs", [S_ROWS, D], BF16, kind="Internal").ap()
    Ys = nc.dram_tensor("Ys", [S_ROWS, D], F32, kind="Internal").ap()

    dynreg = {}

    def moe_sparse(src, dst, l, zero_fill):
        NI = TOK // 128
        with ExitStack() as st:
            slots_i = sb(st, "slots_i", [128, NI, 2], I32)
            wgt = sb(st, "wgt", [128, NI, 2], F32)
            te_i = sb(st, "te_i", [128, 32], I32)
            widx = sb(st, "widx", [128, 32, 2], I32)
            HsB = p.buf("HsB")
            ZFB = p.buf("ZFB")
            with ExitStack() as sa:
                wr_sb = sb(sa, "wr_sb", [128, 8, 20], F32)
                DMA("sp", wr_sb[:], wr_d[l, :, :, :], [], [wr_sb.b], wr_sb.b)
                brB = sb(sa, "brB", [128, 20], F32)
                DMA("sp", brB[:], br_d[l, 0:1, :].to_broadcast([128, 20]), [], [brB.b], brB.b)
                htok = sb(sa, "htok", [128, NI, D], BF16)
                L = sb(sa, "LA", [128, NI, 20], F32)
                if zero_fill:
                    zt = sb(sa, "zt", [128, 4, D], BF16)
                    MSET("pool", zt[:].rearrange("p a n -> p (a n)"), 0.0, [zt.b])
                    for a in range(S_ROWS // 512):
                        p.op("sp", (lambda e, a=a: e.dma_start(out=Hs[a * 512:(a + 1) * 512, :].rearrange("(a p) n -> p a n", p=128), in_=zt[:])),
                             reads=[zt.b], joins=[ZFB], dma=ZFB)
                def mkA(tid, nth):
                    xt = sb(sa, "xtA", [128, 8, TT], F32)
                    hT = sb(sa, "hTA", [128, 8, TT], BF16)
                    hf = sb(sa, "hfA", [128, 8, 128], F32)

                    def run():
                        for t8 in range(tid, TOK // TT, nth):
                            s = (t8 * TT) // SEQ
                            load_x(src, t8 * TT, xt)
                            for dc in range(8):
                                p.op("act", (lambda e, o=hT[:, dc, :], i=xt[:, dc, :], b=cv(l, 3, dc, s), sc=cv(l, 4, dc, s):
                                             e.activation(out=o, in_=i, func=AF.Identity, bias=b, scale=sc)),
                                     reads=[xt.b, cond.b], writes=[hT.b] if dc == 0 else [], joins=[] if dc == 0 else [hT.b])
                            for sub in range(4):
                                i = t8 * 4 + sub
                                for dc in range(8):
                                    p.op("dve", (lambda e, o=hf[:, dc, :], i_=xt[:, dc, sub * 128:(sub + 1) * 128], b=cv(l, 3, dc, s), sc=cv(l, 4, dc, s):
                                                 e.tensor_scalar(out=o, in0=i_, scalar1=sc, scalar2=b, op0=ALU.mult, op1=ALU.add)),
                                         reads=[xt.b, cond.b], writes=[hf.b] if dc == 0 else [], joins=[] if dc == 0 else [hf.b])
                                lps = PC.next()
                                for kc in range(8):
                                    MM(lps[:, 0:20], hf[:, kc, :], wr_sb[:, kc, :], kc == 0, kc == 7, [hf.b, wr_sb.b], lps.b)
                                p.op("dve", (lambda e, o=L[:, i, :], a=lps[:, 0:20]: e.tensor_tensor(out=o, in0=a, in1=brB[:], op=ALU.add)),
                                     reads=[lps.b, brB.b], joins=[L.b])
                                tp = PA.next()
                                tpb = tp.t[:].bitcast(BF16)
                                for kc in range(8):
                                    p.op("pe", (lambda e, o=tpb[:, kc * 128:(kc + 1) * 128], a=hT[:, kc, sub * 128:(sub + 1) * 128]: e.transpose(o, a, identb[:])),
                                         reads=[hT.b, identb.b], writes=[tp.b] if kc == 0 else [], joins=[] if kc == 0 else [tp.b])
                                p.op("act", (lambda e, o=htok[:, i, :], a=tpb[:, 0:1024]: e.activation(out=o, in_=a, func=AF.Identity)),
                                     reads=[tp.b], joins=[htok.b])

                    return run
                p.run_threads([mkA(0, NTHR), mkA(1, NTHR)] if NTHR == 2 else [mkA(0, 1)], head=HEAD_A)

                def rt(name, shape, dt=F32):
                    return sb(sa, "rs_" + name, shape, dt)
                gmax = rt("gmax", [128, NI]); gsel = rt("gsel", [128, NI, 4]); gd = rt("gd", [128, NI, 4])
                gsum = rt("gsum", [128, NI]); gw = rt("gw", [128, NI]); tmp4 = rt("tmp4", [128, NI, 4, 4])
                ig = rt("ig", [128, NI, 4]); m1_ = rt("m1", [128, NI]); oh1 = rt("oh1", [128, NI, 4])
                ig2 = rt("ig2", [128, NI, 4]); m2_ = rt("m2", [128, NI]); oh2 = rt("oh2", [128, NI, 4])
                dd = rt("dd", [128, NI]); w1 = rt("w1", [128, NI]); w2 = rt("w2", [128, NI])
                M1 = rt("M1", [128, NI, 4, 4]); M2 = rt("M2", [128, NI, 4, 4]); Mm = rt("Mm", [128, NI, 16])
                mb = rt("mb", [128, NI, 16], BF16); Mp = rt("Mp", [128, NI + 1, 16], BF16)
                rank = rt("rank", [128, NI, 16]); cnt = rt("cnt", [128, 16]); cmp8 = rt("cmp8", [128, 16, 8])
                ncap = rt("ncap", [128, 16]); cap = rt("cap", [128, 16]); sa_ = rt("sa", [128, 16]); sb_ = rt("sb", [128, 16])
                start = rt("start", [128, 16]); pos = rt("pos", [128, NI, 16]); tmpp = rt("tmpp", [128, NI, 16])
                slots_f = rt("slots_f", [128, NI, 2]); cmpj = rt("cmpj", [128, 32, 16]); tef = rt("tef", [128, 32])

                def bc3(t):
                    return t[:].unsqueeze(2).to_broadcast([128, NI, 4])
                Lg = L[:, :, 0:4]
                Le = L[:, :, 4:20].rearrange("p t (g j) -> p t g j", g=4)
                RED(gmax[:], Lg, ALU.max, [L.b], [gmax.b])
                TTo("dve", gsel[:], Lg, bc3(gmax), ALU.is_equal, [L.b, gmax.b], [gsel.b])
                TTo("dve", gd[:], Lg, bc3(gmax), ALU.subtract, [L.b, gmax.b], [gd.b])
                ACT(gd[:], gd[:], AF.Exp, [gd.b], [gd.b])
                RED(gsum[:], gd[:], ALU.add, [gd.b], [gsum.b])
                RECIP(gw[:], gsum[:], [gsum.b], [gw.b])
                TTo("dve", tmp4[:], Le, gsel[:].unsqueeze(3).to_broadcast([128, NI, 4, 4]), ALU.mult, [L.b, gsel.b], [tmp4.b])
                RED(ig[:], tmp4[:].rearrange("p t g j -> p t j g"), ALU.add, [tmp4.b], [ig.b])
                RED(m1_[:], ig[:], ALU.max, [ig.b], [m1_.b])
                TTo("dve", oh1[:], ig[:], bc3(m1_), ALU.is_equal, [ig.b, m1_.b], [oh1.b])
                STT(ig2[:], oh1[:], NEG, ig[:], ALU.mult, ALU.add, [oh1.b, ig.b], [ig2.b])
                RED(m2_[:], ig2[:], ALU.max, [ig2.b], [m2_.b])
                TTo("dve", oh2[:], ig2[:], bc3(m2_), ALU.is_equal, [ig2.b, m2_.b], [oh2.b])
                TTo("dve", dd[:], m2_[:], m1_[:], ALU.subtract, [m1_.b, m2_.b], [dd.b])
                ACT(dd[:], dd[:], AF.Exp, [dd.b], [dd.b])
                TS("dve", w1[:], dd[:], 1.0, None, ALU.add, None, [dd.b], [w1.b])
                RECIP(w1[:], w1[:], [w1.b], [w1.b])
                TTo("dve", w2[:], dd[:], w1[:], ALU.mult, [dd.b, w1.b], [w2.b])
                TTo("dve", wgt[:, :, 0], w1[:], gw[:], ALU.mult, [w1.b, gw.b], [wgt.b])
                p.op("dve", lambda e: e.tensor_tensor(out=wgt[:, :, 1], in0=w2[:], in1=gw[:], op=ALU.mult), reads=[w2.b, gw.b], joins=[wgt.b])
                TTo("dve", M1[:], gsel[:].unsqueeze(3).to_broadcast([128, NI, 4, 4]), oh1[:].unsqueeze(2).to_broadcast([128, NI, 4, 4]), ALU.mult,
                    [gsel.b, oh1.b], [M1.b])
                TTo("dve", M2[:], gsel[:].unsqueeze(3).to_broadcast([128, NI, 4, 4]), oh2[:].unsqueeze(2).to_broadcast([128, NI, 4, 4]), ALU.mult,
                    [gsel.b, oh2.b], [M2.b])
                M1v = M1[:].rearrange("p t g j -> p t (g j)")
                M2v = M2[:].rearrange("p t g j -> p t (g j)")
                TTo("dve", Mm[:], M1v, M2v, ALU.add, [M1.b, M2.b], [Mm.b])
                CP("dve", mb[:], Mm[:], [Mm.b], [mb.b])
                MSET("dve", Mp[:, 0, :], 0.0, [Mp.b])
                for i in range(NI):
                    p.op("dve", (lambda e, i=i: e.tensor_tensor(out=Mp[:, i + 1, :], in0=Mp[:, i, :], in1=mb[:, i, :], op=ALU.add)),
                         reads=[mb.b], writes=[Mp.b])
                rps = PB.next()
                for i in range(NI):
                    p.op("pe", (lambda e, i=i: e.matmul(rps[:, i * 16:(i + 1) * 16], ustrb[:], mb[:, i, :], start=True, stop=False)),
                         reads=[ustrb.b, mb.b], writes=[rps.b] if i == 0 else [], joins=[] if i == 0 else [rps.b])
                    p.op("pe", (lambda e, i=i: e.matmul(rps[:, i * 16:(i + 1) * 16], onesb[:], Mp[:, i, :], start=False, stop=True)),
                         reads=[onesb.b, Mp.b], joins=[rps.b])
                ACT(rank[:].rearrange("p t e -> p (t e)"), rps[:], AF.Identity, [rps.b], [rank.b])
                cps_ = PC.next()
                MM(cps_[:, 0:16], onesb[:], Mp[:, NI, :], True, True, [onesb.b, Mp.b], cps_.b)
                ACT(cnt[:], cps_[:, 0:16], AF.Identity, [cps_.b], [cnt.b])
                TTo("dve", cmp8[:], cnt[:].unsqueeze(2).to_broadcast([128, 16, 8]), thr8.unsqueeze(1).to_broadcast([128, 16, 8]), ALU.is_gt,
                    [cnt.b, cst.b], [cmp8.b])
                RED(ncap[:], cmp8[:], ALU.add, [cmp8.b], [ncap.b])
                TS("dve", cap[:], ncap[:], float(T_S), None, ALU.mult, None, [ncap.b], [cap.b])
                srcb, dstb = cap, sa_
                for sh in (1, 2, 4, 8):
                    p.op("dve", (lambda e, a=srcb, d_=dstb, sh=sh: e.tensor_copy(out=d_[:, 0:sh], in_=a[:, 0:sh])), reads=[srcb.b], writes=[dstb.b])
                    p.op("dve", (lambda e, a=srcb, d_=dstb, sh=sh: e.tensor_tensor(out=d_[:, sh:16], in0=a[:, sh:16], in1=a[:, 0:16 - sh], op=ALU.add)),
                         reads=[srcb.b], joins=[dstb.b])
                    srcb, dstb = dstb, (sb_ if dstb is sa_ else sa_)
                incl = srcb
                TTo("dve", start[:], incl[:], cap[:], ALU.subtract, [incl.b, cap.b], [start.b])
                TTo("dve", pos[:], rank[:], start[:].unsqueeze(1).to_broadcast([128, NI, 16]), ALU.add, [rank.b, start.b], [pos.b])
                TTo("dve", tmpp[:], M1v, pos[:], ALU.mult, [M1.b, pos.b], [tmpp.b])
                RED(slots_f[:, :, 0], tmpp[:], ALU.add, [tmpp.b], [slots_f.b])
                TTo("dve", tmpp[:], M2v, pos[:], ALU.mult, [M2.b, pos.b], [tmpp.b])
                p.op("dve", lambda e: e.tensor_reduce(out=slots_f[:, :, 1], in_=tmpp[:], axis=AX.X, op=ALU.add), reads=[tmpp.b], joins=[slots_f.b])
                CP("dve", slots_i[:], slots_f[:], [slots_f.b], [slots_i.b])
                TTo("dve", cmpj[:], incl[:].unsqueeze(1).to_broadcast([128, 32, 16]), jt32.unsqueeze(2).to_broadcast([128, 32, 16]), ALU.is_le,
                    [incl.b, cst.b], [cmpj.b])
                RED(tef[:], cmpj[:], ALU.add, [cmpj.b], [tef.b])
                TS("dve", tef[:], tef[:], 15.0, None, ALU.min, None, [tef.b], [tef.b])
                CP("dve", te_i[:], tef[:], [tef.b], [te_i.b])
                TS("dve", tef[:], tef[:], float(l * 16), 128.0, ALU.add, ALU.mult, [tef.b], [tef.b])
                TS("dve", tef[:], tef[:], pcol, 2.0, ALU.add, ALU.mult, [tef.b, cst.b], [tef.b])
                CP("dve", widx[:, :, 0], tef[:], [tef.b], [widx.b])
                TS("dve", tef[:], tef[:], 1.0, None, ALU.add, None, [tef.b], [tef.b])
                p.op("dve", lambda e: e.tensor_copy(out=widx[:, :, 1], in_=tef[:]), reads=[tef.b], joins=[widx.b])
                for i in range(NI):
                    for k in range(2):
                        p.op("pool", (lambda e, i=i, k=k: e.indirect_dma_start(
                            out=Hs[:, :], out_offset=bass.IndirectOffsetOnAxis(ap=slots_i[:, i, k:k + 1].bitcast(U32), axis=0),
                            in_=htok[:, i, :], in_offset=None)),
                            reads=[htok.b, slots_i.b, ZFB], joins=[HsB], dma=HsB)
                if debug:
                    d1 = nc.dram_tensor("dbg_slots", [128, NI, 2], I32, kind="ExternalOutput").ap()
                    DMA("sp", d1[:, :, :], slots_i[:], [slots_i.b], [], slots_i.b)
                    d2 = nc.dram_tensor("dbg_wgt", [128, NI, 2], F32, kind="ExternalOutput").ap()
                    DMA("sp", d2[:, :, :], wgt[:], [wgt.b], [], wgt.b)
                    d3 = nc.dram_tensor("dbg_te", [128, 32], I32, kind="ExternalOutput").ap()
                    DMA("sp", d3[:, :], te_i[:], [te_i.b], [], te_i.b)
                    d4 = nc.dram_tensor("dbg_mm", [128, NI, 16], F32, kind="ExternalOutput").ap()
                    DMA("sp", d4[:, :, :], Mm[:], [Mm.b], [], Mm.b)
                    d5 = nc.dram_tensor("dbg_rank", [128, NI, 16], F32, kind="ExternalOutput").ap()
                    DMA("sp", d5[:, :, :], rank[:], [rank.b], [], rank.b)
                    d6 = nc.dram_tensor("dbg_start", [128, 16], F32, kind="ExternalOutput").ap()
                    DMA("sp", d6[:, :], start[:], [start.b], [], start.b)
                p.barrier()
            if "B" not in MOE_DEBUG:
                return
            with ExitStack() as sbk:
                def mkB(tid, nth):
                    nb = 2 if nth == 1 else 1
                    wgs = Ring([sb(sbk, "swg%d" % i, [128, 8, 512], BF16) for i in range(2)])
                    wus = Ring([sb(sbk, "swu%d" % i, [128, 8, 512], BF16) for i in range(2)])
                    wds = Ring([sb(sbk, "swd%d" % i, [128, 4, 1024], BF16) for i in range(2)])
                    hrows = Ring([sb(sbk, "hrow%d" % i, [128, D], BF16) for i in range(4)])
                    hsTs = Ring([sb(sbk, "hsT%d" % i, [128, 8, T_S], BF16) for i in range(nb)])
                    acts = Ring([sb(sbk, "sact%d" % i, [128, 4, T_S], BF16) for i in range(nb)])
                    sgs = Ring([sb(sbk, "ssg%d" % i, [128, T_S], F32) for i in range(2)])
                    yrows = Ring([sb(sbk, "yrow%d" % i, [128, 512], F32) for i in range(4)])

                    def run():
                        for j in range(tid, NTL, nth):
                            wg, wu, wd = wgs.next(), wus.next(), wds.next()

                            for (dst_t, src5) in ((wg, w_gate), (wu, w_up), (wd, w_down)):
                                for hh in range(2):
                                    p.op("pool", (lambda e, dst_t=dst_t, src5=src5, j=j, hh=hh: e.indirect_dma_start(
                                        out=dst_t[:].rearrange("p (h k) n -> p h (k n)", h=2)[:, hh, :], out_offset=None,
                                        in_=src5.rearrange("l e p (h k) n -> (l e p h) (k n)", h=2),
                                        in_offset=bass.IndirectOffsetOnAxis(ap=widx[:, j, hh:hh + 1].bitcast(U32), axis=0))),
                                        reads=[widx.b], writes=[dst_t.b] if hh == 0 else [], joins=[] if hh == 0 else [dst_t.b], dma=dst_t.b)
                            hsT = hsTs.next()
                            for sub in range(4):
                                hr = hrows.next()
                                r0 = j * T_S + sub * 128
                                DMA("sp", hr[:], Hs[r0:r0 + 128, :], [HsB], [hr.b], hr.b)
                                tp = PA.next()
                                tpb = tp.t[:].bitcast(BF16)
                                for kc in range(8):
                                    p.op("pe", (lambda e, o=tpb[:, kc * 128:(kc + 1) * 128], a=hr[:, kc * 128:(kc + 1) * 128]: e.transpose(o, a, identb[:])),
                                         reads=[hr.b, identb.b], writes=[tp.b] if kc == 0 else [], joins=[] if kc == 0 else [tp.b])
                                eng = "act" if sub % 2 == 0 else "dve"
                                o_ = hsT[:, :, sub * 128:(sub + 1) * 128]
                                i_ = tpb[:, 0:1024].rearrange("p (k t) -> p k t", k=8)
                                if eng == "act":
                                    p.op("act", (lambda e, o_=o_, i_=i_: e.activation(out=o_, in_=i_, func=AF.Identity)),
                                         reads=[tp.b], writes=[hsT.b] if sub == 0 else [], joins=[] if sub == 0 else [hsT.b])
                                else:
                                    p.op("dve", (lambda e, o_=o_, i_=i_: e.tensor_copy(out=o_, in_=i_)),
                                         reads=[tp.b], writes=[hsT.b] if sub == 0 else [], joins=[] if sub == 0 else [hsT.b])
                            act = acts.next()
                            for fc in range(4):
                                gps = PA.next()
                                for kc in range(8):
                                    MM(gps[:], wg[:, kc, fc * 128:(fc + 1) * 128], hsT[:, kc, :], kc == 0, kc == 7, [wg.b, hsT.b], gps.b)
                                ups = PA.next()
                                for kc in range(8):
                                    MM(ups[:], wu[:, kc, fc * 128:(fc + 1) * 128], hsT[:, kc, :], kc == 0, kc == 7, [wu.b, hsT.b], ups.b)
                                sg = sgs.next()
                                ACT(sg[:], gps[:], AF.Silu, [gps.b], [sg.b])
                                p.op("dve", (lambda e, o=act[:, fc, :], a=sg[:], b=ups[:]: e.tensor_tensor(out=o, in0=a, in1=b, op=ALU.mult)),
                                     reads=[sg.b, ups.b], writes=[act.b] if fc == 0 else [], joins=[] if fc == 0 else [act.b])
                            for sub in range(4):
                                for dh in range(2):
                                    dps = (PB if dh == 0 else PC).next()
                                    for fc in range(4):
                                        MM(dps[:], act[:, fc, sub * 128:(sub + 1) * 128], wd[:, fc, dh * 512:(dh + 1) * 512], fc == 0, fc == 3, [wd.b, act.b], dps.b)
                                    yr = yrows.next()
                                    if dh == 0:
                                        ACT(yr[:], dps[:], AF.Identity, [dps.b], [yr.b])
                                    else:
                                        CP("dve", yr[:], dps[:], [dps.b], [yr.b])
                                    r0 = j * T_S + sub * 128
                                    DMA("pool", Ys[r0:r0 + 128, dh * 512:(dh + 1) * 512], yr[:], [yr.b], [], yr.b)
                    return run
                p.run_threads([mkB(0, NTHR), mkB(1, NTHR)] if NTHR == 2 else [mkB(0, 1)], head=HEAD_B)
                p.barrier()
            if "C" not in MOE_DEBUG:
                return
            with ExitStack() as sc_:
                def mk_thread(tid, nth):
                    g1s = Ring([sb(sc_, "g1_%d" % i, [128, D], F32) for i in range(2)])
                    g2s = Ring([sb(sc_, "g2_%d" % i, [128, D], F32) for i in range(2)])
                    yT = sb(sc_, "yTc", [128, 8, TT], F32)
                    xt = sb(sc_, "xtC", [128, 8, TT], F32)
                    tmpc = Ring([sb(sc_, "tmpC%d" % i, [128, TT], F32) for i in range(2)])
                    lnr = {"rb": sb(sc_, "lnC_rb", [128, 8, TT], BF16), "sq": sb(sc_, "lnC_sq", [128, 8, TT], BF16), "sd": sb(sc_, "lnC_sd", [128, TT], F32), "mu": sb(sc_, "lnC_mu", [128, TT], F32)}

                    def run():
                        for t8 in range(tid, TOK // TT, nth):
                            s = (t8 * TT) // SEQ
                            tok0 = t8 * TT
                            load_x(src, tok0, xt)
                            for sub in range(4):
                                i = t8 * 4 + sub
                                g1, g2 = g1s.next(), g2s.next()
                                p.op("pool", (lambda e, g1=g1, i=i: e.indirect_dma_start(
                                    out=g1[:], out_offset=None, in_=Ys[:, :],
                                    in_offset=bass.IndirectOffsetOnAxis(ap=slots_i[:, i, 0:1].bitcast(U32), axis=0))),
                                    reads=[slots_i.b], writes=[g1.b], dma=g1.b)
                                p.op("pool", (lambda e, g2=g2, i=i: e.indirect_dma_start(
                                    out=g2[:], out_offset=None, in_=Ys[:, :],
                                    in_offset=bass.IndirectOffsetOnAxis(ap=slots_i[:, i, 1:2].bitcast(U32), axis=0))),
                                    reads=[slots_i.b], writes=[g2.b], dma=g2.b)
                                ACT(g1[:], g1[:], AF.Identity, [g1.b, wgt.b], [g1.b], scale=wgt[:, i, 0:1])
                                STT(g1[:], g2[:], wgt[:, i, 1:2], g1[:], ALU.mult, ALU.add, [g2.b, wgt.b, g1.b], [g1.b])
                                for half in range(2):
                                    tp = PA.next()
                                    for q in range(4):
                                        dc = half * 4 + q
                                        p.op("pe", (lambda e, o=tp[:, q * 128:(q + 1) * 128], a=g1[:, dc * 128:(dc + 1) * 128]: e.transpose(o, a, ident)),
                                             reads=[g1.b, cst.b], writes=[tp.b] if q == 0 else [], joins=[] if q == 0 else [tp.b])
                                    first = (sub == 0 and half == 0)
                                    p.op("act", (lambda e, o=yT[:, half * 4:(half + 1) * 4, sub * 128:(sub + 1) * 128], a=tp[:].rearrange("p (q t) -> p q t", q=4):
                                                 e.activation(out=o, in_=a, func=AF.Identity)),
                                         reads=[tp.b], writes=[yT.b] if first else [], joins=[] if first else [yT.b])
                            for dc in range(8):
                                tm = tmpc.next()
                                ACT(tm[:], yT[:, dc, :], AF.Identity, [yT.b, cond.b], [tm.b], scale=cv(l, 5, dc, s))
                                p.op("dve", (lambda e, dc=dc, tm=tm: e.scalar_tensor_tensor(out=xt[:, dc, :], in0=xt[:, dc, :], scalar=ALPHA, in1=tm[:],
                                                                                      op0=ALU.mult, op1=ALU.add)),
                                     reads=[xt.b, tm.b] if dc == 0 else [tm.b], writes=[xt.b] if dc == 0 else [], joins=[] if dc == 0 else [xt.b])
                            ln_fm(xt, 8, on1024, lnr)
                            ln_out_store(xt, 2, 3, l, dst, tok0)
                    return run
                p.run_threads([mk_thread(0, NTHR), mk_thread(1, NTHR)] if NTHR == 2 else [mk_thread(0, 1)], head=HEAD_C)
                p.barrier()

    SCALE = 128.0 ** -0.5

    def attn(src, dst, l):
        with ExitStack() as st:
            wout = sb(st, "wo_at", [128, 8, 1024], BF16)
            for j in range(2):
                p.op("pool", (lambda e, j=j: e.dma_start(out=wout[:, :, j * 512:(j + 1) * 512],
                                                          in_=od_w_out[:, j * 512:(j + 1) * 512].rearrange("(kc p) n -> p kc n", p=128))),
                     writes=[wout.b] if j == 0 else [], joins=[] if j == 0 else [wout.b], dma=wout.b)
            if "w" in ATTN_DEBUG:
                kT = sb(st, "kT", [128, 8, SEQ], BF16)
                qT = sb(st, "qT", [128, 8, SEQ], BF16)
            else:
                qT = sb(st, "qT", [128, 8, SEQ], BF16)
                kT = sb(st, "kT", [128, 8, SEQ], BF16)
            v1 = sb(st, "v1", [128, 16, 8, 132], BF16)
            oT = sb(st, "oT", [128, 8, SEQ], BF16)
            ksum = sb(st, "ksum", [128, 8, 8], F32)
            gate = sb(st, "gate", [128, 16, 8, 8], F32)
            selt = sb(st, "selt", [128, 16, 8, 8], F32)
            MSET("pool", v1[:].rearrange("p a b c -> p (a b c)"), 1.0, [v1.b])
            def do_seq(s):
                with ExitStack() as sa:
                    xt = sb(sa, "xta", [128, 8, TT], F32)
                    wr2 = Ring([sb(sa, "wqkv%d" % i, [128, 8, 512], BF16) for i in range(2)])
                    qf = Ring([sb(sa, "qf%d" % i, [128, TT], F32) for i in range(2)])
                    top8 = sb(sa, "top8", [128, 2, 8, 8], F32)
                    for t in range(4):
                        load_x(src, s * SEQ + t * TT, xt)
                        for dc in range(8):
                            first = (t == 0 and dc == 0)
                            p.op("act", (lambda e, o=oT[:, dc, t * TT:(t + 1) * TT], i=xt[:, dc, :], b=cv(l, 0, dc, s), sc=cv(l, 1, dc, s):
                                         e.activation(out=o, in_=i, func=AF.Identity, bias=b, scale=sc)),
                                 reads=[xt.b, cond.b], writes=[oT.b] if first else [], joins=[] if first else [oT.b])
                    for which, half in ((1, 0), (1, 1), (0, 0), (0, 1), (2, 0), (2, 1)):
                        if "kqv"[which] not in ATTN_DEBUG + "kqv" * ("A" not in ATTN_DEBUG or "x" not in ATTN_DEBUG):
                            continue
                        w = wr2.next()
                        c0 = which * 1024 + half * 512
                        if "m" in ATTN_DEBUG:
                            c0 = 1024 + half * 512
                        DMA("pool", w[:], od_w_qkv[:, c0:c0 + 512].rearrange("(kc p) n -> p kc n", p=128), [], [w.b], w.b)
                        for t in range(4):
                            if which in (0, 1):
                                for j in range(4):
                                    h = half * 4 + j
                                    ps = PA.next()
                                    for kc in range(8):
                                        MM(ps[:], w[:, kc, j * 128:(j + 1) * 128], oT[:, kc, t * TT:(t + 1) * TT], kc == 0, kc == 7, [w.b, oT.b], ps.b)
                                    if which == 1:
                                        p.op("act", (lambda e, o=kT[:, h, t * TT:(t + 1) * TT], i=ps[:]: e.activation(out=o, in_=i, func=AF.Identity)),
                                             reads=[ps.b], joins=[kT.b])
                                        p.op("dve", (lambda e, o=ksum[:, h, 2 * t:2 * t + 2], i=ps[:].rearrange("p (b k) -> p b k", b=2):
                                                     e.tensor_reduce(out=o, in_=i, axis=AX.X, op=ALU.add)),
                                             reads=[ps.b], joins=[ksum.b])
                                    else:
                                        q32 = qf.next()
                                        if "n" not in ATTN_DEBUG:
                                            ACT(q32[:], ps[:], AF.Identity, [ps.b], [q32.b])
                                        p.op("act", (lambda e, o=qT[:, h, t * TT:(t + 1) * TT], i=ps[:]: e.activation(out=o, in_=i, func=AF.Identity)),
                                             reads=[ps.b], joins=[qT.b])
                                        if "x" in ATTN_DEBUG and "g" not in ATTN_DEBUG:
                                            continue
                                        gp = PC.next()
                                        for sub in range(4):
                                            p.op("pe", (lambda e, gp=gp, sub=sub, q32=q32, h=h: e.matmul(gp[:, sub * 8:sub * 8 + 8], q32[:, sub * 128:(sub + 1) * 128],
                                                                                                   ksum[:, h, :], start=True, stop=True)),
                                                 reads=[q32.b, ksum.b], writes=[gp.b] if sub == 0 else [], joins=[] if sub == 0 else [gp.b])
                                        p.op("dve", (lambda e, gp=gp, h=h, t=t: e.tensor_copy(out=gate[:, t * 4:t * 4 + 4, h, :],
                                                                                         in_=gp[:, 0:32].rearrange("p (a n) -> p a n", a=4))),
                                             reads=[gp.b], joins=[gate.b])
                            else:
                                for sub in range(4):
                                    ps = PA.next()
                                    for kc in range(8):
                                        MM(ps[:], oT[:, kc, (t * 4 + sub) * 128:(t * 4 + sub + 1) * 128], w[:, kc, :], kc == 0, kc == 7, [w.b, oT.b], ps.b)
                                    eng = "act" if sub % 2 == 0 else "dve"
                                    if eng == "act":
                                        p.op("act", (lambda e, ps=ps, kt=t * 4 + sub, half=half: e.activation(out=v1[:, kt, half * 4:half * 4 + 4, 0:128],
                                                                                               in_=ps[:].rearrange("p (h d) -> p h d", h=4), func=AF.Identity)),
                                             reads=[ps.b], joins=[v1.b])
                                    else:
                                        p.op("dve", (lambda e, ps=ps, kt=t * 4 + sub, half=half: e.tensor_copy(out=v1[:, kt, half * 4:half * 4 + 4, 0:128],
                                                                                                in_=ps[:].rearrange("p (h d) -> p h d", h=4))),
                                             reads=[ps.b], joins=[v1.b])
                    for b in range(4, 8):
                        if "x" in ATTN_DEBUG and "s" not in ATTN_DEBUG:
                            continue
                        p.op("dve", (lambda e, b=b: e.memset(gate[:, 2 * b:2 * b + 2, :, b:8], NEG)), reads=[], joins=[gate.b]) if False else \
                            p.op("dve", (lambda e, b=b: e.memset(gate[:, 2 * b:2 * b + 2, :, b:8], NEG)), writes=[gate.b])
                        for qq in range(2):
                            for h in range(8):
                                first = (qq == 0 and h == 0)
                                p.op("dve", (lambda e, b=b, qq=qq, h=h: e.max(out=top8[:, qq, h, :], in_=gate[:, 2 * b + qq, h, :])),
                                     reads=[gate.b], writes=[top8.b] if first else [], joins=[] if first else [top8.b])
                        p.op("dve", (lambda e, b=b: e.tensor_tensor(out=selt[:, 2 * b:2 * b + 2, :, :], in0=gate[:, 2 * b:2 * b + 2, :, :],
                                                                     in1=top8[:, :, :, 2:3].to_broadcast([128, 2, 8, 8]), op=ALU.is_ge)),
                             reads=[gate.b, top8.b], writes=[selt.b])
                    p.barrier()
                if "B" not in ATTN_DEBUG:
                    return
                with ExitStack() as sbk:
                    def mkAt(tid, nth):
                        Es = Ring([sb(sbk, "E%d" % i, [128, 256], BF16) for i in range(6)])
                        accs = Ring([sb(sbk, "acc%d" % i, [128, 132], F32) for i in range(4)])
                        rden = Ring([sb(sbk, "rden%d" % i, [128, 1], F32) for i in range(4)])
                        o32 = Ring([sb(sbk, "o32_%d" % i, [128, 128], F32) for i in range(4)])

                        def run():
                            for h in range(tid, 8, nth):
                                for b in range(8):
                                    dense = b < 4
                                    order = [b] + list(range(b))
                                    accp = [accs.next(), accs.next()]
                                    ops_ = [None, None]
                                    for idx, n in enumerate(order):
                                        own = (n == b)
                                        newgrp = (idx == 0) or (not dense)
                                        lastgrp = (idx == len(order) - 1) or (not dense)
                                        if newgrp:
                                            ops_ = [PB.next(), PC.next()]
                                        Et = []
                                        for kt in range(2):
                                            sp_ = PA.next()
                                            MM(sp_[:, 0:256], kT[:, h, (2 * n + kt) * 128:(2 * n + kt + 1) * 128], qT[:, h, b * 256:(b + 1) * 256], True, True, [kT.b, qT.b], sp_.b)
                                            E = Es.next()
                                            ACT(E[:], sp_[:, 0:256], AF.Exp, [sp_.b], [E.b], scale=SCALE)
                                            if own:
                                                c0 = kt * 128
                                                p.op("dve", (lambda e, E=E, c0=c0: e.tensor_tensor(out=E[:, c0:c0 + 128], in0=E[:, c0:c0 + 128], in1=trib[:], op=ALU.mult)),
                                                     reads=[trib.b], writes=[E.b])
                                            Et.append(E)
                                        for qs in range(2):
                                            kts = [kt for kt in range(2) if not (own and kt == 1 and qs == 0)]
                                            for kt in kts:
                                                first = newgrp and (kt == kts[0])
                                                last = lastgrp and (kt == kts[-1])
                                                MM(ops_[qs][:, 0:129], Et[kt][:, qs * 128:(qs + 1) * 128], v1[:, 2 * n + kt, h, 0:129], first, last, [Et[kt].b, v1.b], ops_[qs].b)
                                            if lastgrp:
                                                acc = accp[qs]
                                                if dense or own:
                                                    ACT(acc[:, 0:129], ops_[qs][:, 0:129], AF.Identity, [ops_[qs].b], [acc.b])
                                                else:
                                                    STT(acc[:, 0:129], ops_[qs][:, 0:129], selt[:, 2 * b + qs, h, n:n + 1], acc[:, 0:129], ALU.mult, ALU.add,
                                                        [ops_[qs].b, selt.b], [acc.b])
                                    for qs in range(2):
                                        acc = accp[qs]
                                        rd = rden.next()
                                        RECIP(rd[:], acc[:, 128:129], [acc.b], [rd.b])
                                        o = o32.next()
                                        TS("dve", o[:], acc[:, 0:128], rd[:, 0:1], None, ALU.mult, None, [acc.b, rd.b], [o.b])
                                        tp = PA.next()
                                        p.op("pe", (lambda e, tp=tp, o=o: e.transpose(tp[:, 0:128], o[:], ident)), reads=[o.b, cst.b], writes=[tp.b])
                                        qt = 2 * b + qs
                                        p.op("act", (lambda e, tp=tp, h=h, qt=qt: e.activation(out=oT[:, h, qt * 128:(qt + 1) * 128], in_=tp[:, 0:128], func=AF.Identity)),
                                             reads=[tp.b], joins=[oT.b])
                        return run
                    p.run_threads([mkAt(0, NTHR), mkAt(1, NTHR)] if NTHR == 2 else [mkAt(0, 1)], head=HEAD_AT)
                    if debug and s == 0:
                        dbo = nc.dram_tensor("dbg_oT", [128, 8, SEQ], BF16, kind="ExternalOutput").ap()
                        DMA("sp", dbo[:, :, :], oT[:], [oT.b], [], oT.b)
                        dbq = nc.dram_tensor("dbg_qT", [128, 8, SEQ], BF16, kind="ExternalOutput").ap()
                        DMA("sp", dbq[:, :, :], qT[:], [qT.b], [], qT.b)
                        dbk = nc.dram_tensor("dbg_kT", [128, 8, SEQ], BF16, kind="ExternalOutput").ap()
                        DMA("sp", dbk[:, :, :], kT[:], [kT.b], [], kT.b)
                        dbv = nc.dram_tensor("dbg_v1", [128, 16, 8, 132], BF16, kind="ExternalOutput").ap()
                        DMA("sp", dbv[:, :, :, :], v1[:], [v1.b], [], v1.b)
                    p.barrier()
                if "C" not in ATTN_DEBUG:
                    return
                with ExitStack() as sc_:
                    xt = sb(sc_, "xtc", [128, 8, TT], F32)
                    tmp = Ring([sb(sc_, "tmpc%d" % i, [128, TT], F32) for i in range(2)])
                    lnr = {"rb": sb(sc_, "lnc_rb", [128, 8, TT], BF16), "sq": sb(sc_, "lnc_sq", [128, 8, TT], BF16), "sd": sb(sc_, "lnc_sd", [128, TT], F32), "mu": sb(sc_, "lnc_mu", [128, TT], F32)}
                    for t in range(4):
                        tok0 = s * SEQ + t * TT
                        load_x(src, tok0, xt)
                        for dc in range(8):
                            ps = PA.next()
                            for kc in range(8):
                                MM(ps[:], wout[:, kc, dc * 128:(dc + 1) * 128], oT[:, kc, t * TT:(t + 1) * TT], kc == 0, kc == 7, [wout.b, oT.b], ps.b)
                            tm = tmp.next()
                            ACT(tm[:], ps[:], AF.Identity, [ps.b, cond.b], [tm.b], scale=cv(l, 2, dc, s))
                            p.op("dve", (lambda e, dc=dc, tm=tm: e.scalar_tensor_tensor(out=xt[:, dc, :], in0=xt[:, dc, :], scalar=ALPHA, in1=tm[:],
                                                                                  op0=ALU.mult, op1=ALU.add)),
                                 reads=[xt.b, tm.b] if dc == 0 else [tm.b], writes=[xt.b] if dc == 0 else [], joins=[] if dc == 0 else [xt.b])
                        ln_fm(xt, 8, on1024, lnr)
                        ln_out_store(xt, 0, 1, l, dst, tok0)
                    p.barrier()

            for s_ in range(NSEQ):
                do_seq(s_)

    if only == "attn":
        attn(xT, yT, 1)
        stages = 0
    def moe_any(src, dst, l, zero_fill):
        if SPARSE:
            moe_sparse(src, dst, l, zero_fill)
        else:
            moe(src, dst, l)

    if only == "moe":
        moe_any(xT, yT, 0, True)
        stages = 0
    if stages >= 1:
        mixer0(xT, yT if stages == 1 else XA, 0)
    if stages >= 2:
        moe_any(XA, yT if stages == 2 else XB, 0, True)
    if stages >= 3:
        attn(XB, yT if stages == 3 else XA, 1)
    if stages >= 4:
        moe_any(XA, yT, 1, False)

    p.barrier()
    p.emit()
    return nc, es


def prep_inputs(inputs):
    f = lambda a: np.ascontiguousarray(np.asarray(a, dtype=np.float32))
    x = f(inputs["x"])
    c = f(inputs["c"])
    shared = {}
    shared["ada_w"] = f(inputs["ada_w"])
    shared["ada_bT"] = f(inputs["ada_b"].reshape(2, 48, 128).transpose(2, 0, 1))
    lnp = np.stack([inputs["ln_mix_g"], inputs["ln_mix_b"], inputs["ln_ffn_g"], inputs["ln_ffn_b"]], 1)
    shared["lnp"] = f(lnp.reshape(2, 4, 8, 128).transpose(3, 0, 1, 2))
    shared["ev_w_in"] = f(inputs["ev_w_in"][0])
    shared["ev_w_out"] = f(inputs["ev_w_out"][0])
    sg = np.stack([inputs["ev_sgu_ln_g"][0], inputs["ev_sgu_ln_b"][0]], 0)
    shared["sgp"] = f(sg.reshape(2, 4, 128).transpose(2, 0, 1))
    shared["wsT"] = f(inputs["ev_w_s"][0].transpose(2, 0, 1))
    shared["bs"] = f(inputs["ev_b_s"][0].reshape(1, 512))
    shared["wdw"] = f(inputs["ev_w_dw"][0].reshape(31, 4, 128).transpose(2, 1, 0))
    cv = np.stack([inputs["ev_b_dw"][0], inputs["ev_conv_ln_g"][0], inputs["ev_conv_ln_b"][0]], 0)
    shared["cvp"] = f(cv.reshape(3, 4, 128).transpose(2, 0, 1))
    shared["od_w_qkv"] = f(inputs["od_w_qkv"][0])
    shared["od_w_out"] = f(inputs["od_w_out"][0])
    wr = np.concatenate([inputs["moe_w_grp"], inputs["moe_w_er"].transpose(0, 2, 1, 3).reshape(2, D, 16)], -1)
    shared["wr"] = f(wr.reshape(2, 8, 128, 20).transpose(0, 2, 1, 3))
    shared["br"] = f(np.concatenate([inputs["moe_b_grp"], inputs["moe_b_er"].reshape(2, 16)], -1).reshape(2, 1, 20))
    shared["moe_w_gate"] = f(np.asarray(inputs["moe_w_gate"]).reshape(2, 16, 8, 128, 512).transpose(0, 1, 3, 2, 4))
    shared["moe_w_up"] = f(np.asarray(inputs["moe_w_up"]).reshape(2, 16, 8, 128, 512).transpose(0, 1, 3, 2, 4))
    shared["moe_w_down"] = f(np.asarray(inputs["moe_w_down"]).reshape(2, 16, 4, 128, D).transpose(0, 1, 3, 2, 4))
    cst = np.zeros((128, 2473), np.float32)
    cst[:, 2472] = np.arange(128, dtype=np.float32)
    cst[:, 2432:2440] = np.arange(8, dtype=np.float32)[None, :] * 512.0
    cst[:, 2440:2472] = np.arange(32, dtype=np.float32)[None, :] * 512.0
    cst[:, 0:128] = np.eye(128, dtype=np.float32)
    cst[:, 128:256] = np.triu(np.ones((128, 128), np.float32))
    cst[:, 256:384] = 1.0
    for e in range(16):
        cst[e, 384 + e * 128:384 + (e + 1) * 128] = 1.0
    shared["consts"] = cst
    maps = []
    for core in range(NCORES):
        m = dict(shared)
        xs = x[core * NSEQ:(core + 1) * NSEQ]
        m["xT"] = f(xs.reshape(TOK, 8, 128).transpose(1, 2, 0))
        m["cT"] = f(c[core * NSEQ:(core + 1) * NSEQ].reshape(NSEQ, 8, 128).transpose(2, 1, 0))
        maps.append(m)
    return maps


_CACHE = {}


def kernel(**inputs):
    maps = prep_inputs(inputs)
    if "nc" not in _CACHE:
        _CACHE["nc"] = build()
    nc, _ = _CACHE["nc"]
    res = run_bass_kernel_spmd(nc, maps, core_ids=list(range(NCORES)))
    out = np.empty((NCORES * NSEQ, SEQ, D), np.float32)
    for core in range(NCORES):
        yT = np.asarray(res.results[core]["yT"])
        out[core * NSEQ:(core + 1) * NSEQ] = yT.transpose(2, 0, 1).reshape(NSEQ, SEQ, D)
    return out
```

```python
import threading
import numpy as np
from contextlib import ExitStack
import concourse.bass as bass
import concourse.mybir as mybir
from concourse.bass_utils import run_bass_kernel_spmd

F32 = mybir.dt.float32
BF16 = mybir.dt.bfloat16
I32 = mybir.dt.int32
U32 = mybir.dt.uint32
AF = mybir.ActivationFunctionType
ALU = mybir.AluOpType
AX = mybir.AxisListType

NCORES = 8
D = 1024
SEQ = 2048
NSEQ = 2
TOK = NSEQ * SEQ
TT = 512
ALPHA = 4.0 ** 0.25
EPS = 1e-5
NEG = -1.0e30
ATTN_DEBUG = "ABC"
SPARSE = True
MOE_DEBUG = "ABC"
NTHR = 2
HEAD_M = 250
HEAD_A = 56
HEAD_B = 83
HEAD_C = 70
HEAD_AT = 300

ENGS = ("pe", "act", "dve", "pool", "sp")


class Buf:
    __slots__ = ("name", "writers", "readers", "dsem", "dcount", "excl", "dslot")

    def __init__(self, name):
        self.name = name
        self.writers = []
        self.readers = []
        self.dsem = None
        self.dcount = 0
        self.excl = False
        self.dslot = None


class Op:
    __slots__ = ("eng", "fn", "waits", "is_dma", "is_nop", "dbuf", "value", "needed", "eidx", "semval", "epoch")

    def __init__(self, eng, fn):
        self.eng = eng
        self.fn = fn
        self.waits = []
        self.is_dma = False
        self.is_nop = False
        self.dbuf = None
        self.value = 0
        self.needed = False
        self.eidx = 0
        self.semval = 0
        self.epoch = 0


class Turns:
    def __init__(self, n):
        self.n = n
        self.cur = 0
        self.alive = [True] * n
        self.cv = threading.Condition()
        self.local = threading.local()
        self.head = 0

    def _advance(self):
        for k in range(1, self.n + 1):
            c = (self.cur + k) % self.n
            if self.alive[c]:
                self.cur = c
                return

    def start(self, tid):
        self.local.tid = tid
        with self.cv:
            while self.cur != tid:
                self.cv.wait()

    def yield_turn(self):
        tid = self.local.tid
        if tid == 0 and self.head > 0:
            self.head -= 1
            return
        with self.cv:
            self._advance()
            self.cv.notify_all()
            while self.cur != tid:
                self.cv.wait()

    def finish(self):
        tid = self.local.tid
        with self.cv:
            self.alive[tid] = False
            if any(self.alive):
                self._advance()
            self.cv.notify_all()


class Prog:
    def __init__(self, nc, es):
        self.nc = nc
        self.es = es
        self.ops = {e: [] for e in ENGS}
        self.clock = {e: {} for e in ENGS}
        self.esem = {}
        self.bufs = []
        self.same_engine_sync = True
        self.epoch = 0
        self.turns = None
        self.slots = []
        self.free_slots = []

    def buf(self, name):
        b = Buf(name)
        self.bufs.append(b)
        return b

    def _dep(self, op, d):
        E = op.eng
        if d.is_dma:
            key = ("d", id(d.dbuf))
            if self.clock[E].get(key, 0) >= d.value:
                return
            self.clock[E][key] = d.value
            op.waits.append(("d", d.dbuf, d.value))
        else:
            if d.is_nop:
                return
            if d.eng == E and (E == "pe" or not self.same_engine_sync):
                return
            key = ("e", d.eng)
            if self.clock[E].get(key, 0) >= d.eidx:
                return
            self.clock[E][key] = d.eidx
            d.needed = True
            op.waits.append(("e", d.eng, d))

    def op(self, eng, fn, reads=(), writes=(), joins=(), dma=None):
        o = Op(eng, fn)
        o.epoch = self.epoch
        lst = self.ops[eng]
        lst.append(o)
        o.eidx = len(lst)
        if dma is not None:
            o.is_dma = True
            if dma.dslot is None:
                if self.free_slots:
                    dma.dslot = self.free_slots.pop()
                else:
                    dma.dslot = Buf("slot%d" % len(self.slots))
                    self.slots.append(dma.dslot)
            sl = dma.dslot
            o.dbuf = sl
            sl.dcount += 16
            o.value = sl.dcount
        deps = []
        for b in reads:
            deps.extend(b.writers)
            if b.excl:
                deps.extend(r for r in b.readers if r.eng != eng)
        for b in writes:
            deps.extend(b.writers)
            deps.extend(b.readers)
        for b in joins:
            if b.readers:
                deps.extend(b.writers)
                deps.extend(b.readers)
        best = {}
        for d in deps:
            if d.is_dma:
                key = ("d", id(d.dbuf))
                v = d.value
            else:
                if d.is_nop:
                    continue
                key = ("e", d.eng)
                v = d.eidx
            if key not in best or best[key][0] < v:
                best[key] = (v, d)
        for key in best:
            self._dep(o, best[key][1])
        for b in reads:
            b.readers.append(o)
        for b in writes:
            b.writers = [o]
            b.readers = []
        for b in joins:
            if b.readers:
                b.writers = [o]
                b.readers = []
            else:
                b.writers.append(o)
        if self.turns is not None:
            self.turns.yield_turn()
        return o

    def run_threads(self, fns, head=0):
        if len(fns) == 1:
            fns[0]()
            return
        turns = Turns(len(fns))
        turns.head = head
        self.turns = turns
        errs = []

        def wrap(tid, fn):
            turns.start(tid)
            try:
                fn()
            except BaseException as ex:
                errs.append(ex)
            finally:
                turns.finish()
        ths = [threading.Thread(target=wrap, args=(i, f)) for i, f in enumerate(fns)]
        for t in ths:
            t.start()
        for t in ths:
            t.join()
        self.turns = None
        if errs:
            raise errs[0]

    def barrier(self):
        lasts = {}
        for e in ENGS:
            for o in reversed(self.ops[e]):
                if not o.is_dma and not o.is_nop:
                    lasts[e] = o
                    break
        dm = [(b, b.dcount) for b in self.slots if b.dcount > 0]
        for e in ENGS:
            o = Op(e, lambda eng: eng.nop())
            o.is_nop = True
            self.ops[e].append(o)
            o.eidx = len(self.ops[e])
            for e2, l in lasts.items():
                if e2 == e:
                    continue
                key = ("e", e2)
                if self.clock[e].get(key, 0) >= l.eidx:
                    continue
                self.clock[e][key] = l.eidx
                l.needed = True
                o.waits.append(("e", e2, l))
            for b, v in dm:
                key = ("d", id(b))
                if self.clock[e].get(key, 0) >= v:
                    continue
                self.clock[e][key] = v
                o.waits.append(("d", b, v))
        for b in self.bufs:
            b.writers = []
            b.readers = []
            if b.dslot is not None:
                self.free_slots.append(b.dslot)
                b.dslot = None

    def emit(self):
        nc, es = self.nc, self.es
        for e in ENGS:
            eps_ = sorted(set(o.epoch for o in self.ops[e] if o.needed))
            for ep in eps_:
                self.esem[(e, ep)] = es.enter_context(nc.semaphore("es_%s_%d" % (e, ep)))
        n = 0
        for e in ENGS:
            for o in self.ops[e]:
                if o.is_dma and o.dbuf.dsem is None:
                    o.dbuf.dsem = es.enter_context(nc.semaphore("ds%d" % n))
                    n += 1
        self.ndsem = n
        for e in ENGS:
            c = {}
            for o in self.ops[e]:
                if o.needed:
                    c[o.epoch] = c.get(o.epoch, 0) + 1
                    o.semval = c[o.epoch]
        block = es.enter_context(nc.Block())

        def run(ename):
            def body(eng):
                for o in self.ops[ename]:
                    for (k, a, b) in o.waits:
                        if k == "d":
                            eng.wait_ge(a.dsem, b)
                        else:
                            eng.wait_ge(self.esem[(a, b.epoch)], b.semval)
                    ins = o.fn(eng)
                    if o.is_dma:
                        ins.then_inc(o.dbuf.dsem, 16)
                    elif o.needed:
                        ins.then_inc(self.esem[(ename, o.epoch)], 1)
            return body

        block.tensor(run("pe"))
        block.scalar(run("act"))
        block.vector(run("dve"))
        block.gpsimd(run("pool"))
        block.sync(run("sp"))


class TB:
    def __init__(self, p, t, name):
        self.t = t
        self.b = p.buf(name)

    def __getitem__(self, k):
        return self.t[k]


class Ring:
    def __init__(self, items):
        self.items = items
        self.i = 0

    def next(self):
        r = self.items[self.i % len(self.items)]
        self.i += 1
        return r


def build(stages=4, debug=False, only=None):
    nc = bass.Bass("TRN2", target_bir_lowering=False)

    def din(name, shape):
        return nc.dram_tensor(name, list(shape), F32, kind="ExternalInput").ap()

    xT = din("xT", [8, 128, TOK])
    cT = din("cT", [128, 8, NSEQ])
    ada_w = din("ada_w", [2, D, 6 * D])
    ada_bT = din("ada_bT", [128, 2, 48])
    lnp = din("lnp", [128, 2, 4, 8])
    ev_w_in = din("ev_w_in", [D, 2048])
    ev_w_out = din("ev_w_out", [D, D])
    sgp_d = din("sgp", [128, 2, 4])
    wsT_d = din("wsT", [128, 4, 128])
    bs_d = din("bs", [1, 512])
    wdw_d = din("wdw", [128, 4, 31])
    cvp_d = din("cvp", [128, 3, 4])
    od_w_qkv = din("od_w_qkv", [D, 3 * D])
    od_w_out = din("od_w_out", [D, D])
    wr_d = din("wr", [2, 128, 8, 20])
    br_d = din("br", [2, 1, 20])
    w_gate = din("moe_w_gate", [2, 16, 128, 8, 512])
    w_up = din("moe_w_up", [2, 16, 128, 8, 512])
    w_down = din("moe_w_down", [2, 16, 128, 4, D])
    consts_d = din("consts", [128, 2473])

    yT = nc.dram_tensor("yT", [8, 128, TOK], F32, kind="ExternalOutput").ap()
    XA = nc.dram_tensor("XA", [8, 128, TOK], F32, kind="Internal").ap()
    XB = nc.dram_tensor("XB", [8, 128, TOK], F32, kind="Internal").ap()
    dbg = {}

    es = ExitStack()
    p = Prog(nc, es)

    uniq = [0]

    def sb(st, name, shape, dt):
        uniq[0] += 1
        name = "%s_u%d" % (name, uniq[0])
        return TB(p, st.enter_context(nc.sbuf_tensor(name, list(shape), dt)), name)

    def MM(out, lhsT, rhs, first, last, reads, wb):
        p.op("pe", lambda e: e.matmul(out, lhsT, rhs, start=first, stop=last), reads=reads,
             writes=[wb] if first else [], joins=[] if first else [wb])

    def MMg(out, lhsT, rhs, reads, wb, newgroup):
        p.op("pe", lambda e: e.matmul(out, lhsT, rhs, start=True, stop=True), reads=reads,
             writes=[wb] if newgroup else [], joins=[] if newgroup else [wb])

    def ACT(out, in_, func, reads, writes, bias=None, scale=None):
        kw = {}
        if bias is not None:
            kw["bias"] = bias
        if scale is not None:
            kw["scale"] = scale
        p.op("act", lambda e: e.activation(out=out, in_=in_, func=func, **kw), reads=reads, writes=writes)

    def TTo(eng, out, in0, in1, op, reads, writes):
        p.op(eng, lambda e: e.tensor_tensor(out=out, in0=in0, in1=in1, op=op), reads=reads, writes=writes)

    def TS(eng, out, in0, s1, s2, op0, op1, reads, writes):
        if op1 is None:
            p.op(eng, lambda e: e.tensor_scalar(out=out, in0=in0, scalar1=s1, scalar2=None, op0=op0),
                 reads=reads, writes=writes)
        else:
            p.op(eng, lambda e: e.tensor_scalar(out=out, in0=in0, scalar1=s1, scalar2=s2, op0=op0, op1=op1),
                 reads=reads, writes=writes)

    def STT(out, in0, scalar, in1, op0, op1, reads, writes):
        p.op("dve", lambda e: e.scalar_tensor_tensor(out=out, in0=in0, scalar=scalar, in1=in1, op0=op0, op1=op1),
             reads=reads, writes=writes)

    def CP(eng, out, in_, reads, writes):
        p.op(eng, lambda e: e.tensor_copy(out=out, in_=in_), reads=reads, writes=writes)

    def RED(out, in_, op, reads, writes):
        p.op("dve", lambda e: e.tensor_reduce(out=out, in_=in_, axis=AX.X, op=op), reads=reads, writes=writes)

    def RECIP(out, in_, reads, writes):
        p.op("dve", lambda e: e.reciprocal(out=out, in_=in_), reads=reads, writes=writes)

    def MSET(eng, ap, val, writes):
        p.op(eng, lambda e: e.memset(ap, val), writes=writes)

    def DMA(q, out, in_, reads, writes, dbuf):
        p.op(q, lambda e: e.dma_start(out=out, in_=in_), reads=reads, writes=writes, dma=dbuf)

    G = ExitStack()
    es.enter_context(G)
    psb = [TB(p, G.enter_context(nc.psum_tensor("ps%d" % i, [128, 512], F32)), "ps%d" % i) for i in range(8)]
    for t_ in psb:
        t_.b.excl = True
    class TRing:
        def __init__(self, items):
            self.full = Ring(items)
            h = len(items) // 2
            self.sub = [Ring(items[:h]), Ring(items[h:])]

        def next(self):
            if p.turns is not None:
                return self.sub[p.turns.local.tid % 2].next()
            return self.full.next()
    PA = TRing(psb[0:4])
    PB = TRing(psb[4:6])
    PC = TRing(psb[6:8])

    cst = sb(G, "cst", [128, 2473], F32)
    DMA("sp", cst[:], consts_d[:, :], [], [cst.b], cst.b)
    ident = cst[:, 0:128]
    tri = cst[:, 128:256]
    ones = cst[:, 256:384]
    sel = cst[0:16, 384:2432]
    thr8 = cst[:, 2432:2440]
    jt32 = cst[:, 2440:2472]
    pcol = cst[:, 2472:2473]

    identb = sb(G, "identb", [128, 128], BF16)
    trib = sb(G, "trib", [128, 128], BF16)
    onesb = sb(G, "onesb", [128, 128], BF16)
    on1024 = sb(G, "on1024", [128, 128], BF16)
    on512 = sb(G, "on512", [128, 128], BF16)
    CP("dve", identb[:], ident, [cst.b], [identb.b])
    CP("dve", trib[:], tri, [cst.b], [trib.b])
    CP("dve", onesb[:], ones, [cst.b], [onesb.b])
    TS("dve", on1024[:], ones, 1.0 / 1024, None, ALU.mult, None, [cst.b], [on1024.b])
    TS("dve", on512[:], ones, 1.0 / 512, None, ALU.mult, None, [cst.b], [on512.b])
    ustrb = sb(G, "ustrb", [128, 128], BF16)
    TTo("dve", ustrb[:], tri, ident, ALU.subtract, [cst.b], [ustrb.b])
    epst = sb(G, "epst", [128, 1], F32)
    MSET("dve", epst[:], EPS, [epst.b])

    lnp_sb = sb(G, "lnp_sb", [128, 2, 4, 8], F32)
    DMA("sp", lnp_sb[:], lnp[:, :, :, :], [], [lnp_sb.b], lnp_sb.b)
    cond = sb(G, "cond", [128, 2, 48, NSEQ], F32)

    with ExitStack() as st:
        cT_sb = sb(st, "cT_sb", [128, 8, NSEQ], F32)
        csil = sb(st, "csil", [128, 8, NSEQ], BF16)
        adab = sb(st, "adab", [128, 2, 48], F32)
        DMA("sp", cT_sb[:], cT[:, :, :], [], [cT_sb.b], cT_sb.b)
        DMA("sp", adab[:], ada_bT[:, :, :], [], [adab.b], adab.b)
        ACT(csil[:], cT_sb[:], AF.Silu, [cT_sb.b], [csil.b])
        wb2 = [sb(st, "adaw%d" % i, [128, 8, 1024], BF16) for i in range(2)]
        k = 0
        for l in range(2):
            for s6 in range(6):
                w = wb2[k % 2]
                k += 1
                DMA("pool", w[:], ada_w[l, :, s6 * 1024:(s6 + 1) * 1024].rearrange("(kc p) n -> p kc n", p=128),
                    [], [w.b], w.b)
                ps = PC.next()
                for dc in range(8):
                    for kc in range(8):
                        first = (dc == 0 and kc == 0)
                        p.op("pe", (lambda e, o=ps[:, dc * 2:dc * 2 + 2], a=w[:, kc, dc * 128:(dc + 1) * 128],
                                    b=csil[:, kc, :], f=(kc == 0), la=(kc == 7): e.matmul(o, a, b, start=f, stop=la)),
                             reads=[w.b, csil.b], writes=[ps.b] if first else [], joins=[] if first else [ps.b])
                add1 = 1.0 if s6 in (1, 2, 4, 5) else 0.0
                for s in range(NSEQ):
                    STT(cond[:, l, s6 * 8:(s6 + 1) * 8, s], ps[:, s:16:2], add1, adab[:, l, s6 * 8:(s6 + 1) * 8],
                        ALU.add, ALU.add, [ps.b, adab.b], [cond.b])
        p.barrier()

    def cv(l, split, dc, s):
        return cond[:, l, split * 8 + dc, s:s + 1]

    def load_x(src, tok0, xt, W=TT):
        DMA("sp", xt[:], src[:, :, tok0:tok0 + W].rearrange("kc p t -> p kc t"), [], [xt.b], xt.b)

    def modulate(xt, hT, l, split_sh, split_sc, s):
        for dc in range(8):
            ACT(hT[:, dc, :], xt[:, dc, :], AF.Identity, [xt.b, cond.b], [hT.b] if dc == 0 else [],
                bias=cv(l, split_sh, dc, s), scale=cv(l, split_sc, dc, s)) if dc == 0 else \
                p.op("act", (lambda e, o=hT[:, dc, :], i=xt[:, dc, :], b=cv(l, split_sh, dc, s), sc=cv(l, split_sc, dc, s):
                             e.activation(out=o, in_=i, func=AF.Identity, bias=b, scale=sc)),
                     reads=[xt.b, cond.b], joins=[hT.b])

    def ln_fm(r, nch, onb, lnT, W=TT):
        rb, sd, mu = lnT["rb"], lnT["sd"], lnT["mu"]
        if "sqf" in lnT:
            sqf, sqb = lnT["sqf"], lnT["sqb"]
        else:
            sqf, sqb = (lambda c, t_=lnT["sq"]: t_[:, c, :]), [lnT["sq"].b]
        for c in range(nch):
            p.op("act", (lambda e, o=sqf(c), i=r[:, c, :]: e.activation(out=o, in_=i, func=AF.Square)),
                 reads=[r.b], writes=sqb if c == 0 else [], joins=[] if c == 0 else sqb)
            p.op("dve", (lambda e, o=rb[:, c, :], i=r[:, c, :]: e.tensor_copy(out=o, in_=i)),
                 reads=[r.b], writes=[rb.b] if c == 0 else [], joins=[] if c == 0 else [rb.b])
        mps = PB.next()
        for c in range(nch):
            MM(mps[:, 0:W], onb[:], rb[:, c, :], c == 0, c == nch - 1, [onb.b, rb.b], mps.b)
        vps = PC.next()
        for c in range(nch):
            MM(vps[:, 0:W], onb[:], sqf(c), c == 0, c == nch - 1, [onb.b] + sqb, vps.b)
        ACT(mu[:], mps[:, 0:W], AF.Identity, [mps.b], [mu.b])
        STT(sd[:], mu[:], -1.0, mu[:], ALU.mult, ALU.mult, [mu.b], [sd.b])
        STT(sd[:], vps[:, 0:W], EPS, sd[:], ALU.add, ALU.add, [vps.b, sd.b], [sd.b])
        ACT(sd[:], sd[:], AF.Sqrt, [sd.b], [sd.b])
        RECIP(sd[:], sd[:], [sd.b], [sd.b])
        for c in range(nch):
            p.op("dve", (lambda e, o=r[:, c, :], a=r[:, c, :], b=mps[:, 0:W]: e.tensor_tensor(out=o, in0=a, in1=b, op=ALU.subtract)),
                 reads=[mps.b, r.b] if c == 0 else [mps.b], writes=[r.b] if c == 0 else [], joins=[] if c == 0 else [r.b])
        for c in range(nch):
            p.op("dve", (lambda e, o=r[:, c, :], a=r[:, c, :], b=sd[:]: e.tensor_tensor(out=o, in0=a, in1=b, op=ALU.mult)),
                 reads=[sd.b, r.b] if c == 0 else [sd.b], writes=[r.b] if c == 0 else [], joins=[] if c == 0 else [r.b])

    def ln_out_store(r, gi, bi, l, dst, tok0, W=TT):
        xo = r
        for dc in range(8):
            p.op("act", (lambda e, o=xo[:, dc, :], i=r[:, dc, :], sc=lnp_sb[:, l, gi, dc:dc + 1], b=lnp_sb[:, l, bi, dc:dc + 1]:
                         e.activation(out=o, in_=i, func=AF.Identity, bias=b, scale=sc)),
                 reads=[r.b, lnp_sb.b] if dc == 0 else [lnp_sb.b], writes=[xo.b] if dc == 0 else [], joins=[] if dc == 0 else [xo.b])
        DMA("act", dst[:, :, tok0:tok0 + W].rearrange("kc p t -> p kc t"), xo[:], [xo.b], [], xo.b)

    def mixer0(src, dst, l):
        with ExitStack() as st:
            win = sb(st, "win", [128, 8, 2048], BF16)
            wout = sb(st, "wout", [128, 8, 1024], BF16)
            for j in range(4):
                p.op("pool", (lambda e, j=j: e.dma_start(out=win[:, :, j * 512:(j + 1) * 512],
                                                          in_=ev_w_in[:, j * 512:(j + 1) * 512].rearrange("(kc p) n -> p kc n", p=128))),
                     writes=[win.b] if j == 0 else [], joins=[] if j == 0 else [win.b], dma=win.b)
            for j in range(2):
                p.op("pool", (lambda e, j=j: e.dma_start(out=wout[:, :, j * 512:(j + 1) * 512],
                                                          in_=ev_w_out[:, j * 512:(j + 1) * 512].rearrange("(kc p) n -> p kc n", p=128))),
                     writes=[wout.b] if j == 0 else [], joins=[] if j == 0 else [wout.b], dma=wout.b)
            sgp = sb(st, "sgp_sb", [128, 2, 4], F32)
            DMA("sp", sgp[:], sgp_d[:, :, :], [], [sgp.b], sgp.b)
            cvp = sb(st, "cvp_sb", [128, 3, 4], F32)
            DMA("sp", cvp[:], cvp_d[:, :, :], [], [cvp.b], cvp.b)
            wsTm = sb(st, "wsTm", [128, 4, 128], BF16)
            C4 = sb(st, "C4", [128, 4, 128], F32)
            diag = sb(st, "diag", [128, 4, 31, 128], BF16)
            stmp = ExitStack()
            wsT = sb(stmp, "wsT_sb", [128, 4, 128], F32)
            DMA("sp", wsT[:], wsT_d[:, :, :], [], [wsT.b], wsT.b)
            bsB = sb(stmp, "bsB", [128, 4, 128], F32)
            DMA("sp", bsB[:].rearrange("p g q -> p (g q)"), bs_d[0:1, :].to_broadcast([128, 512]), [], [bsB.b], bsB.b)
            wdw = sb(stmp, "wdw_sb", [128, 4, 31], F32)
            DMA("sp", wdw[:], wdw_d[:, :, :], [], [wdw.b], wdw.b)
            for g in range(4):
                p.op("dve", (lambda e, g=g: e.tensor_tensor(out=wsTm[:, g, :], in0=wsT[:, g, :], in1=tri, op=ALU.mult)),
                     reads=[wsT.b, cst.b], writes=[wsTm.b] if g == 0 else [], joins=[] if g == 0 else [wsTm.b])
            rps = PC.next()
            for g in range(4):
                p.op("pe", (lambda e, g=g: e.matmul(rps[:, g * 128:(g + 1) * 128], onesb[:], wsTm[:, g, :], start=True, stop=True)),
                     reads=[onesb.b, wsTm.b], writes=[rps.b] if g == 0 else [], joins=[] if g == 0 else [rps.b])
            for g in range(4):
                p.op("dve", (lambda e, g=g: e.scalar_tensor_tensor(out=C4[:, g, :], in0=rps[:, g * 128:(g + 1) * 128],
                                                                    scalar=sgp[:, 1, g:g + 1], in1=bsB[:, g, :],
                                                                    op0=ALU.mult, op1=ALU.add)),
                     reads=[rps.b, sgp.b, bsB.b], writes=[C4.b] if g == 0 else [], joins=[] if g == 0 else [C4.b])
            for c in range(4):
                for k in range(31):
                    p.op("dve", (lambda e, c=c, k=k: e.tensor_scalar(out=diag[:, c, k, :], in0=ident, scalar1=wdw[:, c, k:k + 1],
                                                                      scalar2=None, op0=ALU.mult)),
                         reads=[cst.b, wdw.b], writes=[diag.b] if (c == 0 and k == 0) else [],
                         joins=[] if (c == 0 and k == 0) else [diag.b])

            p.barrier()
            stmp.close()
            WM = 256
            NSB = WM // 128

            def mkM(s):
                GL = sb(st, "GL", [128, 4, 30 + SEQ], BF16).t
                GLh = p.buf("GLh")
                GLb = [p.buf("GLb%d" % i) for i in range(SEQ // WM)]
                MSET("pool", GL[:, :, 0:30], 0.0, [GLh])
                xt = sb(st, "xt", [128, 8, WM], F32)
                hT = sb(st, "hT", [128, 8, WM], BF16)
                uT = sb(st, "uT", [128, 4, WM], F32)
                vg = Ring([sb(st, "vg%d" % i, [128, TT], F32) for i in range(1)])
                st6 = sb(st, "st6", [128, 6], F32)
                mv = sb(st, "mv", [128, 2], F32)
                rs = sb(st, "rs", [128, 1], F32)
                vn = sb(st, "vn", [128, NSB, TT], BF16)
                t1 = sb(st, "t1", [128, WM], F32)
                yab = sb(st, "yab", [128, 8, WM], BF16)

                class _V:
                    def __init__(self, lo, name):
                        self.lo = lo
                        self.b = p.buf(name)

                    def __getitem__(self, k):
                        a, c, d_ = k
                        return yab[a, self.lo + c, d_]
                ya = _V(0, "ya")
                yb = _V(4, "yb")
                sg = Ring([sb(st, "sg%d" % i, [128, WM], F32) for i in range(2)])
                yc = sb(st, "yc", [128, 4, WM], F32)
                tmp = Ring([sb(st, "tmp%d" % i, [128, WM], F32) for i in range(2)])
                _rb = sb(st, "lnr_rb", [128, 8, WM], BF16)
                _sd = sb(st, "lnr_sd", [128, WM], F32)
                lnr = {"rb": _rb, "sd": _sd, "mu": t1, "sqf": (lambda c: yab[:, c, :]), "sqb": [ya.b, yb.b]}
                lnc = {"rb": _rb, "sd": _sd, "mu": t1, "sqf": (lambda c: yab[:, 4 + c, :]), "sqb": [yb.b]}

                def run():
                    for t in range(SEQ // WM):
                        tok0 = s * SEQ + t * WM
                        load_x(src, tok0, xt, WM)
                        modulate(xt, hT, l, 0, 1, s)
                        for fo in range(4):
                            ps = PA.next()
                            for kc in range(8):
                                MM(ps[:, 0:WM], win[:, kc, fo * 128:(fo + 1) * 128], hT[:, kc, :], kc == 0, kc == 7, [win.b, hT.b], ps.b)
                            p.op("act", (lambda e, o=uT[:, fo, :], i=ps[:, 0:WM]: e.activation(out=o, in_=i, func=AF.Gelu)),
                                 reads=[ps.b], writes=[uT.b] if fo == 0 else [], joins=[] if fo == 0 else [uT.b])
                        for sub in range(NSB):
                            ps = PA.next()
                            for kc in range(8):
                                MM(ps[:], hT[:, kc, sub * 128:(sub + 1) * 128], win[:, kc, 512:1024], kc == 0, kc == 7, [win.b, hT.b], ps.b)
                            v = vg.next()
                            ACT(v[:], ps[:], AF.Gelu, [ps.b], [v.b])
                            p.op("dve", (lambda e, v=v: e.bn_stats(out=st6[:], in_=v[:])), reads=[v.b], writes=[st6.b])
                            p.op("dve", lambda e: e.bn_aggr(out=mv[:], in_=st6[:]), reads=[st6.b], writes=[mv.b])
                            ACT(rs[:], mv[:, 1:2], AF.Sqrt, [mv.b, epst.b], [rs.b], bias=epst[:, 0:1])
                            RECIP(rs[:], rs[:], [rs.b], [rs.b])
                            p.op("dve", (lambda e, v=v, sub=sub: e.tensor_scalar(out=vn[:, sub, :], in0=v[:], scalar1=mv[:, 0:1], scalar2=rs[:, 0:1],
                                                                                  op0=ALU.subtract, op1=ALU.mult)),
                                 reads=[v.b, mv.b, rs.b], writes=[vn.b] if sub == 0 else [], joins=[] if sub == 0 else [vn.b])
                        for g in range(4):
                            ps = PA.next()
                            for sub in range(NSB):
                                p.op("pe", (lambda e, ps=ps, g=g, sub=sub: e.matmul(ps[:, sub * 128:(sub + 1) * 128], vn[:, sub, g * 128:(g + 1) * 128],
                                                                                     wsTm[:, g, :], start=True, stop=True)),
                                     reads=[vn.b, wsTm.b], writes=[ps.b] if sub == 0 else [], joins=[] if sub == 0 else [ps.b])
                            STT(t1[:].rearrange("p (c q) -> p c q", c=NSB), ps[:, 0:WM].rearrange("p (c q) -> p c q", c=NSB), sgp[:, 0, g:g + 1],
                                C4[:, g:g + 1, :].to_broadcast([128, NSB, 128]), ALU.mult, ALU.add, [ps.b, sgp.b, C4.b], [t1.b])
                            p.op("dve", (lambda e, g=g: e.tensor_tensor(out=ya[:, g, :], in0=t1[:], in1=uT[:, g, :], op=ALU.mult)),
                                 reads=[t1.b, uT.b], writes=[ya.b] if g == 0 else [], joins=[] if g == 0 else [ya.b])
                        for fo in range(4):
                            pa = PA.next()
                            for kc in range(8):
                                MM(pa[:, 0:WM], win[:, kc, 1024 + fo * 128:1024 + (fo + 1) * 128], hT[:, kc, :], kc == 0, kc == 7, [win.b, hT.b], pa.b)
                            pg = PA.next()
                            for kc in range(8):
                                MM(pg[:, 0:WM], win[:, kc, 1536 + fo * 128:1536 + (fo + 1) * 128], hT[:, kc, :], kc == 0, kc == 7, [win.b, hT.b], pg.b)
                            sgt = sg.next()
                            ACT(sgt[:], pg[:, 0:WM], AF.Sigmoid, [pg.b], [sgt.b])
                            p.op("dve", (lambda e, fo=fo, pa=pa, sgt=sgt, t=t: e.tensor_tensor(out=GL[:, fo, 30 + t * WM:30 + (t + 1) * WM], in0=pa[:, 0:WM], in1=sgt[:], op=ALU.mult)),
                                 reads=[pa.b, sgt.b], writes=[GLb[t]] if fo == 0 else [], joins=[] if fo == 0 else [GLb[t]])
                        glreads = [diag.b, GLb[t], GLh] + ([GLb[t - 1]] if t > 0 else [])
                        for c in range(4):
                            ps = PA.next()
                            for k in range(31):
                                MM(ps[:, 0:WM], diag[:, c, k, :], GL[:, c, t * WM + k:t * WM + k + WM], k == 0, k == 30, glreads, ps.b)
                            p.op("act", (lambda e, c=c, ps=ps: e.activation(out=yc[:, c, :], in_=ps[:, 0:WM], func=AF.Identity, bias=cvp[:, 0, c:c + 1])),
                                 reads=[ps.b, cvp.b], writes=[yc.b] if c == 0 else [], joins=[] if c == 0 else [yc.b])
                        ln_fm(yc, 4, on512, lnc, WM)
                        for c in range(4):
                            p.op("act", (lambda e, c=c: e.activation(out=yb[:, c, :], in_=yc[:, c, :], func=AF.Silu,
                                                                      bias=cvp[:, 2, c:c + 1], scale=cvp[:, 1, c:c + 1])),
                                 reads=[yc.b, cvp.b], writes=[yb.b] if c == 0 else [], joins=[] if c == 0 else [yb.b])
                        for dc in range(8):
                            ps = PA.next()
                            for kc in range(8):
                                src_y = ya if kc < 4 else yb
                                MM(ps[:, 0:WM], wout[:, kc, dc * 128:(dc + 1) * 128], src_y[:, kc % 4, :], kc == 0, kc == 7, [wout.b, ya.b, yb.b], ps.b)
                            tm = tmp.next()
                            ACT(tm[:], ps[:, 0:WM], AF.Identity, [ps.b, cond.b], [tm.b], scale=cv(l, 2, dc, s))
                            p.op("dve", (lambda e, dc=dc, tm=tm: e.scalar_tensor_tensor(out=xt[:, dc, :], in0=xt[:, dc, :], scalar=ALPHA, in1=tm[:],
                                                                                  op0=ALU.mult, op1=ALU.add)),
                                 reads=[xt.b, tm.b] if dc == 0 else [tm.b], writes=[xt.b] if dc == 0 else [], joins=[] if dc == 0 else [xt.b])
                        ln_fm(xt, 8, on1024, lnr, WM)
                        ln_out_store(xt, 0, 1, l, dst, tok0, WM)
                return run
            p.run_threads([mkM(0), mkM(1)], head=HEAD_M)
            p.barrier()

    ST = 1024
    NTI = ST // 128

    def moe(src, dst, l):
        with ExitStack() as st:
            wr_sb = sb(st, "wr_sb", [128, 8, 20], F32)
            DMA("sp", wr_sb[:], wr_d[l, :, :, :], [], [wr_sb.b], wr_sb.b)
            brB = sb(st, "brB", [128, 20], F32)
            DMA("sp", brB[:], br_d[l, 0:1, :].to_broadcast([128, 20]), [], [brB.b], brB.b)
            hT = sb(st, "hTm", [128, 8, ST], BF16)
            yacc_t = sb(st, "yacc", [128, 2, 8, TT], F32).t
            yb_ = [[p.buf("yacc%d_%d" % (a, b)) for b in range(8)] for a in range(2)]
            wgs = Ring([sb(st, "wg%d" % i, [128, 8, 512], BF16) for i in range(2)])
            wus = Ring([sb(st, "wu%d" % i, [128, 8, 512], BF16) for i in range(2)])
            wds = Ring([sb(st, "wd%d" % i, [128, 4, 1024], BF16) for i in range(2)])
            xt = sb(st, "xtm", [128, 8, TT], F32)
            hf = sb(st, "hf", [128, 8, 128], F32)
            acts = Ring([sb(st, "act%d" % i, [128, 4, TT], BF16) for i in range(2)])
            sgs = Ring([sb(st, "sgm%d" % i, [128, TT], F32) for i in range(2)])
            t1s = Ring([sb(st, "t1m%d" % i, [128, TT], F32) for i in range(2)])
            lnr = {"rb": sb(st, "lnm_rb", [128, 8, TT], BF16), "sq": sb(st, "lnm_sq", [128, 8, TT], BF16), "sd": sb(st, "lnm_sd", [128, TT], F32), "mu": sb(st, "lnm_mu", [128, TT], F32)}
            L = sb(st, "Lrt", [128, NTI, 20], F32)
            cwT = sb(st, "cwT", [16, ST], F32)

            def rt(name, shape):
                return sb(st, "rt_" + name, shape, F32)
            gmax = rt("gmax", [128, NTI]); gsel = rt("gsel", [128, NTI, 4]); gd = rt("gd", [128, NTI, 4])
            gsum = rt("gsum", [128, NTI]); gw = rt("gw", [128, NTI]); tmp4 = rt("tmp4", [128, NTI, 4, 4])
            ig = rt("ig", [128, NTI, 4]); m1 = rt("m1", [128, NTI]); oh1 = rt("oh1", [128, NTI, 4])
            ig2 = rt("ig2", [128, NTI, 4]); m2 = rt("m2", [128, NTI]); oh2 = rt("oh2", [128, NTI, 4])
            dd = rt("dd", [128, NTI]); w1 = rt("w1", [128, NTI]); w2 = rt("w2", [128, NTI])
            a1 = rt("a1", [128, NTI, 4]); a2 = rt("a2", [128, NTI, 4]); cw = rt("cw", [128, NTI, 4, 4])

            def bc3(t):
                return t[:].unsqueeze(2).to_broadcast([128, NTI, 4])

            for sti in range(TOK // ST):
                s = (sti * ST) // SEQ
                base = sti * ST
                for tt in range(ST // TT):
                    load_x(src, base + tt * TT, xt)
                    for dc in range(8):
                        p.op("act", (lambda e, o=hT[:, dc, tt * TT:(tt + 1) * TT], i=xt[:, dc, :], b=cv(l, 3, dc, s), sc=cv(l, 4, dc, s):
                                     e.activation(out=o, in_=i, func=AF.Identity, bias=b, scale=sc)),
                             reads=[xt.b, cond.b], writes=[hT.b] if (dc == 0 and tt == 0) else [],
                             joins=[] if (dc == 0 and tt == 0) else [hT.b])
                    for sub in range(4):
                        for dc in range(8):
                            p.op("dve", (lambda e, o=hf[:, dc, :], i=xt[:, dc, sub * 128:(sub + 1) * 128], b=cv(l, 3, dc, s), sc=cv(l, 4, dc, s):
                                         e.tensor_scalar(out=o, in0=i, scalar1=sc, scalar2=b, op0=ALU.mult, op1=ALU.add)),
                                 reads=[xt.b, cond.b], writes=[hf.b] if dc == 0 else [], joins=[] if dc == 0 else [hf.b])
                        lps = PC.next()
                        for kc in range(8):
                            MM(lps[:, 0:20], hf[:, kc, :], wr_sb[:, kc, :], kc == 0, kc == 7, [hf.b, wr_sb.b], lps.b)
                        i16 = tt * 4 + sub
                        p.op("dve", (lambda e, o=L[:, i16, :], a=lps[:, 0:20]: e.tensor_tensor(out=o, in0=a, in1=brB[:], op=ALU.add)),
                             reads=[lps.b, brB.b], writes=[L.b] if i16 == 0 else [], joins=[] if i16 == 0 else [L.b])
                Lg = L[:, :, 0:4]
                Le = L[:, :, 4:20].rearrange("p t (g j) -> p t g j", g=4)
                RED(gmax[:], Lg, ALU.max, [L.b], [gmax.b])
                TTo("dve", gsel[:], Lg, bc3(gmax), ALU.is_equal, [L.b, gmax.b], [gsel.b])
                TTo("dve", gd[:], Lg, bc3(gmax), ALU.subtract, [L.b, gmax.b], [gd.b])
                ACT(gd[:], gd[:], AF.Exp, [gd.b], [gd.b])
                RED(gsum[:], gd[:], ALU.add, [gd.b], [gsum.b])
                RECIP(gw[:], gsum[:], [gsum.b], [gw.b])
                TTo("dve", tmp4[:], Le, gsel[:].unsqueeze(3).to_broadcast([128, NTI, 4, 4]), ALU.mult, [L.b, gsel.b], [tmp4.b])
                RED(ig[:], tmp4[:].rearrange("p t g j -> p t j g"), ALU.add, [tmp4.b], [ig.b])
                RED(m1[:], ig[:], ALU.max, [ig.b], [m1.b])
                TTo("dve", oh1[:], ig[:], bc3(m1), ALU.is_equal, [ig.b, m1.b], [oh1.b])
                STT(ig2[:], oh1[:], NEG, ig[:], ALU.mult, ALU.add, [oh1.b, ig.b], [ig2.b])
                RED(m2[:], ig2[:], ALU.max, [ig2.b], [m2.b])
                TTo("dve", oh2[:], ig2[:], bc3(m2), ALU.is_equal, [ig2.b, m2.b], [oh2.b])
                TTo("dve", dd[:], m2[:], m1[:], ALU.subtract, [m1.b, m2.b], [dd.b])
                ACT(dd[:], dd[:], AF.Exp, [dd.b], [dd.b])
                TS("dve", w1[:], dd[:], 1.0, None, ALU.add, None, [dd.b], [w1.b])
                RECIP(w1[:], w1[:], [w1.b], [w1.b])
                TTo("dve", w2[:], dd[:], w1[:], ALU.mult, [dd.b, w1.b], [w2.b])
                TTo("dve", w1[:], w1[:], gw[:], ALU.mult, [w1.b, gw.b], [w1.b])
                TTo("dve", w2[:], w2[:], gw[:], ALU.mult, [w2.b, gw.b], [w2.b])
                TTo("dve", a1[:], oh1[:], bc3(w1), ALU.mult, [oh1.b, w1.b], [a1.b])
                TTo("dve", a2[:], oh2[:], bc3(w2), ALU.mult, [oh2.b, w2.b], [a2.b])
                TTo("dve", a1[:], a1[:], a2[:], ALU.add, [a1.b, a2.b], [a1.b])
                TTo("dve", cw[:], gsel[:].unsqueeze(3).to_broadcast([128, NTI, 4, 4]),
                    a1[:].unsqueeze(2).to_broadcast([128, NTI, 4, 4]), ALU.mult, [gsel.b, a1.b], [cw.b])
                for i in range(NTI):
                    tp = PC.next()
                    p.op("pe", (lambda e, tp=tp, i=i: e.transpose(tp[0:16, 0:128], cw[:, i, :, :].rearrange("p g j -> p (g j)"), ident)),
                         reads=[cw.b, cst.b], writes=[tp.b])
                    p.op("act", (lambda e, tp=tp, i=i: e.activation(out=cwT[0:16, i * 128:(i + 1) * 128], in_=tp[0:16, 0:128], func=AF.Identity)),
                         reads=[tp.b], writes=[cwT.b] if i == 0 else [], joins=[] if i == 0 else [cwT.b])
                for ex in range(16):
                    wg, wu, wd = wgs.next(), wus.next(), wds.next()
                    DMA("pool", wg[:], w_gate[l, ex], [], [wg.b], wg.b)
                    DMA("pool", wu[:], w_up[l, ex], [], [wu.b], wu.b)
                    DMA("pool", wd[:], w_down[l, ex], [], [wd.b], wd.b)
                    for tt in range(ST // TT):
                        cps = PC.next()
                        MM(cps[:], sel[:, ex * 128:(ex + 1) * 128], cwT[0:16, tt * TT:(tt + 1) * TT], True, True, [cst.b, cwT.b], cps.b)
                        act = acts.next()
                        for fc in range(4):
                            gps = PA.next()
                            for kc in range(8):
                                MM(gps[:], wg[:, kc, fc * 128:(fc + 1) * 128], hT[:, kc, tt * TT:(tt + 1) * TT], kc == 0, kc == 7, [wg.b, hT.b], gps.b)
                            ups = PA.next()
                            for kc in range(8):
                                MM(ups[:], wu[:, kc, fc * 128:(fc + 1) * 128], hT[:, kc, tt * TT:(tt + 1) * TT], kc == 0, kc == 7, [wu.b, hT.b], ups.b)
                            sg = sgs.next()
                            t1 = t1s.next()
                            ACT(sg[:], gps[:], AF.Silu, [gps.b], [sg.b])
                            TTo("dve", t1[:], sg[:], ups[:], ALU.mult, [sg.b, ups.b], [t1.b])
                            p.op("dve", (lambda e, o=act[:, fc, :], a=t1[:], b=cps[:]: e.tensor_tensor(out=o, in0=a, in1=b, op=ALU.mult)),
                                 reads=[t1.b, cps.b], writes=[act.b] if fc == 0 else [], joins=[] if fc == 0 else [act.b])
                        for dc in range(8):
                            dps = PB.next()
                            for fc in range(4):
                                MM(dps[:], wd[:, fc, dc * 128:(dc + 1) * 128], act[:, fc, :], fc == 0, fc == 3, [wd.b, act.b], dps.b)
                            if ex == 0:
                                p.op("act", (lambda e, o=yacc_t[:, tt, dc, :], i=dps[:]: e.activation(out=o, in_=i, func=AF.Identity)),
                                     reads=[dps.b], writes=[yb_[tt][dc]])
                            else:
                                p.op("dve", (lambda e, o=yacc_t[:, tt, dc, :], i=dps[:]: e.tensor_tensor(out=o, in0=o, in1=i, op=ALU.add)),
                                     reads=[dps.b], writes=[yb_[tt][dc]])
                for tt in range(ST // TT):
                    tok0 = base + tt * TT
                    load_x(src, tok0, xt)
                    for dc in range(8):
                        tm = t1s.next()
                        ACT(tm[:], yacc_t[:, tt, dc, :], AF.Identity, [yb_[tt][dc], cond.b], [tm.b], scale=cv(l, 5, dc, s))
                        p.op("dve", (lambda e, dc=dc, tm=tm: e.scalar_tensor_tensor(out=xt[:, dc, :], in0=xt[:, dc, :], scalar=ALPHA, in1=tm[:],
                                                                              op0=ALU.mult, op1=ALU.add)),
                             reads=[xt.b, tm.b] if dc == 0 else [tm.b], writes=[xt.b] if dc == 0 else [], joins=[] if dc == 0 else [xt.b])
                    ln_fm(xt, 8, on1024, lnr)
                    ln_out_store(xt, 2, 3, l, dst, tok0)
            p.barrier()

    T_S = 512
    NTL = 31
    S_ROWS = NTL * T_S
    Hs = nc.dram_tensor("Hs", [S_ROWS, D], BF16, kind="Internal").ap()
    Ys = nc.dram_tensor("Ys", [S_ROWS, D], F32, kind="Internal").ap()

    dynreg = {}

    def moe_sparse(src, dst, l, zero_fill):
        NI = TOK // 128
        with ExitStack() as st:
            slots_i = sb(st, "slots_i", [128, NI, 2], I32)
            wgt = sb(st, "wgt", [128, NI, 2], F32)
            te_i = sb(st, "te_i", [128, 32], I32)
            widx = sb(st, "widx", [128, 32, 2], I32)
            HsB = p.buf("HsB")
            ZFB = p.buf("ZFB")
            with ExitStack() as sa:
                wr_sb = sb(sa, "wr_sb", [128, 8, 20], F32)
                DMA("sp", wr_sb[:], wr_d[l, :, :, :], [], [wr_sb.b], wr_sb.b)
                brB = sb(sa, "brB", [128, 20], F32)
                DMA("sp", brB[:], br_d[l, 0:1, :].to_broadcast([128, 20]), [], [brB.b], brB.b)
                htok = sb(sa, "htok", [128, NI, D], BF16)
                L = sb(sa, "LA", [128, NI, 20], F32)
                if zero_fill:
                    zt = sb(sa, "zt", [128, 4, D], BF16)
                    MSET("pool", zt[:].rearrange("p a n -> p (a n)"), 0.0, [zt.b])
                    for a in range(S_ROWS // 512):
                        p.op("sp", (lambda e, a=a: e.dma_start(out=Hs[a * 512:(a + 1) * 512, :].rearrange("(a p) n -> p a n", p=128), in_=zt[:])),
                             reads=[zt.b], joins=[ZFB], dma=ZFB)
                def mkA(tid, nth):
                    xt = sb(sa, "xtA", [128, 8, TT], F32)
                    hT = sb(sa, "hTA", [128, 8, TT], BF16)
                    hf = sb(sa, "hfA", [128, 8, 128], F32)

                    def run():
                        for t8 in range(tid, TOK // TT, nth):
                            s = (t8 * TT) // SEQ
                            load_x(src, t8 * TT, xt)
                            for dc in range(8):
                                p.op("act", (lambda e, o=hT[:, dc, :], i=xt[:, dc, :], b=cv(l, 3, dc, s), sc=cv(l, 4, dc, s):
                                             e.activation(out=o, in_=i, func=AF.Identity, bias=b, scale=sc)),
                                     reads=[xt.b, cond.b], writes=[hT.b] if dc == 0 else [], joins=[] if dc == 0 else [hT.b])
                            for sub in range(4):
                                i = t8 * 4 + sub
                                for dc in range(8):
                                    p.op("dve", (lambda e, o=hf[:, dc, :], i_=xt[:, dc, sub * 128:(sub + 1) * 128], b=cv(l, 3, dc, s), sc=cv(l, 4, dc, s):
                                                 e.tensor_scalar(out=o, in0=i_, scalar1=sc, scalar2=b, op0=ALU.mult, op1=ALU.add)),
                                         reads=[xt.b, cond.b], writes=[hf.b] if dc == 0 else [], joins=[] if dc == 0 else [hf.b])
                                lps = PC.next()
                                for kc in range(8):
                                    MM(lps[:, 0:20], hf[:, kc, :], wr_sb[:, kc, :], kc == 0, kc == 7, [hf.b, wr_sb.b], lps.b)
                                p.op("dve", (lambda e, o=L[:, i, :], a=lps[:, 0:20]: e.tensor_tensor(out=o, in0=a, in1=brB[:], op=ALU.add)),
                                     reads=[lps.b, brB.b], joins=[L.b])
                                tp = PA.next()
                                tpb = tp.t[:].bitcast(BF16)
                                for kc in range(8):
                                    p.op("pe", (lambda e, o=tpb[:, kc * 128:(kc + 1) * 128], a=hT[:, kc, sub * 128:(sub + 1) * 128]: e.transpose(o, a, identb[:])),
                                         reads=[hT.b, identb.b], writes=[tp.b] if kc == 0 else [], joins=[] if kc == 0 else [tp.b])
                                p.op("act", (lambda e, o=htok[:, i, :], a=tpb[:, 0:1024]: e.activation(out=o, in_=a, func=AF.Identity)),
                                     reads=[tp.b], joins=[htok.b])

                    return run
                p.run_threads([mkA(0, NTHR), mkA(1, NTHR)] if NTHR == 2 else [mkA(0, 1)], head=HEAD_A)

                def rt(name, shape, dt=F32):
                    return sb(sa, "rs_" + name, shape, dt)
                gmax = rt("gmax", [128, NI]); gsel = rt("gsel", [128, NI, 4]); gd = rt("gd", [128, NI, 4])
                gsum = rt("gsum", [128, NI]); gw = rt("gw", [128, NI]); tmp4 = rt("tmp4", [128, NI, 4, 4])
                ig = rt("ig", [128, NI, 4]); m1_ = rt("m1", [128, NI]); oh1 = rt("oh1", [128, NI, 4])
                ig2 = rt("ig2", [128, NI, 4]); m2_ = rt("m2", [128, NI]); oh2 = rt("oh2", [128, NI, 4])
                dd = rt("dd", [128, NI]); w1 = rt("w1", [128, NI]); w2 = rt("w2", [128, NI])
                M1 = rt("M1", [128, NI, 4, 4]); M2 = rt("M2", [128, NI, 4, 4]); Mm = rt("Mm", [128, NI, 16])
                mb = rt("mb", [128, NI, 16], BF16); Mp = rt("Mp", [128, NI + 1, 16], BF16)
                rank = rt("rank", [128, NI, 16]); cnt = rt("cnt", [128, 16]); cmp8 = rt("cmp8", [128, 16, 8])
                ncap = rt("ncap", [128, 16]); cap = rt("cap", [128, 16]); sa_ = rt("sa", [128, 16]); sb_ = rt("sb", [128, 16])
                start = rt("start", [128, 16]); pos = rt("pos", [128, NI, 16]); tmpp = rt("tmpp", [128, NI, 16])
                slots_f = rt("slots_f", [128, NI, 2]); cmpj = rt("cmpj", [128, 32, 16]); tef = rt("tef", [128, 32])

                def bc3(t):
                    return t[:].unsqueeze(2).to_broadcast([128, NI, 4])
                Lg = L[:, :, 0:4]
                Le = L[:, :, 4:20].rearrange("p t (g j) -> p t g j", g=4)
                RED(gmax[:], Lg, ALU.max, [L.b], [gmax.b])
                TTo("dve", gsel[:], Lg, bc3(gmax), ALU.is_equal, [L.b, gmax.b], [gsel.b])
                TTo("dve", gd[:], Lg, bc3(gmax), ALU.subtract, [L.b, gmax.b], [gd.b])
                ACT(gd[:], gd[:], AF.Exp, [gd.b], [gd.b])
                RED(gsum[:], gd[:], ALU.add, [gd.b], [gsum.b])
                RECIP(gw[:], gsum[:], [gsum.b], [gw.b])
                TTo("dve", tmp4[:], Le, gsel[:].unsqueeze(3).to_broadcast([128, NI, 4, 4]), ALU.mult, [L.b, gsel.b], [tmp4.b])
                RED(ig[:], tmp4[:].rearrange("p t g j -> p t j g"), ALU.add, [tmp4.b], [ig.b])
                RED(m1_[:], ig[:], ALU.max, [ig.b], [m1_.b])
                TTo("dve", oh1[:], ig[:], bc3(m1_), ALU.is_equal, [ig.b, m1_.b], [oh1.b])
                STT(ig2[:], oh1[:], NEG, ig[:], ALU.mult, ALU.add, [oh1.b, ig.b], [ig2.b])
                RED(m2_[:], ig2[:], ALU.max, [ig2.b], [m2_.b])
                TTo("dve", oh2[:], ig2[:], bc3(m2_), ALU.is_equal, [ig2.b, m2_.b], [oh2.b])
                TTo("dve", dd[:], m2_[:], m1_[:], ALU.subtract, [m1_.b, m2_.b], [dd.b])
                ACT(dd[:], dd[:], AF.Exp, [dd.b], [dd.b])
                TS("dve", w1[:], dd[:], 1.0, None, ALU.add, None, [dd.b], [w1.b])
                RECIP(w1[:], w1[:], [w1.b], [w1.b])
                TTo("dve", w2[:], dd[:], w1[:], ALU.mult, [dd.b, w1.b], [w2.b])
                TTo("dve", wgt[:, :, 0], w1[:], gw[:], ALU.mult, [w1.b, gw.b], [wgt.b])
                p.op("dve", lambda e: e.tensor_tensor(out=wgt[:, :, 1], in0=w2[:], in1=gw[:], op=ALU.mult), reads=[w2.b, gw.b], joins=[wgt.b])
                TTo("dve", M1[:], gsel[:].unsqueeze(3).to_broadcast([128, NI, 4, 4]), oh1[:].unsqueeze(2).to_broadcast([128, NI, 4, 4]), ALU.mult,
                    [gsel.b, oh1.b], [M1.b])
                TTo("dve", M2[:], gsel[:].unsqueeze(3).to_broadcast([128, NI, 4, 4]), oh2[:].unsqueeze(2).to_broadcast([128, NI, 4, 4]), ALU.mult,
                    [gsel.b, oh2.b], [M2.b])
                M1v = M1[:].rearrange("p t g j -> p t (g j)")
                M2v = M2[:].rearrange("p t g j -> p t (g j)")
                TTo("dve", Mm[:], M1v, M2v, ALU.add, [M1.b, M2.b], [Mm.b])
                CP("dve", mb[:], Mm[:], [Mm.b], [mb.b])
                MSET("dve", Mp[:, 0, :], 0.0, [Mp.b])
                for i in range(NI):
                    p.op("dve", (lambda e, i=i: e.tensor_tensor(out=Mp[:, i + 1, :], in0=Mp[:, i, :], in1=mb[:, i, :], op=ALU.add)),
                         reads=[mb.b], writes=[Mp.b])
                rps = PB.next()
                for i in range(NI):
                    p.op("pe", (lambda e, i=i: e.matmul(rps[:, i * 16:(i + 1) * 16], ustrb[:], mb[:, i, :], start=True, stop=False)),
                         reads=[ustrb.b, mb.b], writes=[rps.b] if i == 0 else [], joins=[] if i == 0 else [rps.b])
                    p.op("pe", (lambda e, i=i: e.matmul(rps[:, i * 16:(i + 1) * 16], onesb[:], Mp[:, i, :], start=False, stop=True)),
                         reads=[onesb.b, Mp.b], joins=[rps.b])
                ACT(rank[:].rearrange("p t e -> p (t e)"), rps[:], AF.Identity, [rps.b], [rank.b])
                cps_ = PC.next()
                MM(cps_[:, 0:16], onesb[:], Mp[:, NI, :], True, True, [onesb.b, Mp.b], cps_.b)
                ACT(cnt[:], cps_[:, 0:16], AF.Identity, [cps_.b], [cnt.b])
                TTo("dve", cmp8[:], cnt[:].unsqueeze(2).to_broadcast([128, 16, 8]), thr8.unsqueeze(1).to_broadcast([128, 16, 8]), ALU.is_gt,
                    [cnt.b, cst.b], [cmp8.b])
                RED(ncap[:], cmp8[:], ALU.add, [cmp8.b], [ncap.b])
                TS("dve", cap[:], ncap[:], float(T_S), None, ALU.mult, None, [ncap.b], [cap.b])
                srcb, dstb = cap, sa_
                for sh in (1, 2, 4, 8):
                    p.op("dve", (lambda e, a=srcb, d_=dstb, sh=sh: e.tensor_copy(out=d_[:, 0:sh], in_=a[:, 0:sh])), reads=[srcb.b], writes=[dstb.b])
                    p.op("dve", (lambda e, a=srcb, d_=dstb, sh=sh: e.tensor_tensor(out=d_[:, sh:16], in0=a[:, sh:16], in1=a[:, 0:16 - sh], op=ALU.add)),
                         reads=[srcb.b], joins=[dstb.b])
                    srcb, dstb = dstb, (sb_ if dstb is sa_ else sa_)
                incl = srcb
                TTo("dve", start[:], incl[:], cap[:], ALU.subtract, [incl.b, cap.b], [start.b])
                TTo("dve", pos[:], rank[:], start[:].unsqueeze(1).to_broadcast([128, NI, 16]), ALU.add, [rank.b, start.b], [pos.b])
                TTo("dve", tmpp[:], M1v, pos[:], ALU.mult, [M1.b, pos.b], [tmpp.b])
                RED(slots_f[:, :, 0], tmpp[:], ALU.add, [tmpp.b], [slots_f.b])
                TTo("dve", tmpp[:], M2v, pos[:], ALU.mult, [M2.b, pos.b], [tmpp.b])
                p.op("dve", lambda e: e.tensor_reduce(out=slots_f[:, :, 1], in_=tmpp[:], axis=AX.X, op=ALU.add), reads=[tmpp.b], joins=[slots_f.b])
                CP("dve", slots_i[:], slots_f[:], [slots_f.b], [slots_i.b])
                TTo("dve", cmpj[:], incl[:].unsqueeze(1).to_broadcast([128, 32, 16]), jt32.unsqueeze(2).to_broadcast([128, 32, 16]), ALU.is_le,
                    [incl.b, cst.b], [cmpj.b])
                RED(tef[:], cmpj[:], ALU.add, [cmpj.b], [tef.b])
                TS("dve", tef[:], tef[:], 15.0, None, ALU.min, None, [tef.b], [tef.b])
                CP("dve", te_i[:], tef[:], [tef.b], [te_i.b])
                TS("dve", tef[:], tef[:], float(l * 16), 128.0, ALU.add, ALU.mult, [tef.b], [tef.b])
                TS("dve", tef[:], tef[:], pcol, 2.0, ALU.add, ALU.mult, [tef.b, cst.b], [tef.b])
                CP("dve", widx[:, :, 0], tef[:], [tef.b], [widx.b])
                TS("dve", tef[:], tef[:], 1.0, None, ALU.add, None, [tef.b], [tef.b])
                p.op("dve", lambda e: e.tensor_copy(out=widx[:, :, 1], in_=tef[:]), reads=[tef.b], joins=[widx.b])
                for i in range(NI):
                    for k in range(2):
                        p.op("pool", (lambda e, i=i, k=k: e.indirect_dma_start(
                            out=Hs[:, :], out_offset=bass.IndirectOffsetOnAxis(ap=slots_i[:, i, k:k + 1].bitcast(U32), axis=0),
                            in_=htok[:, i, :], in_offset=None)),
                            reads=[htok.b, slots_i.b, ZFB], joins=[HsB], dma=HsB)
                if debug:
                    d1 = nc.dram_tensor("dbg_slots", [128, NI, 2], I32, kind="ExternalOutput").ap()
                    DMA("sp", d1[:, :, :], slots_i[:], [slots_i.b], [], slots_i.b)
                    d2 = nc.dram_tensor("dbg_wgt", [128, NI, 2], F32, kind="ExternalOutput").ap()
                    DMA("sp", d2[:, :, :], wgt[:], [wgt.b], [], wgt.b)
                    d3 = nc.dram_tensor("dbg_te", [128, 32], I32, kind="ExternalOutput").ap()
                    DMA("sp", d3[:, :], te_i[:], [te_i.b], [], te_i.b)
                    d4 = nc.dram_tensor("dbg_mm", [128, NI, 16], F32, kind="ExternalOutput").ap()
                    DMA("sp", d4[:, :, :], Mm[:], [Mm.b], [], Mm.b)
                    d5 = nc.dram_tensor("dbg_rank", [128, NI, 16], F32, kind="ExternalOutput").ap()
                    DMA("sp", d5[:, :, :], rank[:], [rank.b], [], rank.b)
                    d6 = nc.dram_tensor("dbg_start", [128, 16], F32, kind="ExternalOutput").ap()
                    DMA("sp", d6[:, :], start[:], [start.b], [], start.b)
                p.barrier()
            if "B" not in MOE_DEBUG:
                return
            with ExitStack() as sbk:
                def mkB(tid, nth):
                    nb = 2 if nth == 1 else 1
                    wgs = Ring([sb(sbk, "swg%d" % i, [128, 8, 512], BF16) for i in range(2)])
                    wus = Ring([sb(sbk, "swu%d" % i, [128, 8, 512], BF16) for i in range(2)])
                    wds = Ring([sb(sbk, "swd%d" % i, [128, 4, 1024], BF16) for i in range(2)])
                    hrows = Ring([sb(sbk, "hrow%d" % i, [128, D], BF16) for i in range(4)])
                    hsTs = Ring([sb(sbk, "hsT%d" % i, [128, 8, T_S], BF16) for i in range(nb)])
                    acts = Ring([sb(sbk, "sact%d" % i, [128, 4, T_S], BF16) for i in range(nb)])
                    sgs = Ring([sb(sbk, "ssg%d" % i, [128, T_S], F32) for i in range(2)])
                    yrows = Ring([sb(sbk, "yrow%d" % i, [128, 512], F32) for i in range(4)])

                    def run():
                        for j in range(tid, NTL, nth):
                            wg, wu, wd = wgs.next(), wus.next(), wds.next()

                            for (dst_t, src5) in ((wg, w_gate), (wu, w_up), (wd, w_down)):
                                for hh in range(2):
                                    p.op("pool", (lambda e, dst_t=dst_t, src5=src5, j=j, hh=hh: e.indirect_dma_start(
                                        out=dst_t[:].rearrange("p (h k) n -> p h (k n)", h=2)[:, hh, :], out_offset=None,
                                        in_=src5.rearrange("l e p (h k) n -> (l e p h) (k n)", h=2),
                                        in_offset=bass.IndirectOffsetOnAxis(ap=widx[:, j, hh:hh + 1].bitcast(U32), axis=0))),
                                        reads=[widx.b], writes=[dst_t.b] if hh == 0 else [], joins=[] if hh == 0 else [dst_t.b], dma=dst_t.b)
                            hsT = hsTs.next()
                            for sub in range(4):
                                hr = hrows.next()
                                r0 = j * T_S + sub * 128
                                DMA("sp", hr[:], Hs[r0:r0 + 128, :], [HsB], [hr.b], hr.b)
                                tp = PA.next()
                                tpb = tp.t[:].bitcast(BF16)
                                for kc in range(8):
                                    p.op("pe", (lambda e, o=tpb[:, kc * 128:(kc + 1) * 128], a=hr[:, kc * 128:(kc + 1) * 128]: e.transpose(o, a, identb[:])),
                                         reads=[hr.b, identb.b], writes=[tp.b] if kc == 0 else [], joins=[] if kc == 0 else [tp.b])
                                eng = "act" if sub % 2 == 0 else "dve"
                                o_ = hsT[:, :, sub * 128:(sub + 1) * 128]
                                i_ = tpb[:, 0:1024].rearrange("p (k t) -> p k t", k=8)
                                if eng == "act":
                                    p.op("act", (lambda e, o_=o_, i_=i_: e.activation(out=o_, in_=i_, func=AF.Identity)),
                                         reads=[tp.b], writes=[hsT.b] if sub == 0 else [], joins=[] if sub == 0 else [hsT.b])
                                else:
                                    p.op("dve", (lambda e, o_=o_, i_=i_: e.tensor_copy(out=o_, in_=i_)),
                                         reads=[tp.b], writes=[hsT.b] if sub == 0 else [], joins=[] if sub == 0 else [hsT.b])
                            act = acts.next()
                            for fc in range(4):
                                gps = PA.next()
                                for kc in range(8):
                                    MM(gps[:], wg[:, kc, fc * 128:(fc + 1) * 128], hsT[:, kc, :], kc == 0, kc == 7, [wg.b, hsT.b], gps.b)
                                ups = PA.next()
                                for kc in range(8):
                                    MM(ups[:], wu[:, kc, fc * 128:(fc + 1) * 128], hsT[:, kc, :], kc == 0, kc == 7, [wu.b, hsT.b], ups.b)
                                sg = sgs.next()
                                ACT(sg[:], gps[:], AF.Silu, [gps.b], [sg.b])
                                p.op("dve", (lambda e, o=act[:, fc, :], a=sg[:], b=ups[:]: e.tensor_tensor(out=o, in0=a, in1=b, op=ALU.mult)),
                                     reads=[sg.b, ups.b], writes=[act.b] if fc == 0 else [], joins=[] if fc == 0 else [act.b])
                            for sub in range(4):
                                for dh in range(2):
                                    dps = (PB if dh == 0 else PC).next()
                                    for fc in range(4):
                                        MM(dps[:], act[:, fc, sub * 128:(sub + 1) * 128], wd[:, fc, dh * 512:(dh + 1) * 512], fc == 0, fc == 3, [wd.b, act.b], dps.b)
                                    yr = yrows.next()
                                    if dh == 0:
                                        ACT(yr[:], dps[:], AF.Identity, [dps.b], [yr.b])
                                    else:
                                        CP("dve", yr[:], dps[:], [dps.b], [yr.b])
                                    r0 = j * T_S + sub * 128
                                    DMA("pool", Ys[r0:r0 + 128, dh * 512:(dh + 1) * 512], yr[:], [yr.b], [], yr.b)
                    return run
                p.run_threads([mkB(0, NTHR), mkB(1, NTHR)] if NTHR == 2 else [mkB(0, 1)], head=HEAD_B)
                p.barrier()
            if "C" not in MOE_DEBUG:
                return
            with ExitStack() as sc_:
                def mk_thread(tid, nth):
                    g1s = Ring([sb(sc_, "g1_%d" % i, [128, D], F32) for i in range(2)])
                    g2s = Ring([sb(sc_, "g2_%d" % i, [128, D], F32) for i in range(2)])
                    yT = sb(sc_, "yTc", [128, 8, TT], F32)
                    xt = sb(sc_, "xtC", [128, 8, TT], F32)
                    tmpc = Ring([sb(sc_, "tmpC%d" % i, [128, TT], F32) for i in range(2)])
                    lnr = {"rb": sb(sc_, "lnC_rb", [128, 8, TT], BF16), "sq": sb(sc_, "lnC_sq", [128, 8, TT], BF16), "sd": sb(sc_, "lnC_sd", [128, TT], F32), "mu": sb(sc_, "lnC_mu", [128, TT], F32)}

                    def run():
                        for t8 in range(tid, TOK // TT, nth):
                            s = (t8 * TT) // SEQ
                            tok0 = t8 * TT
                            load_x(src, tok0, xt)
                            for sub in range(4):
                                i = t8 * 4 + sub
                                g1, g2 = g1s.next(), g2s.next()
                                p.op("pool", (lambda e, g1=g1, i=i: e.indirect_dma_start(
                                    out=g1[:], out_offset=None, in_=Ys[:, :],
                                    in_offset=bass.IndirectOffsetOnAxis(ap=slots_i[:, i, 0:1].bitcast(U32), axis=0))),
                                    reads=[slots_i.b], writes=[g1.b], dma=g1.b)
                                p.op("pool", (lambda e, g2=g2, i=i: e.indirect_dma_start(
                                    out=g2[:], out_offset=None, in_=Ys[:, :],
                                    in_offset=bass.IndirectOffsetOnAxis(ap=slots_i[:, i, 1:2].bitcast(U32), axis=0))),
                                    reads=[slots_i.b], writes=[g2.b], dma=g2.b)
                                ACT(g1[:], g1[:], AF.Identity, [g1.b, wgt.b], [g1.b], scale=wgt[:, i, 0:1])
                                STT(g1[:], g2[:], wgt[:, i, 1:2], g1[:], ALU.mult, ALU.add, [g2.b, wgt.b, g1.b], [g1.b])
                                for half in range(2):
                                    tp = PA.next()
                                    for q in range(4):
                                        dc = half * 4 + q
                                        p.op("pe", (lambda e, o=tp[:, q * 128:(q + 1) * 128], a=g1[:, dc * 128:(dc + 1) * 128]: e.transpose(o, a, ident)),
                                             reads=[g1.b, cst.b], writes=[tp.b] if q == 0 else [], joins=[] if q == 0 else [tp.b])
                                    first = (sub == 0 and half == 0)
                                    p.op("act", (lambda e, o=yT[:, half * 4:(half + 1) * 4, sub * 128:(sub + 1) * 128], a=tp[:].rearrange("p (q t) -> p q t", q=4):
                                                 e.activation(out=o, in_=a, func=AF.Identity)),
                                         reads=[tp.b], writes=[yT.b] if first else [], joins=[] if first else [yT.b])
                            for dc in range(8):
                                tm = tmpc.next()
                                ACT(tm[:], yT[:, dc, :], AF.Identity, [yT.b, cond.b], [tm.b], scale=cv(l, 5, dc, s))
                                p.op("dve", (lambda e, dc=dc, tm=tm: e.scalar_tensor_tensor(out=xt[:, dc, :], in0=xt[:, dc, :], scalar=ALPHA, in1=tm[:],
                                                                                      op0=ALU.mult, op1=ALU.add)),
                                     reads=[xt.b, tm.b] if dc == 0 else [tm.b], writes=[xt.b] if dc == 0 else [], joins=[] if dc == 0 else [xt.b])
                            ln_fm(xt, 8, on1024, lnr)
                            ln_out_store(xt, 2, 3, l, dst, tok0)
                    return run
                p.run_threads([mk_thread(0, NTHR), mk_thread(1, NTHR)] if NTHR == 2 else [mk_thread(0, 1)], head=HEAD_C)
                p.barrier()

    SCALE = 128.0 ** -0.5

    def attn(src, dst, l):
        with ExitStack() as st:
            wout = sb(st, "wo_at", [128, 8, 1024], BF16)
            for j in range(2):
                p.op("pool", (lambda e, j=j: e.dma_start(out=wout[:, :, j * 512:(j + 1) * 512],
                                                          in_=od_w_out[:, j * 512:(j + 1) * 512].rearrange("(kc p) n -> p kc n", p=128))),
                     writes=[wout.b] if j == 0 else [], joins=[] if j == 0 else [wout.b], dma=wout.b)
            if "w" in ATTN_DEBUG:
                kT = sb(st, "kT", [128, 8, SEQ], BF16)
                qT = sb(st, "qT", [128, 8, SEQ], BF16)
            else:
                qT = sb(st, "qT", [128, 8, SEQ], BF16)
                kT = sb(st, "kT", [128, 8, SEQ], BF16)
            v1 = sb(st, "v1", [128, 16, 8, 132], BF16)
            oT = sb(st, "oT", [128, 8, SEQ], BF16)
            ksum = sb(st, "ksum", [128, 8, 8], F32)
            gate = sb(st, "gate", [128, 16, 8, 8], F32)
            selt = sb(st, "selt", [128, 16, 8, 8], F32)
            MSET("pool", v1[:].rearrange("p a b c -> p (a b c)"), 1.0, [v1.b])
            def do_seq(s):
                with ExitStack() as sa:
                    xt = sb(sa, "xta", [128, 8, TT], F32)
                    wr2 = Ring([sb(sa, "wqkv%d" % i, [128, 8, 512], BF16) for i in range(2)])
                    qf = Ring([sb(sa, "qf%d" % i, [128, TT], F32) for i in range(2)])
                    top8 = sb(sa, "top8", [128, 2, 8, 8], F32)
                    for t in range(4):
                        load_x(src, s * SEQ + t * TT, xt)
                        for dc in range(8):
                            first = (t == 0 and dc == 0)
                            p.op("act", (lambda e, o=oT[:, dc, t * TT:(t + 1) * TT], i=xt[:, dc, :], b=cv(l, 0, dc, s), sc=cv(l, 1, dc, s):
                                         e.activation(out=o, in_=i, func=AF.Identity, bias=b, scale=sc)),
                                 reads=[xt.b, cond.b], writes=[oT.b] if first else [], joins=[] if first else [oT.b])
                    for which, half in ((1, 0), (1, 1), (0, 0), (0, 1), (2, 0), (2, 1)):
                        if "kqv"[which] not in ATTN_DEBUG + "kqv" * ("A" not in ATTN_DEBUG or "x" not in ATTN_DEBUG):
                            continue
                        w = wr2.next()
                        c0 = which * 1024 + half * 512
                        if "m" in ATTN_DEBUG:
                            c0 = 1024 + half * 512
                        DMA("pool", w[:], od_w_qkv[:, c0:c0 + 512].rearrange("(kc p) n -> p kc n", p=128), [], [w.b], w.b)
                        for t in range(4):
                            if which in (0, 1):
                                for j in range(4):
                                    h = half * 4 + j
                                    ps = PA.next()
                                    for kc in range(8):
                                        MM(ps[:], w[:, kc, j * 128:(j + 1) * 128], oT[:, kc, t * TT:(t + 1) * TT], kc == 0, kc == 7, [w.b, oT.b], ps.b)
                                    if which == 1:
                                        p.op("act", (lambda e, o=kT[:, h, t * TT:(t + 1) * TT], i=ps[:]: e.activation(out=o, in_=i, func=AF.Identity)),
                                             reads=[ps.b], joins=[kT.b])
                                        p.op("dve", (lambda e, o=ksum[:, h, 2 * t:2 * t + 2], i=ps[:].rearrange("p (b k) -> p b k", b=2):
                                                     e.tensor_reduce(out=o, in_=i, axis=AX.X, op=ALU.add)),
                                             reads=[ps.b], joins=[ksum.b])
                                    else:
                                        q32 = qf.next()
                                        if "n" not in ATTN_DEBUG:
                                            ACT(q32[:], ps[:], AF.Identity, [ps.b], [q32.b])
                                        p.op("act", (lambda e, o=qT[:, h, t * TT:(t + 1) * TT], i=ps[:]: e.activation(out=o, in_=i, func=AF.Identity)),
                                             reads=[ps.b], joins=[qT.b])
                                        if "x" in ATTN_DEBUG and "g" not in ATTN_DEBUG:
                                            continue
                                        gp = PC.next()
                                        for sub in range(4):
                                            p.op("pe", (lambda e, gp=gp, sub=sub, q32=q32, h=h: e.matmul(gp[:, sub * 8:sub * 8 + 8], q32[:, sub * 128:(sub + 1) * 128],
                                                                                                   ksum[:, h, :], start=True, stop=True)),
                                                 reads=[q32.b, ksum.b], writes=[gp.b] if sub == 0 else [], joins=[] if sub == 0 else [gp.b])
                                        p.op("dve", (lambda e, gp=gp, h=h, t=t: e.tensor_copy(out=gate[:, t * 4:t * 4 + 4, h, :],
                                                                                         in_=gp[:, 0:32].rearrange("p (a n) -> p a n", a=4))),
                                             reads=[gp.b], joins=[gate.b])
                            else:
                                for sub in range(4):
                                    ps = PA.next()
                                    for kc in range(8):
                                        MM(ps[:], oT[:, kc, (t * 4 + sub) * 128:(t * 4 + sub + 1) * 128], w[:, kc, :], kc == 0, kc == 7, [w.b, oT.b], ps.b)
                                    eng = "act" if sub % 2 == 0 else "dve"
                                    if eng == "act":
                                        p.op("act", (lambda e, ps=ps, kt=t * 4 + sub, half=half: e.activation(out=v1[:, kt, half * 4:half * 4 + 4, 0:128],
                                                                                               in_=ps[:].rearrange("p (h d) -> p h d", h=4), func=AF.Identity)),
                                             reads=[ps.b], joins=[v1.b])
                                    else:
                                        p.op("dve", (lambda e, ps=ps, kt=t * 4 + sub, half=half: e.tensor_copy(out=v1[:, kt, half * 4:half * 4 + 4, 0:128],
                                                                                                in_=ps[:].rearrange("p (h d) -> p h d", h=4))),
                                             reads=[ps.b], joins=[v1.b])
                    for b in range(4, 8):
                        if "x" in ATTN_DEBUG and "s" not in ATTN_DEBUG:
                            continue
                        p.op("dve", (lambda e, b=b: e.memset(gate[:, 2 * b:2 * b + 2, :, b:8], NEG)), reads=[], joins=[gate.b]) if False else \
                            p.op("dve", (lambda e, b=b: e.memset(gate[:, 2 * b:2 * b + 2, :, b:8], NEG)), writes=[gate.b])
                        for qq in range(2):
                            for h in range(8):
                                first = (qq == 0 and h == 0)
                                p.op("dve", (lambda e, b=b, qq=qq, h=h: e.max(out=top8[:, qq, h, :], in_=gate[:, 2 * b + qq, h, :])),
                                     reads=[gate.b], writes=[top8.b] if first else [], joins=[] if first else [top8.b])
                        p.op("dve", (lambda e, b=b: e.tensor_tensor(out=selt[:, 2 * b:2 * b + 2, :, :], in0=gate[:, 2 * b:2 * b + 2, :, :],
                                                                     in1=top8[:, :, :, 2:3].to_broadcast([128, 2, 8, 8]), op=ALU.is_ge)),
                             reads=[gate.b, top8.b], writes=[selt.b])
                    p.barrier()
                if "B" not in ATTN_DEBUG:
                    return
                with ExitStack() as sbk:
                    def mkAt(tid, nth):
                        Es = Ring([sb(sbk, "E%d" % i, [128, 256], BF16) for i in range(6)])
                        accs = Ring([sb(sbk, "acc%d" % i, [128, 132], F32) for i in range(4)])
                        rden = Ring([sb(sbk, "rden%d" % i, [128, 1], F32) for i in range(4)])
                        o32 = Ring([sb(sbk, "o32_%d" % i, [128, 128], F32) for i in range(4)])

                        def run():
                            for h in range(tid, 8, nth):
                                for b in range(8):
                                    dense = b < 4
                                    order = [b] + list(range(b))
                                    accp = [accs.next(), accs.next()]
                                    ops_ = [None, None]
                                    for idx, n in enumerate(order):
                                        own = (n == b)
                                        newgrp = (idx == 0) or (not dense)
                                        lastgrp = (idx == len(order) - 1) or (not dense)
                                        if newgrp:
                                            ops_ = [PB.next(), PC.next()]
                                        Et = []
                                        for kt in range(2):
                                            sp_ = PA.next()
                                            MM(sp_[:, 0:256], kT[:, h, (2 * n + kt) * 128:(2 * n + kt + 1) * 128], qT[:, h, b * 256:(b + 1) * 256], True, True, [kT.b, qT.b], sp_.b)
                                            E = Es.next()
                                            ACT(E[:], sp_[:, 0:256], AF.Exp, [sp_.b], [E.b], scale=SCALE)
                                            if own:
                                                c0 = kt * 128
                                                p.op("dve", (lambda e, E=E, c0=c0: e.tensor_tensor(out=E[:, c0:c0 + 128], in0=E[:, c0:c0 + 128], in1=trib[:], op=ALU.mult)),
                                                     reads=[trib.b], writes=[E.b])
                                            Et.append(E)
                                        for qs in range(2):
                                            kts = [kt for kt in range(2) if not (own and kt == 1 and qs == 0)]
                                            for kt in kts:
                                                first = newgrp and (kt == kts[0])
                                                last = lastgrp and (kt == kts[-1])
                                                MM(ops_[qs][:, 0:129], Et[kt][:, qs * 128:(qs + 1) * 128], v1[:, 2 * n + kt, h, 0:129], first, last, [Et[kt].b, v1.b], ops_[qs].b)
                                            if lastgrp:
                                                acc = accp[qs]
                                                if dense or own:
                                                    ACT(acc[:, 0:129], ops_[qs][:, 0:129], AF.Identity, [ops_[qs].b], [acc.b])
                                                else:
                                                    STT(acc[:, 0:129], ops_[qs][:, 0:129], selt[:, 2 * b + qs, h, n:n + 1], acc[:, 0:129], ALU.mult, ALU.add,
                                                        [ops_[qs].b, selt.b], [acc.b])
                                    for qs in range(2):
                                        acc = accp[qs]
                                        rd = rden.next()
                                        RECIP(rd[:], acc[:, 128:129], [acc.b], [rd.b])
                                        o = o32.next()
                                        TS("dve", o[:], acc[:, 0:128], rd[:, 0:1], None, ALU.mult, None, [acc.b, rd.b], [o.b])
                                        tp = PA.next()
                                        p.op("pe", (lambda e, tp=tp, o=o: e.transpose(tp[:, 0:128], o[:], ident)), reads=[o.b, cst.b], writes=[tp.b])
                                        qt = 2 * b + qs
                                        p.op("act", (lambda e, tp=tp, h=h, qt=qt: e.activation(out=oT[:, h, qt * 128:(qt + 1) * 128], in_=tp[:, 0:128], func=AF.Identity)),
                                             reads=[tp.b], joins=[oT.b])
                        return run
                    p.run_threads([mkAt(0, NTHR), mkAt(1, NTHR)] if NTHR == 2 else [mkAt(0, 1)], head=HEAD_AT)
                    if debug and s == 0:
                        dbo = nc.dram_tensor("dbg_oT", [128, 8, SEQ], BF16, kind="ExternalOutput").ap()
                        DMA("sp", dbo[:, :, :], oT[:], [oT.b], [], oT.b)
                        dbq = nc.dram_tensor("dbg_qT", [128, 8, SEQ], BF16, kind="ExternalOutput").ap()
                        DMA("sp", dbq[:, :, :], qT[:], [qT.b], [], qT.b)
                        dbk = nc.dram_tensor("dbg_kT", [128, 8, SEQ], BF16, kind="ExternalOutput").ap()
                        DMA("sp", dbk[:, :, :], kT[:], [kT.b], [], kT.b)
                        dbv = nc.dram_tensor("dbg_v1", [128, 16, 8, 132], BF16, kind="ExternalOutput").ap()
                        DMA("sp", dbv[:, :, :, :], v1[:], [v1.b], [], v1.b)
                    p.barrier()
                if "C" not in ATTN_DEBUG:
                    return
                with ExitStack() as sc_:
                    xt = sb(sc_, "xtc", [128, 8, TT], F32)
                    tmp = Ring([sb(sc_, "tmpc%d" % i, [128, TT], F32) for i in range(2)])
                    lnr = {"rb": sb(sc_, "lnc_rb", [128, 8, TT], BF16), "sq": sb(sc_, "lnc_sq", [128, 8, TT], BF16), "sd": sb(sc_, "lnc_sd", [128, TT], F32), "mu": sb(sc_, "lnc_mu", [128, TT], F32)}
                    for t in range(4):
                        tok0 = s * SEQ + t * TT
                        load_x(src, tok0, xt)
                        for dc in range(8):
                            ps = PA.next()
                            for kc in range(8):
                                MM(ps[:], wout[:, kc, dc * 128:(dc + 1) * 128], oT[:, kc, t * TT:(t + 1) * TT], kc == 0, kc == 7, [wout.b, oT.b], ps.b)
                            tm = tmp.next()
                            ACT(tm[:], ps[:], AF.Identity, [ps.b, cond.b], [tm.b], scale=cv(l, 2, dc, s))
                            p.op("dve", (lambda e, dc=dc, tm=tm: e.scalar_tensor_tensor(out=xt[:, dc, :], in0=xt[:, dc, :], scalar=ALPHA, in1=tm[:],
                                                                                  op0=ALU.mult, op1=ALU.add)),
                                 reads=[xt.b, tm.b] if dc == 0 else [tm.b], writes=[xt.b] if dc == 0 else [], joins=[] if dc == 0 else [xt.b])
                        ln_fm(xt, 8, on1024, lnr)
                        ln_out_store(xt, 0, 1, l, dst, tok0)
                    p.barrier()

            for s_ in range(NSEQ):
                do_seq(s_)

    if only == "attn":
        attn(xT, yT, 1)
        stages = 0
    def moe_any(src, dst, l, zero_fill):
        if SPARSE:
            moe_sparse(src, dst, l, zero_fill)
        else:
            moe(src, dst, l)

    if only == "moe":
        moe_any(xT, yT, 0, True)
        stages = 0
    if stages >= 1:
        mixer0(xT, yT if stages == 1 else XA, 0)
    if stages >= 2:
        moe_any(XA, yT if stages == 2 else XB, 0, True)
    if stages >= 3:
        attn(XB, yT if stages == 3 else XA, 1)
    if stages >= 4:
        moe_any(XA, yT, 1, False)

    p.barrier()
    p.emit()
    return nc, es


def prep_inputs(inputs):
    f = lambda a: np.ascontiguousarray(np.asarray(a, dtype=np.float32))
    x = f(inputs["x"])
    c = f(inputs["c"])
    shared = {}
    shared["ada_w"] = f(inputs["ada_w"])
    shared["ada_bT"] = f(inputs["ada_b"].reshape(2, 48, 128).transpose(2, 0, 1))
    lnp = np.stack([inputs["ln_mix_g"], inputs["ln_mix_b"], inputs["ln_ffn_g"], inputs["ln_ffn_b"]], 1)
    shared["lnp"] = f(lnp.reshape(2, 4, 8, 128).transpose(3, 0, 1, 2))
    shared["ev_w_in"] = f(inputs["ev_w_in"][0])
    shared["ev_w_out"] = f(inputs["ev_w_out"][0])
    sg = np.stack([inputs["ev_sgu_ln_g"][0], inputs["ev_sgu_ln_b"][0]], 0)
    shared["sgp"] = f(sg.reshape(2, 4, 128).transpose(2, 0, 1))
    shared["wsT"] = f(inputs["ev_w_s"][0].transpose(2, 0, 1))
    shared["bs"] = f(inputs["ev_b_s"][0].reshape(1, 512))
    shared["wdw"] = f(inputs["ev_w_dw"][0].reshape(31, 4, 128).transpose(2, 1, 0))
    cv = np.stack([inputs["ev_b_dw"][0], inputs["ev_conv_ln_g"][0], inputs["ev_conv_ln_b"][0]], 0)
    shared["cvp"] = f(cv.reshape(3, 4, 128).transpose(2, 0, 1))
    shared["od_w_qkv"] = f(inputs["od_w_qkv"][0])
    shared["od_w_out"] = f(inputs["od_w_out"][0])
    wr = np.concatenate([inputs["moe_w_grp"], inputs["moe_w_er"].transpose(0, 2, 1, 3).reshape(2, D, 16)], -1)
    shared["wr"] = f(wr.reshape(2, 8, 128, 20).transpose(0, 2, 1, 3))
    shared["br"] = f(np.concatenate([inputs["moe_b_grp"], inputs["moe_b_er"].reshape(2, 16)], -1).reshape(2, 1, 20))
    shared["moe_w_gate"] = f(np.asarray(inputs["moe_w_gate"]).reshape(2, 16, 8, 128, 512).transpose(0, 1, 3, 2, 4))
    shared["moe_w_up"] = f(np.asarray(inputs["moe_w_up"]).reshape(2, 16, 8, 128, 512).transpose(0, 1, 3, 2, 4))
    shared["moe_w_down"] = f(np.asarray(inputs["moe_w_down"]).reshape(2, 16, 4, 128, D).transpose(0, 1, 3, 2, 4))
    cst = np.zeros((128, 2473), np.float32)
    cst[:, 2472] = np.arange(128, dtype=np.float32)
    cst[:, 2432:2440] = np.arange(8, dtype=np.float32)[None, :] * 512.0
    cst[:, 2440:2472] = np.arange(32, dtype=np.float32)[None, :] * 512.0
    cst[:, 0:128] = np.eye(128, dtype=np.float32)
    cst[:, 128:256] = np.triu(np.ones((128, 128), np.float32))
    cst[:, 256:384] = 1.0
    for e in range(16):
        cst[e, 384 + e * 128:384 + (e + 1) * 128] = 1.0
    shared["consts"] = cst
    maps = []
    for core in range(NCORES):
        m = dict(shared)
        xs = x[core * NSEQ:(core + 1) * NSEQ]
        m["xT"] = f(xs.reshape(TOK, 8, 128).transpose(1, 2, 0))
        m["cT"] = f(c[core * NSEQ:(core + 1) * NSEQ].reshape(NSEQ, 8, 128).transpose(2, 1, 0))
        maps.append(m)
    return maps


_CACHE = {}


def kernel(**inputs):
    maps = prep_inputs(inputs)
    if "nc" not in _CACHE:
        _CACHE["nc"] = build()
    nc, _ = _CACHE["nc"]
    res = run_bass_kernel_spmd(nc, maps, core_ids=list(range(NCORES)))
    out = np.empty((NCORES * NSEQ, SEQ, D), np.float32)
    for core in range(NCORES):
        yT = np.asarray(res.results[core]["yT"])
        out[core * NSEQ:(core + 1) * NSEQ] = yT.transpose(2, 0, 1).reshape(NSEQ, SEQ, D)
    return out
```

```python
import threading
import numpy as np
from contextlib import ExitStack
import concourse.bass as bass
import concourse.mybir as mybir
from concourse.bass_utils import run_bass_kernel_spmd

F32 = mybir.dt.float32
BF16 = mybir.dt.bfloat16
I32 = mybir.dt.int32
U32 = mybir.dt.uint32
AF = mybir.ActivationFunctionType
ALU = mybir.AluOpType
AX = mybir.AxisListType

NCORES = 8
D = 1024
SEQ = 2048
NSEQ = 2
TOK = NSEQ * SEQ
TT = 512
ALPHA = 4.0 ** 0.25
EPS = 1e-5
NEG = -1.0e30
ATTN_DEBUG = "ABC"
SPARSE = True
MOE_DEBUG = "ABC"
NTHR = 2
HEAD_M = 250
HEAD_A = 56
HEAD_B = 83
HEAD_C = 70
HEAD_AT = 300

ENGS = ("pe", "act", "dve", "pool", "sp")


class Buf:
    __slots__ = ("name", "writers", "readers", "dsem", "dcount", "excl", "dslot")

    def __init__(self, name):
        self.name = name
        self.writers = []
        self.readers = []
        self.dsem = None
        self.dcount = 0
        self.excl = False
        self.dslot = None


class Op:
    __slots__ = ("eng", "fn", "waits", "is_dma", "is_nop", "dbuf", "value", "needed", "eidx", "semval", "epoch")

    def __init__(self, eng, fn):
        self.eng = eng
        self.fn = fn
        self.waits = []
        self.is_dma = False
        self.is_nop = False
        self.dbuf = None
        self.value = 0
        self.needed = False
        self.eidx = 0
        self.semval = 0
        self.epoch = 0


class Turns:
    def __init__(self, n):
        self.n = n
        self.cur = 0
        self.alive = [True] * n
        self.cv = threading.Condition()
        self.local = threading.local()
        self.head = 0

    def _advance(self):
        for k in range(1, self.n + 1):
            c = (self.cur + k) % self.n
            if self.alive[c]:
                self.cur = c
                return

    def start(self, tid):
        self.local.tid = tid
        with self.cv:
            while self.cur != tid:
                self.cv.wait()

    def yield_turn(self):
        tid = self.local.tid
        if tid == 0 and self.head > 0:
            self.head -= 1
            return
        with self.cv:
            self._advance()
            self.cv.notify_all()
            while self.cur != tid:
                self.cv.wait()

    def finish(self):
        tid = self.local.tid
        with self.cv:
            self.alive[tid] = False
            if any(self.alive):
                self._advance()
            self.cv.notify_all()


class Prog:
    def __init__(self, nc, es):
        self.nc = nc
        self.es = es
        self.ops = {e: [] for e in ENGS}
        self.clock = {e: {} for e in ENGS}
        self.esem = {}
        self.bufs = []
        self.same_engine_sync = True
        self.epoch = 0
        self.turns = None
        self.slots = []
        self.free_slots = []

    def buf(self, name):
        b = Buf(name)
        self.bufs.append(b)
        return b

    def _dep(self, op, d):
        E = op.eng
        if d.is_dma:
            key = ("d", id(d.dbuf))
            if self.clock[E].get(key, 0) >= d.value:
                return
            self.clock[E][key] = d.value
            op.waits.append(("d", d.dbuf, d.value))
        else:
            if d.is_nop:
                return
            if d.eng == E and (E == "pe" or not self.same_engine_sync):
                return
            key = ("e", d.eng)
            if self.clock[E].get(key, 0) >= d.eidx:
                return
            self.clock[E][key] = d.eidx
            d.needed = True
            op.waits.append(("e", d.eng, d))

    def op(self, eng, fn, reads=(), writes=(), joins=(), dma=None):
        o = Op(eng, fn)
        o.epoch = self.epoch
        lst = self.ops[eng]
        lst.append(o)
        o.eidx = len(lst)
        if dma is not None:
            o.is_dma = True
            if dma.dslot is None:
                if self.free_slots:
                    dma.dslot = self.free_slots.pop()
                else:
                    dma.dslot = Buf("slot%d" % len(self.slots))
                    self.slots.append(dma.dslot)
            sl = dma.dslot
            o.dbuf = sl
            sl.dcount += 16
            o.value = sl.dcount
        deps = []
        for b in reads:
            deps.extend(b.writers)
            if b.excl:
                deps.extend(r for r in b.readers if r.eng != eng)
        for b in writes:
            deps.extend(b.writers)
            deps.extend(b.readers)
        for b in joins:
            if b.readers:
                deps.extend(b.writers)
                deps.extend(b.readers)
        best = {}
        for d in deps:
            if d.is_dma:
                key = ("d", id(d.dbuf))
                v = d.value
            else:
                if d.is_nop:
                    continue
                key = ("e", d.eng)
                v = d.eidx
            if key not in best or best[key][0] < v:
                best[key] = (v, d)
        for key in best:
            self._dep(o, best[key][1])
        for b in reads:
            b.readers.append(o)
        for b in writes:
            b.writers = [o]
            b.readers = []
        for b in joins:
            if b.readers:
                b.writers = [o]
                b.readers = []
            else:
                b.writers.append(o)
        if self.turns is not None:
            self.turns.yield_turn()
        return o

    def run_threads(self, fns, head=0):
        if len(fns) == 1:
            fns[0]()
            return
        turns = Turns(len(fns))
        turns.head = head
        self.turns = turns
        errs = []

        def wrap(tid, fn):
            turns.start(tid)
            try:
                fn()
            except BaseException as ex:
                errs.append(ex)
            finally:
                turns.finish()
        ths = [threading.Thread(target=wrap, args=(i, f)) for i, f in enumerate(fns)]
        for t in ths:
            t.start()
        for t in ths:
            t.join()
        self.turns = None
        if errs:
            raise errs[0]

    def barrier(self):
        lasts = {}
        for e in ENGS:
            for o in reversed(self.ops[e]):
                if not o.is_dma and not o.is_nop:
                    lasts[e] = o
                    break
        dm = [(b, b.dcount) for b in self.slots if b.dcount > 0]
        for e in ENGS:
            o = Op(e, lambda eng: eng.nop())
            o.is_nop = True
            self.ops[e].append(o)
            o.eidx = len(self.ops[e])
            for e2, l in lasts.items():
                if e2 == e:
                    continue
                key = ("e", e2)
                if self.clock[e].get(key, 0) >= l.eidx:
                    continue
                self.clock[e][key] = l.eidx
                l.needed = True
                o.waits.append(("e", e2, l))
            for b, v in dm:
                key = ("d", id(b))
                if self.clock[e].get(key, 0) >= v:
                    continue
                self.clock[e][key] = v
                o.waits.append(("d", b, v))
        for b in self.bufs:
            b.writers = []
            b.readers = []
            if b.dslot is not None:
                self.free_slots.append(b.dslot)
                b.dslot = None

    def emit(self):
        nc, es = self.nc, self.es
        for e in ENGS:
            eps_ = sorted(set(o.epoch for o in self.ops[e] if o.needed))
            for ep in eps_:
                self.esem[(e, ep)] = es.enter_context(nc.semaphore("es_%s_%d" % (e, ep)))
        n = 0
        for e in ENGS:
            for o in self.ops[e]:
                if o.is_dma and o.dbuf.dsem is None:
                    o.dbuf.dsem = es.enter_context(nc.semaphore("ds%d" % n))
                    n += 1
        self.ndsem = n
        for e in ENGS:
            c = {}
            for o in self.ops[e]:
                if o.needed:
                    c[o.epoch] = c.get(o.epoch, 0) + 1
                    o.semval = c[o.epoch]
        block = es.enter_context(nc.Block())

        def run(ename):
            def body(eng):
                for o in self.ops[ename]:
                    for (k, a, b) in o.waits:
                        if k == "d":
                            eng.wait_ge(a.dsem, b)
                        else:
                            eng.wait_ge(self.esem[(a, b.epoch)], b.semval)
                    ins = o.fn(eng)
                    if o.is_dma:
                        ins.then_inc(o.dbuf.dsem, 16)
                    elif o.needed:
                        ins.then_inc(self.esem[(ename, o.epoch)], 1)
            return body

        block.tensor(run("pe"))
        block.scalar(run("act"))
        block.vector(run("dve"))
        block.gpsimd(run("pool"))
        block.sync(run("sp"))


class TB:
    def __init__(self, p, t, name):
        self.t = t
        self.b = p.buf(name)

    def __getitem__(self, k):
        return self.t[k]


class Ring:
    def __init__(self, items):
        self.items = items
        self.i = 0

    def next(self):
        r = self.items[self.i % len(self.items)]
        self.i += 1
        return r


def build(stages=4, debug=False, only=None):
    nc = bass.Bass("TRN2", target_bir_lowering=False)

    def din(name, shape):
        return nc.dram_tensor(name, list(shape), F32, kind="ExternalInput").ap()

    xT = din("xT", [8, 128, TOK])
    cT = din("cT", [128, 8, NSEQ])
    ada_w = din("ada_w", [2, D, 6 * D])
    ada_bT = din("ada_bT", [128, 2, 48])
    lnp = din("lnp", [128, 2, 4, 8])
    ev_w_in = din("ev_w_in", [D, 2048])
    ev_w_out = din("ev_w_out", [D, D])
    sgp_d = din("sgp", [128, 2, 4])
    wsT_d = din("wsT", [128, 4, 128])
    bs_d = din("bs", [1, 512])
    wdw_d = din("wdw", [128, 4, 31])
    cvp_d = din("cvp", [128, 3, 4])
    od_w_qkv = din("od_w_qkv", [D, 3 * D])
    od_w_out = din("od_w_out", [D, D])
    wr_d = din("wr", [2, 128, 8, 20])
    br_d = din("br", [2, 1, 20])
    w_gate = din("moe_w_gate", [2, 16, 128, 8, 512])
    w_up = din("moe_w_up", [2, 16, 128, 8, 512])
    w_down = din("moe_w_down", [2, 16, 128, 4, D])
    consts_d = din("consts", [128, 2473])

    yT = nc.dram_tensor("yT", [8, 128, TOK], F32, kind="ExternalOutput").ap()
    XA = nc.dram_tensor("XA", [8, 128, TOK], F32, kind="Internal").ap()
    XB = nc.dram_tensor("XB", [8, 128, TOK], F32, kind="Internal").ap()
    dbg = {}

    es = ExitStack()
    p = Prog(nc, es)

    uniq = [0]

    def sb(st, name, shape, dt):
        uniq[0] += 1
        name = "%s_u%d" % (name, uniq[0])
        return TB(p, st.enter_context(nc.sbuf_tensor(name, list(shape), dt)), name)

    def MM(out, lhsT, rhs, first, last, reads, wb):
        p.op("pe", lambda e: e.matmul(out, lhsT, rhs, start=first, stop=last), reads=reads,
             writes=[wb] if first else [], joins=[] if first else [wb])

    def MMg(out, lhsT, rhs, reads, wb, newgroup):
        p.op("pe", lambda e: e.matmul(out, lhsT, rhs, start=True, stop=True), reads=reads,
             writes=[wb] if newgroup else [], joins=[] if newgroup else [wb])

    def ACT(out, in_, func, reads, writes, bias=None, scale=None):
        kw = {}
        if bias is not None:
            kw["bias"] = bias
        if scale is not None:
            kw["scale"] = scale
        p.op("act", lambda e: e.activation(out=out, in_=in_, func=func, **kw), reads=reads, writes=writes)

    def TTo(eng, out, in0, in1, op, reads, writes):
        p.op(eng, lambda e: e.tensor_tensor(out=out, in0=in0, in1=in1, op=op), reads=reads, writes=writes)

    def TS(eng, out, in0, s1, s2, op0, op1, reads, writes):
        if op1 is None:
            p.op(eng, lambda e: e.tensor_scalar(out=out, in0=in0, scalar1=s1, scalar2=None, op0=op0),
                 reads=reads, writes=writes)
        else:
            p.op(eng, lambda e: e.tensor_scalar(out=out, in0=in0, scalar1=s1, scalar2=s2, op0=op0, op1=op1),
                 reads=reads, writes=writes)

    def STT(out, in0, scalar, in1, op0, op1, reads, writes):
        p.op("dve", lambda e: e.scalar_tensor_tensor(out=out, in0=in0, scalar=scalar, in1=in1, op0=op0, op1=op1),
             reads=reads, writes=writes)

    def CP(eng, out, in_, reads, writes):
        p.op(eng, lambda e: e.tensor_copy(out=out, in_=in_), reads=reads, writes=writes)

    def RED(out, in_, op, reads, writes):
        p.op("dve", lambda e: e.tensor_reduce(out=out, in_=in_, axis=AX.X, op=op), reads=reads, writes=writes)

    def RECIP(out, in_, reads, writes):
        p.op("dve", lambda e: e.reciprocal(out=out, in_=in_), reads=reads, writes=writes)

    def MSET(eng, ap, val, writes):
        p.op(eng, lambda e: e.memset(ap, val), writes=writes)

    def DMA(q, out, in_, reads, writes, dbuf):
        p.op(q, lambda e: e.dma_start(out=out, in_=in_), reads=reads, writes=writes, dma=dbuf)

    G = ExitStack()
    es.enter_context(G)
    psb = [TB(p, G.enter_context(nc.psum_tensor("ps%d" % i, [128, 512], F32)), "ps%d" % i) for i in range(8)]
    for t_ in psb:
        t_.b.excl = True
    class TRing:
        def __init__(self, items):
            self.full = Ring(items)
            h = len(items) // 2
            self.sub = [Ring(items[:h]), Ring(items[h:])]

        def next(self):
            if p.turns is not None:
                return self.sub[p.turns.local.tid % 2].next()
            return self.full.next()
    PA = TRing(psb[0:4])
    PB = TRing(psb[4:6])
    PC = TRing(psb[6:8])

    cst = sb(G, "cst", [128, 2473], F32)
    DMA("sp", cst[:], consts_d[:, :], [], [cst.b], cst.b)
    ident = cst[:, 0:128]
    tri = cst[:, 128:256]
    ones = cst[:, 256:384]
    sel = cst[0:16, 384:2432]
    thr8 = cst[:, 2432:2440]
    jt32 = cst[:, 2440:2472]
    pcol = cst[:, 2472:2473]

    identb = sb(G, "identb", [128, 128], BF16)
    trib = sb(G, "trib", [128, 128], BF16)
    onesb = sb(G, "onesb", [128, 128], BF16)
    on1024 = sb(G, "on1024", [128, 128], BF16)
    on512 = sb(G, "on512", [128, 128], BF16)
    CP("dve", identb[:], ident, [cst.b], [identb.b])
    CP("dve", trib[:], tri, [cst.b], [trib.b])
    CP("dve", onesb[:], ones, [cst.b], [onesb.b])
    TS("dve", on1024[:], ones, 1.0 / 1024, None, ALU.mult, None, [cst.b], [on1024.b])
    TS("dve", on512[:], ones, 1.0 / 512, None, ALU.mult, None, [cst.b], [on512.b])
    ustrb = sb(G, "ustrb", [128, 128], BF16)
    TTo("dve", ustrb[:], tri, ident, ALU.subtract, [cst.b], [ustrb.b])
    epst = sb(G, "epst", [128, 1], F32)
    MSET("dve", epst[:], EPS, [epst.b])

    lnp_sb = sb(G, "lnp_sb", [128, 2, 4, 8], F32)
    DMA("sp", lnp_sb[:], lnp[:, :, :, :], [], [lnp_sb.b], lnp_sb.b)
    cond = sb(G, "cond", [128, 2, 48, NSEQ], F32)

    with ExitStack() as st:
        cT_sb = sb(st, "cT_sb", [128, 8, NSEQ], F32)
        csil = sb(st, "csil", [128, 8, NSEQ], BF16)
        adab = sb(st, "adab", [128, 2, 48], F32)
        DMA("sp", cT_sb[:], cT[:, :, :], [], [cT_sb.b], cT_sb.b)
        DMA("sp", adab[:], ada_bT[:, :, :], [], [adab.b], adab.b)
        ACT(csil[:], cT_sb[:], AF.Silu, [cT_sb.b], [csil.b])
        wb2 = [sb(st, "adaw%d" % i, [128, 8, 1024], BF16) for i in range(2)]
        k = 0
        for l in range(2):
            for s6 in range(6):
                w = wb2[k % 2]
                k += 1
                DMA("pool", w[:], ada_w[l, :, s6 * 1024:(s6 + 1) * 1024].rearrange("(kc p) n -> p kc n", p=128),
                    [], [w.b], w.b)
                ps = PC.next()
                for dc in range(8):
                    for kc in range(8):
                        first = (dc == 0 and kc == 0)
                        p.op("pe", (lambda e, o=ps[:, dc * 2:dc * 2 + 2], a=w[:, kc, dc * 128:(dc + 1) * 128],
                                    b=csil[:, kc, :], f=(kc == 0), la=(kc == 7): e.matmul(o, a, b, start=f, stop=la)),
                             reads=[w.b, csil.b], writes=[ps.b] if first else [], joins=[] if first else [ps.b])
                add1 = 1.0 if s6 in (1, 2, 4, 5) else 0.0
                for s in range(NSEQ):
                    STT(cond[:, l, s6 * 8:(s6 + 1) * 8, s], ps[:, s:16:2], add1, adab[:, l, s6 * 8:(s6 + 1) * 8],
                        ALU.add, ALU.add, [ps.b, adab.b], [cond.b])
        p.barrier()

    def cv(l, split, dc, s):
        return cond[:, l, split * 8 + dc, s:s + 1]

    def load_x(src, tok0, xt, W=TT):
        DMA("sp", xt[:], src[:, :, tok0:tok0 + W].rearrange("kc p t -> p kc t"), [], [xt.b], xt.b)

    def modulate(xt, hT, l, split_sh, split_sc, s):
        for dc in range(8):
            ACT(hT[:, dc, :], xt[:, dc, :], AF.Identity, [xt.b, cond.b], [hT.b] if dc == 0 else [],
                bias=cv(l, split_sh, dc, s), scale=cv(l, split_sc, dc, s)) if dc == 0 else \
                p.op("act", (lambda e, o=hT[:, dc, :], i=xt[:, dc, :], b=cv(l, split_sh, dc, s), sc=cv(l, split_sc, dc, s):
                             e.activation(out=o, in_=i, func=AF.Identity, bias=b, scale=sc)),
                     reads=[xt.b, cond.b], joins=[hT.b])

    def ln_fm(r, nch, onb, lnT, W=TT):
        rb, sd, mu = lnT["rb"], lnT["sd"], lnT["mu"]
        if "sqf" in lnT:
            sqf, sqb = lnT["sqf"], lnT["sqb"]
        else:
            sqf, sqb = (lambda c, t_=lnT["sq"]: t_[:, c, :]), [lnT["sq"].b]
        for c in range(nch):
            p.op("act", (lambda e, o=sqf(c), i=r[:, c, :]: e.activation(out=o, in_=i, func=AF.Square)),
                 reads=[r.b], writes=sqb if c == 0 else [], joins=[] if c == 0 else sqb)
            p.op("dve", (lambda e, o=rb[:, c, :], i=r[:, c, :]: e.tensor_copy(out=o, in_=i)),
                 reads=[r.b], writes=[rb.b] if c == 0 else [], joins=[] if c == 0 else [rb.b])
        mps = PB.next()
        for c in range(nch):
            MM(mps[:, 0:W], onb[:], rb[:, c, :], c == 0, c == nch - 1, [onb.b, rb.b], mps.b)
        vps = PC.next()
        for c in range(nch):
            MM(vps[:, 0:W], onb[:], sqf(c), c == 0, c == nch - 1, [onb.b] + sqb, vps.b)
        ACT(mu[:], mps[:, 0:W], AF.Identity, [mps.b], [mu.b])
        STT(sd[:], mu[:], -1.0, mu[:], ALU.mult, ALU.mult, [mu.b], [sd.b])
        STT(sd[:], vps[:, 0:W], EPS, sd[:], ALU.add, ALU.add, [vps.b, sd.b], [sd.b])
        ACT(sd[:], sd[:], AF.Sqrt, [sd.b], [sd.b])
        RECIP(vps[:, 0:W], sd[:], [sd.b], [vps.b])
        for c in range(nch):
            p.op("dve", (lambda e, o=r[:, c, :], a=r[:, c, :], b=mps[:, 0:W]: e.tensor_tensor(out=o, in0=a, in1=b, op=ALU.subtract)),
                 reads=[mps.b, r.b] if c == 0 else [mps.b], writes=[r.b] if c == 0 else [], joins=[] if c == 0 else [r.b])
        for c in range(nch):
            p.op("dve", (lambda e, o=r[:, c, :], a=r[:, c, :], b=vps[:, 0:W]: e.tensor_tensor(out=o, in0=a, in1=b, op=ALU.mult)),
                 reads=[vps.b, r.b] if c == 0 else [vps.b], writes=[r.b] if c == 0 else [], joins=[] if c == 0 else [r.b])

    def ln_out_store(r, gi, bi, l, dst, tok0, W=TT):
        xo = r
        for dc in range(8):
            p.op("act", (lambda e, o=xo[:, dc, :], i=r[:, dc, :], sc=lnp_sb[:, l, gi, dc:dc + 1], b=lnp_sb[:, l, bi, dc:dc + 1]:
                         e.activation(out=o, in_=i, func=AF.Identity, bias=b, scale=sc)),
                 reads=[r.b, lnp_sb.b] if dc == 0 else [lnp_sb.b], writes=[xo.b] if dc == 0 else [], joins=[] if dc == 0 else [xo.b])
        DMA("act", dst[:, :, tok0:tok0 + W].rearrange("kc p t -> p kc t"), xo[:], [xo.b], [], xo.b)

    def mixer0(src, dst, l):
        with ExitStack() as st:
            win = sb(st, "win", [128, 8, 2048], BF16)
            wout = sb(st, "wout", [128, 8, 1024], BF16)
            for j in range(4):
                p.op("pool", (lambda e, j=j: e.dma_start(out=win[:, :, j * 512:(j + 1) * 512],
                                                          in_=ev_w_in[:, j * 512:(j + 1) * 512].rearrange("(kc p) n -> p kc n", p=128))),
                     writes=[win.b] if j == 0 else [], joins=[] if j == 0 else [win.b], dma=win.b)
            for j in range(2):
                p.op("pool", (lambda e, j=j: e.dma_start(out=wout[:, :, j * 512:(j + 1) * 512],
                                                          in_=ev_w_out[:, j * 512:(j + 1) * 512].rearrange("(kc p) n -> p kc n", p=128))),
                     writes=[wout.b] if j == 0 else [], joins=[] if j == 0 else [wout.b], dma=wout.b)
            sgp = sb(st, "sgp_sb", [128, 2, 4], F32)
            DMA("sp", sgp[:], sgp_d[:, :, :], [], [sgp.b], sgp.b)
            cvp = sb(st, "cvp_sb", [128, 3, 4], F32)
            DMA("sp", cvp[:], cvp_d[:, :, :], [], [cvp.b], cvp.b)
            wsTm = sb(st, "wsTm", [128, 4, 128], BF16)
            C4 = sb(st, "C4", [128, 4, 128], F32)
            diag = sb(st, "diag", [128, 4, 31, 128], BF16)
            stmp = ExitStack()
            wsT = sb(stmp, "wsT_sb", [128, 4, 128], F32)
            DMA("sp", wsT[:], wsT_d[:, :, :], [], [wsT.b], wsT.b)
            bsB = sb(stmp, "bsB", [128, 4, 128], F32)
            DMA("sp", bsB[:].rearrange("p g q -> p (g q)"), bs_d[0:1, :].to_broadcast([128, 512]), [], [bsB.b], bsB.b)
            wdw = sb(stmp, "wdw_sb", [128, 4, 31], F32)
            DMA("sp", wdw[:], wdw_d[:, :, :], [], [wdw.b], wdw.b)
            for g in range(4):
                p.op("dve", (lambda e, g=g: e.tensor_tensor(out=wsTm[:, g, :], in0=wsT[:, g, :], in1=tri, op=ALU.mult)),
                     reads=[wsT.b, cst.b], writes=[wsTm.b] if g == 0 else [], joins=[] if g == 0 else [wsTm.b])
            rps = PC.next()
            for g in range(4):
                p.op("pe", (lambda e, g=g: e.matmul(rps[:, g * 128:(g + 1) * 128], onesb[:], wsTm[:, g, :], start=True, stop=True)),
                     reads=[onesb.b, wsTm.b], writes=[rps.b] if g == 0 else [], joins=[] if g == 0 else [rps.b])
            for g in range(4):
                p.op("dve", (lambda e, g=g: e.scalar_tensor_tensor(out=C4[:, g, :], in0=rps[:, g * 128:(g + 1) * 128],
                                                                    scalar=sgp[:, 1, g:g + 1], in1=bsB[:, g, :],
                                                                    op0=ALU.mult, op1=ALU.add)),
                     reads=[rps.b, sgp.b, bsB.b], writes=[C4.b] if g == 0 else [], joins=[] if g == 0 else [C4.b])
            for c in range(4):
                for k in range(31):
                    p.op("dve", (lambda e, c=c, k=k: e.tensor_scalar(out=diag[:, c, k, :], in0=ident, scalar1=wdw[:, c, k:k + 1],
                                                                      scalar2=None, op0=ALU.mult)),
                         reads=[cst.b, wdw.b], writes=[diag.b] if (c == 0 and k == 0) else [],
                         joins=[] if (c == 0 and k == 0) else [diag.b])

            p.barrier()
            stmp.close()
            WM = 256
            NSB = WM // 128

            def mkM(s):
                GL = sb(st, "GL", [128, 4, 30 + SEQ], BF16).t
                GLh = p.buf("GLh")
                GLb = [p.buf("GLb%d" % i) for i in range(SEQ // WM)]
                MSET("pool", GL[:, :, 0:30], 0.0, [GLh])
                xt = sb(st, "xt", [128, 8, WM], F32)
                hT = sb(st, "hT", [128, 8, WM], BF16)
                uT = sb(st, "uT", [128, 4, WM], F32)
                vg = Ring([sb(st, "vg%d" % i, [128, TT], F32) for i in range(1)])
                st6 = sb(st, "st6", [128, 6], F32)
                mv = sb(st, "mv", [128, 2], F32)
                rs = sb(st, "rs", [128, 1], F32)
                vn = sb(st, "vn", [128, NSB, TT], BF16)
                t1 = sb(st, "t1", [128, WM], F32)
                yab = sb(st, "yab", [128, 8, WM], BF16)

                class _V:
                    def __init__(self, lo, name):
                        self.lo = lo
                        self.b = p.buf(name)

                    def __getitem__(self, k):
                        a, c, d_ = k
                        return yab[a, self.lo + c, d_]
                ya = _V(0, "ya")
                yb = _V(4, "yb")
                sg = Ring([sb(st, "sg%d" % i, [128, WM], F32) for i in range(2)])
                yc = sb(st, "yc", [128, 4, WM], F32)
                tmp = Ring([sb(st, "tmp%d" % i, [128, WM], F32) for i in range(2)])
                _rb = sb(st, "lnr_rb", [128, 8, WM], BF16)
                _sd = sb(st, "lnr_sd", [128, WM], F32)
                lnr = {"rb": _rb, "sd": _sd, "mu": t1, "sqf": (lambda c: yab[:, c, :]), "sqb": [ya.b, yb.b]}
                lnc = {"rb": _rb, "sd": _sd, "mu": t1, "sqf": (lambda c: yab[:, 4 + c, :]), "sqb": [yb.b]}

                def run():
                    for t in range(SEQ // WM):
                        tok0 = s * SEQ + t * WM
                        load_x(src, tok0, xt, WM)
                        modulate(xt, hT, l, 0, 1, s)
                        for fo in range(4):
                            ps = PA.next()
                            for kc in range(8):
                                MM(ps[:, 0:WM], win[:, kc, fo * 128:(fo + 1) * 128], hT[:, kc, :], kc == 0, kc == 7, [win.b, hT.b], ps.b)
                            p.op("act", (lambda e, o=uT[:, fo, :], i=ps[:, 0:WM]: e.activation(out=o, in_=i, func=AF.Gelu)),
                                 reads=[ps.b], writes=[uT.b] if fo == 0 else [], joins=[] if fo == 0 else [uT.b])
                        for sub in range(NSB):
                            ps = PA.next()
                            for kc in range(8):
                                MM(ps[:], hT[:, kc, sub * 128:(sub + 1) * 128], win[:, kc, 512:1024], kc == 0, kc == 7, [win.b, hT.b], ps.b)
                            v = vg.next()
                            ACT(v[:], ps[:], AF.Gelu, [ps.b], [v.b])
                            p.op("dve", (lambda e, v=v: e.bn_stats(out=st6[:], in_=v[:])), reads=[v.b], writes=[st6.b])
                            p.op("dve", lambda e: e.bn_aggr(out=mv[:], in_=st6[:]), reads=[st6.b], writes=[mv.b])
                            ACT(rs[:], mv[:, 1:2], AF.Sqrt, [mv.b, epst.b], [rs.b], bias=epst[:, 0:1])
                            RECIP(rs[:], rs[:], [rs.b], [rs.b])
                            p.op("dve", (lambda e, v=v, sub=sub: e.tensor_scalar(out=vn[:, sub, :], in0=v[:], scalar1=mv[:, 0:1], scalar2=rs[:, 0:1],
                                                                                  op0=ALU.subtract, op1=ALU.mult)),
                                 reads=[v.b, mv.b, rs.b], writes=[vn.b] if sub == 0 else [], joins=[] if sub == 0 else [vn.b])
                        for g in range(4):
                            ps = PA.next()
                            for sub in range(NSB):
                                p.op("pe", (lambda e, ps=ps, g=g, sub=sub: e.matmul(ps[:, sub * 128:(sub + 1) * 128], vn[:, sub, g * 128:(g + 1) * 128],
                                                                                     wsTm[:, g, :], start=True, stop=True)),
                                     reads=[vn.b, wsTm.b], writes=[ps.b] if sub == 0 else [], joins=[] if sub == 0 else [ps.b])
                            STT(t1[:].rearrange("p (c q) -> p c q", c=NSB), ps[:, 0:WM].rearrange("p (c q) -> p c q", c=NSB), sgp[:, 0, g:g + 1],
                                C4[:, g:g + 1, :].to_broadcast([128, NSB, 128]), ALU.mult, ALU.add, [ps.b, sgp.b, C4.b], [t1.b])
                            p.op("dve", (lambda e, g=g: e.tensor_tensor(out=ya[:, g, :], in0=t1[:], in1=uT[:, g, :], op=ALU.mult)),
                                 reads=[t1.b, uT.b], writes=[ya.b] if g == 0 else [], joins=[] if g == 0 else [ya.b])
                        for fo in range(4):
                            pa = PA.next()
                            for kc in range(8):
                                MM(pa[:, 0:WM], win[:, kc, 1024 + fo * 128:1024 + (fo + 1) * 128], hT[:, kc, :], kc == 0, kc == 7, [win.b, hT.b], pa.b)
                            pg = PA.next()
                            for kc in range(8):
                                MM(pg[:, 0:WM], win[:, kc, 1536 + fo * 128:1536 + (fo + 1) * 128], hT[:, kc, :], kc == 0, kc == 7, [win.b, hT.b], pg.b)
                            sgt = sg.next()
                            ACT(sgt[:], pg[:, 0:WM], AF.Sigmoid, [pg.b], [sgt.b])
                            p.op("dve", (lambda e, fo=fo, pa=pa, sgt=sgt, t=t: e.tensor_tensor(out=GL[:, fo, 30 + t * WM:30 + (t + 1) * WM], in0=pa[:, 0:WM], in1=sgt[:], op=ALU.mult)),
                                 reads=[pa.b, sgt.b], writes=[GLb[t]] if fo == 0 else [], joins=[] if fo == 0 else [GLb[t]])
                        glreads = [diag.b, GLb[t], GLh] + ([GLb[t - 1]] if t > 0 else [])
                        for c in range(4):
                            ps = PA.next()
                            for k in range(31):
                                MM(ps[:, 0:WM], diag[:, c, k, :], GL[:, c, t * WM + k:t * WM + k + WM], k == 0, k == 30, glreads, ps.b)
                            p.op("act", (lambda e, c=c, ps=ps: e.activation(out=yc[:, c, :], in_=ps[:, 0:WM], func=AF.Identity, bias=cvp[:, 0, c:c + 1])),
                                 reads=[ps.b, cvp.b], writes=[yc.b] if c == 0 else [], joins=[] if c == 0 else [yc.b])
                        ln_fm(yc, 4, on512, lnc, WM)
                        for c in range(4):
                            p.op("act", (lambda e, c=c: e.activation(out=yb[:, c, :], in_=yc[:, c, :], func=AF.Silu,
                                                                      bias=cvp[:, 2, c:c + 1], scale=cvp[:, 1, c:c + 1])),
                                 reads=[yc.b, cvp.b], writes=[yb.b] if c == 0 else [], joins=[] if c == 0 else [yb.b])
                        for dc in range(8):
                            ps = PA.next()
                            for kc in range(8):
                                src_y = ya if kc < 4 else yb
                                MM(ps[:, 0:WM], wout[:, kc, dc * 128:(dc + 1) * 128], src_y[:, kc % 4, :], kc == 0, kc == 7, [wout.b, ya.b, yb.b], ps.b)
                            tm = tmp.next()
                            ACT(tm[:], ps[:, 0:WM], AF.Identity, [ps.b, cond.b], [tm.b], scale=cv(l, 2, dc, s))
                            p.op("dve", (lambda e, dc=dc, tm=tm: e.scalar_tensor_tensor(out=xt[:, dc, :], in0=xt[:, dc, :], scalar=ALPHA, in1=tm[:],
                                                                                  op0=ALU.mult, op1=ALU.add)),
                                 reads=[xt.b, tm.b] if dc == 0 else [tm.b], writes=[xt.b] if dc == 0 else [], joins=[] if dc == 0 else [xt.b])
                        ln_fm(xt, 8, on1024, lnr, WM)
                        ln_out_store(xt, 0, 1, l, dst, tok0, WM)
                return run
            p.run_threads([mkM(0), mkM(1)], head=HEAD_M)
            p.barrier()

    ST = 1024
    NTI = ST // 128

    def moe(src, dst, l):
        with ExitStack() as st:
            wr_sb = sb(st, "wr_sb", [128, 8, 20], F32)
            DMA("sp", wr_sb[:], wr_d[l, :, :, :], [], [wr_sb.b], wr_sb.b)
            brB = sb(st, "brB", [128, 20], F32)
            DMA("sp", brB[:], br_d[l, 0:1, :].to_broadcast([128, 20]), [], [brB.b], brB.b)
            hT = sb(st, "hTm", [128, 8, ST], BF16)
            yacc_t = sb(st, "yacc", [128, 2, 8, TT], F32).t
            yb_ = [[p.buf("yacc%d_%d" % (a, b)) for b in range(8)] for a in range(2)]
            wgs = Ring([sb(st, "wg%d" % i, [128, 8, 512], BF16) for i in range(2)])
            wus = Ring([sb(st, "wu%d" % i, [128, 8, 512], BF16) for i in range(2)])
            wds = Ring([sb(st, "wd%d" % i, [128, 4, 1024], BF16) for i in range(2)])
            xt = sb(st, "xtm", [128, 8, TT], F32)
            hf = sb(st, "hf", [128, 8, 128], F32)
            acts = Ring([sb(st, "act%d" % i, [128, 4, TT], BF16) for i in range(2)])
            sgs = Ring([sb(st, "sgm%d" % i, [128, TT], F32) for i in range(2)])
            t1s = Ring([sb(st, "t1m%d" % i, [128, TT], F32) for i in range(2)])
            lnr = {"rb": sb(st, "lnm_rb", [128, 8, TT], BF16), "sq": sb(st, "lnm_sq", [128, 8, TT], BF16), "sd": sb(st, "lnm_sd", [128, TT], F32), "mu": sb(st, "lnm_mu", [128, TT], F32)}
            L = sb(st, "Lrt", [128, NTI, 20], F32)
            cwT = sb(st, "cwT", [16, ST], F32)

            def rt(name, shape):
                return sb(st, "rt_" + name, shape, F32)
            gmax = rt("gmax", [128, NTI]); gsel = rt("gsel", [128, NTI, 4]); gd = rt("gd", [128, NTI, 4])
            gsum = rt("gsum", [128, NTI]); gw = rt("gw", [128, NTI]); tmp4 = rt("tmp4", [128, NTI, 4, 4])
            ig = rt("ig", [128, NTI, 4]); m1 = rt("m1", [128, NTI]); oh1 = rt("oh1", [128, NTI, 4])
            ig2 = rt("ig2", [128, NTI, 4]); m2 = rt("m2", [128, NTI]); oh2 = rt("oh2", [128, NTI, 4])
            dd = rt("dd", [128, NTI]); w1 = rt("w1", [128, NTI]); w2 = rt("w2", [128, NTI])
            a1 = rt("a1", [128, NTI, 4]); a2 = rt("a2", [128, NTI, 4]); cw = rt("cw", [128, NTI, 4, 4])

            def bc3(t):
                return t[:].unsqueeze(2).to_broadcast([128, NTI, 4])

            for sti in range(TOK // ST):
                s = (sti * ST) // SEQ
                base = sti * ST
                for tt in range(ST // TT):
                    load_x(src, base + tt * TT, xt)
                    for dc in range(8):
                        p.op("act", (lambda e, o=hT[:, dc, tt * TT:(tt + 1) * TT], i=xt[:, dc, :], b=cv(l, 3, dc, s), sc=cv(l, 4, dc, s):
                                     e.activation(out=o, in_=i, func=AF.Identity, bias=b, scale=sc)),
                             reads=[xt.b, cond.b], writes=[hT.b] if (dc == 0 and tt == 0) else [],
                             joins=[] if (dc == 0 and tt == 0) else [hT.b])
                    for sub in range(4):
                        for dc in range(8):
                            p.op("dve", (lambda e, o=hf[:, dc, :], i=xt[:, dc, sub * 128:(sub + 1) * 128], b=cv(l, 3, dc, s), sc=cv(l, 4, dc, s):
                                         e.tensor_scalar(out=o, in0=i, scalar1=sc, scalar2=b, op0=ALU.mult, op1=ALU.add)),
                                 reads=[xt.b, cond.b], writes=[hf.b] if dc == 0 else [], joins=[] if dc == 0 else [hf.b])
                        lps = PC.next()
                        for kc in range(8):
                            MM(lps[:, 0:20], hf[:, kc, :], wr_sb[:, kc, :], kc == 0, kc == 7, [hf.b, wr_sb.b], lps.b)
                        i16 = tt * 4 + sub
                        p.op("dve", (lambda e, o=L[:, i16, :], a=lps[:, 0:20]: e.tensor_tensor(out=o, in0=a, in1=brB[:], op=ALU.add)),
                             reads=[lps.b, brB.b], writes=[L.b] if i16 == 0 else [], joins=[] if i16 == 0 else [L.b])
                Lg = L[:, :, 0:4]
                Le = L[:, :, 4:20].rearrange("p t (g j) -> p t g j", g=4)
                RED(gmax[:], Lg, ALU.max, [L.b], [gmax.b])
                TTo("dve", gsel[:], Lg, bc3(gmax), ALU.is_equal, [L.b, gmax.b], [gsel.b])
                TTo("dve", gd[:], Lg, bc3(gmax), ALU.subtract, [L.b, gmax.b], [gd.b])
                ACT(gd[:], gd[:], AF.Exp, [gd.b], [gd.b])
                RED(gsum[:], gd[:], ALU.add, [gd.b], [gsum.b])
                RECIP(gw[:], gsum[:], [gsum.b], [gw.b])
                TTo("dve", tmp4[:], Le, gsel[:].unsqueeze(3).to_broadcast([128, NTI, 4, 4]), ALU.mult, [L.b, gsel.b], [tmp4.b])
                RED(ig[:], tmp4[:].rearrange("p t g j -> p t j g"), ALU.add, [tmp4.b], [ig.b])
                RED(m1[:], ig[:], ALU.max, [ig.b], [m1.b])
                TTo("dve", oh1[:], ig[:], bc3(m1), ALU.is_equal, [ig.b, m1.b], [oh1.b])
                STT(ig2[:], oh1[:], NEG, ig[:], ALU.mult, ALU.add, [oh1.b, ig.b], [ig2.b])
                RED(m2[:], ig2[:], ALU.max, [ig2.b], [m2.b])
                TTo("dve", oh2[:], ig2[:], bc3(m2), ALU.is_equal, [ig2.b, m2.b], [oh2.b])
                TTo("dve", dd[:], m2[:], m1[:], ALU.subtract, [m1.b, m2.b], [dd.b])
                ACT(dd[:], dd[:], AF.Exp, [dd.b], [dd.b])
                TS("dve", w1[:], dd[:], 1.0, None, ALU.add, None, [dd.b], [w1.b])
                RECIP(w1[:], w1[:], [w1.b], [w1.b])
                TTo("dve", w2[:], dd[:], w1[:], ALU.mult, [dd.b, w1.b], [w2.b])
                TTo("dve", w1[:], w1[:], gw[:], ALU.mult, [w1.b, gw.b], [w1.b])
                TTo("dve", w2[:], w2[:], gw[:], ALU.mult, [w2.b, gw.b], [w2.b])
                TTo("dve", a1[:], oh1[:], bc3(w1), ALU.mult, [oh1.b, w1.b], [a1.b])
                TTo("dve", a2[:], oh2[:], bc3(w2), ALU.mult, [oh2.b, w2.b], [a2.b])
                TTo("dve", a1[:], a1[:], a2[:], ALU.add, [a1.b, a2.b], [a1.b])
                TTo("dve", cw[:], gsel[:].unsqueeze(3).to_broadcast([128, NTI, 4, 4]),
                    a1[:].unsqueeze(2).to_broadcast([128, NTI, 4, 4]), ALU.mult, [gsel.b, a1.b], [cw.b])
                for i in range(NTI):
                    tp = PC.next()
                    p.op("pe", (lambda e, tp=tp, i=i: e.transpose(tp[0:16, 0:128], cw[:, i, :, :].rearrange("p g j -> p (g j)"), ident)),
                         reads=[cw.b, cst.b], writes=[tp.b])
                    p.op("act", (lambda e, tp=tp, i=i: e.activation(out=cwT[0:16, i * 128:(i + 1) * 128], in_=tp[0:16, 0:128], func=AF.Identity)),
                         reads=[tp.b], writes=[cwT.b] if i == 0 else [], joins=[] if i == 0 else [cwT.b])
                for ex in range(16):
                    wg, wu, wd = wgs.next(), wus.next(), wds.next()
                    DMA("pool", wg[:], w_gate[l, ex], [], [wg.b], wg.b)
                    DMA("pool", wu[:], w_up[l, ex], [], [wu.b], wu.b)
                    DMA("pool", wd[:], w_down[l, ex], [], [wd.b], wd.b)
                    for tt in range(ST // TT):
                        cps = PC.next()
                        MM(cps[:], sel[:, ex * 128:(ex + 1) * 128], cwT[0:16, tt * TT:(tt + 1) * TT], True, True, [cst.b, cwT.b], cps.b)
                        act = acts.next()
                        for fc in range(4):
                            gps = PA.next()
                            for kc in range(8):
                                MM(gps[:], wg[:, kc, fc * 128:(fc + 1) * 128], hT[:, kc, tt * TT:(tt + 1) * TT], kc == 0, kc == 7, [wg.b, hT.b], gps.b)
                            ups = PA.next()
                            for kc in range(8):
                                MM(ups[:], wu[:, kc, fc * 128:(fc + 1) * 128], hT[:, kc, tt * TT:(tt + 1) * TT], kc == 0, kc == 7, [wu.b, hT.b], ups.b)
                            sg = sgs.next()
                            t1 = t1s.next()
                            ACT(sg[:], gps[:], AF.Silu, [gps.b], [sg.b])
                            TTo("dve", t1[:], sg[:], ups[:], ALU.mult, [sg.b, ups.b], [t1.b])
                            p.op("dve", (lambda e, o=act[:, fc, :], a=t1[:], b=cps[:]: e.tensor_tensor(out=o, in0=a, in1=b, op=ALU.mult)),
                                 reads=[t1.b, cps.b], writes=[act.b] if fc == 0 else [], joins=[] if fc == 0 else [act.b])
                        for dc in range(8):
                            dps = PB.next()
                            for fc in range(4):
                                MM(dps[:], wd[:, fc, dc * 128:(dc + 1) * 128], act[:, fc, :], fc == 0, fc == 3, [wd.b, act.b], dps.b)
                            if ex == 0:
                                p.op("act", (lambda e, o=yacc_t[:, tt, dc, :], i=dps[:]: e.activation(out=o, in_=i, func=AF.Identity)),
                                     reads=[dps.b], writes=[yb_[tt][dc]])
                            else:
                                p.op("dve", (lambda e, o=yacc_t[:, tt, dc, :], i=dps[:]: e.tensor_tensor(out=o, in0=o, in1=i, op=ALU.add)),
                                     reads=[dps.b], writes=[yb_[tt][dc]])
                for tt in range(ST // TT):
                    tok0 = base + tt * TT
                    load_x(src, tok0, xt)
                    for dc in range(8):
                        tm = t1s.next()
                        ACT(tm[:], yacc_t[:, tt, dc, :], AF.Identity, [yb_[tt][dc], cond.b], [tm.b], scale=cv(l, 5, dc, s))
                        p.op("dve", (lambda e, dc=dc, tm=tm: e.scalar_tensor_tensor(out=xt[:, dc, :], in0=xt[:, dc, :], scalar=ALPHA, in1=tm[:],
                                                                              op0=ALU.mult, op1=ALU.add)),
                             reads=[xt.b, tm.b] if dc == 0 else [tm.b], writes=[xt.b] if dc == 0 else [], joins=[] if dc == 0 else [xt.b])
                    ln_fm(xt, 8, on1024, lnr)
                    ln_out_store(xt, 2, 3, l, dst, tok0)
            p.barrier()

    T_S = 512
    NTL = 31
    S_ROWS = NTL * T_S
    Hs = nc.dram_tensor("Hs", [S_ROWS, D], BF16, kind="Internal").ap()
    Ys = nc.dram_tensor("Ys", [S_ROWS, D], F32, kind="Internal").ap()

    dynreg = {}

    def moe_sparse(src, dst, l, zero_fill):
        NI = TOK // 128
        with ExitStack() as st:
            slots_i = sb(st, "slots_i", [128, NI, 2], I32)
            wgt = sb(st, "wgt", [128, NI, 2], F32)
            te_i = sb(st, "te_i", [128, 32], I32)
            widx = sb(st, "widx", [128, 32, 2], I32)
            HsB = p.buf("HsB")
            ZFB = p.buf("ZFB")
            with ExitStack() as sa:
                wr_sb = sb(sa, "wr_sb", [128, 8, 20], F32)
                DMA("sp", wr_sb[:], wr_d[l, :, :, :], [], [wr_sb.b], wr_sb.b)
                brB = sb(sa, "brB", [128, 20], F32)
                DMA("sp", brB[:], br_d[l, 0:1, :].to_broadcast([128, 20]), [], [brB.b], brB.b)
                htok = sb(sa, "htok", [128, NI, D], BF16)
                L = sb(sa, "LA", [128, NI, 20], F32)
                if zero_fill:
                    zt = sb(sa, "zt", [128, 4, D], BF16)
                    MSET("pool", zt[:].rearrange("p a n -> p (a n)"), 0.0, [zt.b])
                    for a in range(S_ROWS // 512):
                        p.op("sp", (lambda e, a=a: e.dma_start(out=Hs[a * 512:(a + 1) * 512, :].rearrange("(a p) n -> p a n", p=128), in_=zt[:])),
                             reads=[zt.b], joins=[ZFB], dma=ZFB)
                def mkA(tid, nth):
                    xt = sb(sa, "xtA", [128, 8, TT], F32)
                    hT = sb(sa, "hTA", [128, 8, TT], BF16)
                    hf = sb(sa, "hfA", [128, 8, 128], F32)

                    def run():
                        for t8 in range(tid, TOK // TT, nth):
                            s = (t8 * TT) // SEQ
                            load_x(src, t8 * TT, xt)
                            for dc in range(8):
                                p.op("act", (lambda e, o=hT[:, dc, :], i=xt[:, dc, :], b=cv(l, 3, dc, s), sc=cv(l, 4, dc, s):
                                             e.activation(out=o, in_=i, func=AF.Identity, bias=b, scale=sc)),
                                     reads=[xt.b, cond.b], writes=[hT.b] if dc == 0 else [], joins=[] if dc == 0 else [hT.b])
                            for sub in range(4):
                                i = t8 * 4 + sub
                                for dc in range(8):
                                    p.op("dve", (lambda e, o=hf[:, dc, :], i_=xt[:, dc, sub * 128:(sub + 1) * 128], b=cv(l, 3, dc, s), sc=cv(l, 4, dc, s):
                                                 e.tensor_scalar(out=o, in0=i_, scalar1=sc, scalar2=b, op0=ALU.mult, op1=ALU.add)),
                                         reads=[xt.b, cond.b], writes=[hf.b] if dc == 0 else [], joins=[] if dc == 0 else [hf.b])
                                lps = PC.next()
                                for kc in range(8):
                                    MM(lps[:, 0:20], hf[:, kc, :], wr_sb[:, kc, :], kc == 0, kc == 7, [hf.b, wr_sb.b], lps.b)
                                p.op("dve", (lambda e, o=L[:, i, :], a=lps[:, 0:20]: e.tensor_tensor(out=o, in0=a, in1=brB[:], op=ALU.add)),
                                     reads=[lps.b, brB.b], joins=[L.b])
                                tp = PA.next()
                                tpb = tp.t[:].bitcast(BF16)
                                for kc in range(8):
                                    p.op("pe", (lambda e, o=tpb[:, kc * 128:(kc + 1) * 128], a=hT[:, kc, sub * 128:(sub + 1) * 128]: e.transpose(o, a, identb[:])),
                                         reads=[hT.b, identb.b], writes=[tp.b] if kc == 0 else [], joins=[] if kc == 0 else [tp.b])
                                p.op("act", (lambda e, o=htok[:, i, :], a=tpb[:, 0:1024]: e.activation(out=o, in_=a, func=AF.Identity)),
                                     reads=[tp.b], joins=[htok.b])

                    return run
                p.run_threads([mkA(0, NTHR), mkA(1, NTHR)] if NTHR == 2 else [mkA(0, 1)], head=HEAD_A)

                def rt(name, shape, dt=F32):
                    return sb(sa, "rs_" + name, shape, dt)
                gmax = rt("gmax", [128, NI]); gsel = rt("gsel", [128, NI, 4]); gd = rt("gd", [128, NI, 4])
                gsum = rt("gsum", [128, NI]); gw = rt("gw", [128, NI]); tmp4 = rt("tmp4", [128, NI, 4, 4])
                ig = rt("ig", [128, NI, 4]); m1_ = rt("m1", [128, NI]); oh1 = rt("oh1", [128, NI, 4])
                ig2 = rt("ig2", [128, NI, 4]); m2_ = rt("m2", [128, NI]); oh2 = rt("oh2", [128, NI, 4])
                dd = rt("dd", [128, NI]); w1 = rt("w1", [128, NI]); w2 = rt("w2", [128, NI])
                M1 = rt("M1", [128, NI, 4, 4]); M2 = rt("M2", [128, NI, 4, 4]); Mm = rt("Mm", [128, NI, 16])
                mb = rt("mb", [128, NI, 16], BF16); Mp = rt("Mp", [128, NI + 1, 16], BF16)
                rank = rt("rank", [128, NI, 16]); cnt = rt("cnt", [128, 16]); cmp8 = rt("cmp8", [128, 16, 8])
                ncap = rt("ncap", [128, 16]); cap = rt("cap", [128, 16]); sa_ = rt("sa", [128, 16]); sb_ = rt("sb", [128, 16])
                start = rt("start", [128, 16]); pos = rt("pos", [128, NI, 16]); tmpp = rt("tmpp", [128, NI, 16])
                slots_f = rt("slots_f", [128, NI, 2]); cmpj = rt("cmpj", [128, 32, 16]); tef = rt("tef", [128, 32])

                def bc3(t):
                    return t[:].unsqueeze(2).to_broadcast([128, NI, 4])
                Lg = L[:, :, 0:4]
                Le = L[:, :, 4:20].rearrange("p t (g j) -> p t g j", g=4)
                RED(gmax[:], Lg, ALU.max, [L.b], [gmax.b])
                TTo("dve", gsel[:], Lg, bc3(gmax), ALU.is_equal, [L.b, gmax.b], [gsel.b])
                TTo("dve", gd[:], Lg, bc3(gmax), ALU.subtract, [L.b, gmax.b], [gd.b])
                ACT(gd[:], gd[:], AF.Exp, [gd.b], [gd.b])
                RED(gsum[:], gd[:], ALU.add, [gd.b], [gsum.b])
                RECIP(gw[:], gsum[:], [gsum.b], [gw.b])
                TTo("dve", tmp4[:], Le, gsel[:].unsqueeze(3).to_broadcast([128, NI, 4, 4]), ALU.mult, [L.b, gsel.b], [tmp4.b])
                RED(ig[:], tmp4[:].rearrange("p t g j -> p t j g"), ALU.add, [tmp4.b], [ig.b])
                RED(m1_[:], ig[:], ALU.max, [ig.b], [m1_.b])
                TTo("dve", oh1[:], ig[:], bc3(m1_), ALU.is_equal, [ig.b, m1_.b], [oh1.b])
                STT(ig2[:], oh1[:], NEG, ig[:], ALU.mult, ALU.add, [oh1.b, ig.b], [ig2.b])
                RED(m2_[:], ig2[:], ALU.max, [ig2.b], [m2_.b])
                TTo("dve", oh2[:], ig2[:], bc3(m2_), ALU.is_equal, [ig2.b, m2_.b], [oh2.b])
                TTo("dve", dd[:], m2_[:], m1_[:], ALU.subtract, [m1_.b, m2_.b], [dd.b])
                ACT(dd[:], dd[:], AF.Exp, [dd.b], [dd.b])
                TS("dve", w1[:], dd[:], 1.0, None, ALU.add, None, [dd.b], [w1.b])
                RECIP(w1[:], w1[:], [w1.b], [w1.b])
                TTo("dve", w2[:], dd[:], w1[:], ALU.mult, [dd.b, w1.b], [w2.b])
                TTo("dve", wgt[:, :, 0], w1[:], gw[:], ALU.mult, [w1.b, gw.b], [wgt.b])
                p.op("dve", lambda e: e.tensor_tensor(out=wgt[:, :, 1], in0=w2[:], in1=gw[:], op=ALU.mult), reads=[w2.b, gw.b], joins=[wgt.b])
                TTo("dve", M1[:], gsel[:].unsqueeze(3).to_broadcast([128, NI, 4, 4]), oh1[:].unsqueeze(2).to_broadcast([128, NI, 4, 4]), ALU.mult,
                    [gsel.b, oh1.b], [M1.b])
                TTo("dve", M2[:], gsel[:].unsqueeze(3).to_broadcast([128, NI, 4, 4]), oh2[:].unsqueeze(2).to_broadcast([128, NI, 4, 4]), ALU.mult,
                    [gsel.b, oh2.b], [M2.b])
                M1v = M1[:].rearrange("p t g j -> p t (g j)")
                M2v = M2[:].rearrange("p t g j -> p t (g j)")
                TTo("dve", Mm[:], M1v, M2v, ALU.add, [M1.b, M2.b], [Mm.b])
                CP("dve", mb[:], Mm[:], [Mm.b], [mb.b])
                MSET("dve", Mp[:, 0, :], 0.0, [Mp.b])
                for i in range(NI):
                    p.op("dve", (lambda e, i=i: e.tensor_tensor(out=Mp[:, i + 1, :], in0=Mp[:, i, :], in1=mb[:, i, :], op=ALU.add)),
                         reads=[mb.b], writes=[Mp.b])
                rps = PB.next()
                for i in range(NI):
                    p.op("pe", (lambda e, i=i: e.matmul(rps[:, i * 16:(i + 1) * 16], ustrb[:], mb[:, i, :], start=True, stop=False)),
                         reads=[ustrb.b, mb.b], writes=[rps.b] if i == 0 else [], joins=[] if i == 0 else [rps.b])
                    p.op("pe", (lambda e, i=i: e.matmul(rps[:, i * 16:(i + 1) * 16], onesb[:], Mp[:, i, :], start=False, stop=True)),
                         reads=[onesb.b, Mp.b], joins=[rps.b])
                ACT(rank[:].rearrange("p t e -> p (t e)"), rps[:], AF.Identity, [rps.b], [rank.b])
                cps_ = PC.next()
                MM(cps_[:, 0:16], onesb[:], Mp[:, NI, :], True, True, [onesb.b, Mp.b], cps_.b)
                ACT(cnt[:], cps_[:, 0:16], AF.Identity, [cps_.b], [cnt.b])
                TTo("dve", cmp8[:], cnt[:].unsqueeze(2).to_broadcast([128, 16, 8]), thr8.unsqueeze(1).to_broadcast([128, 16, 8]), ALU.is_gt,
                    [cnt.b, cst.b], [cmp8.b])
                RED(ncap[:], cmp8[:], ALU.add, [cmp8.b], [ncap.b])
                TS("dve", cap[:], ncap[:], float(T_S), None, ALU.mult, None, [ncap.b], [cap.b])
                srcb, dstb = cap, sa_
                for sh in (1, 2, 4, 8):
                    p.op("dve", (lambda e, a=srcb, d_=dstb, sh=sh: e.tensor_copy(out=d_[:, 0:sh], in_=a[:, 0:sh])), reads=[srcb.b], writes=[dstb.b])
                    p.op("dve", (lambda e, a=srcb, d_=dstb, sh=sh: e.tensor_tensor(out=d_[:, sh:16], in0=a[:, sh:16], in1=a[:, 0:16 - sh], op=ALU.add)),
                         reads=[srcb.b], joins=[dstb.b])
                    srcb, dstb = dstb, (sb_ if dstb is sa_ else sa_)
                incl = srcb
                TTo("dve", start[:], incl[:], cap[:], ALU.subtract, [incl.b, cap.b], [start.b])
                TTo("dve", pos[:], rank[:], start[:].unsqueeze(1).to_broadcast([128, NI, 16]), ALU.add, [rank.b, start.b], [pos.b])
                TTo("dve", tmpp[:], M1v, pos[:], ALU.mult, [M1.b, pos.b], [tmpp.b])
                RED(slots_f[:, :, 0], tmpp[:], ALU.add, [tmpp.b], [slots_f.b])
                TTo("dve", tmpp[:], M2v, pos[:], ALU.mult, [M2.b, pos.b], [tmpp.b])
                p.op("dve", lambda e: e.tensor_reduce(out=slots_f[:, :, 1], in_=tmpp[:], axis=AX.X, op=ALU.add), reads=[tmpp.b], joins=[slots_f.b])
                CP("dve", slots_i[:], slots_f[:], [slots_f.b], [slots_i.b])
                TTo("dve", cmpj[:], incl[:].unsqueeze(1).to_broadcast([128, 32, 16]), jt32.unsqueeze(2).to_broadcast([128, 32, 16]), ALU.is_le,
                    [incl.b, cst.b], [cmpj.b])
                RED(tef[:], cmpj[:], ALU.add, [cmpj.b], [tef.b])
                TS("dve", tef[:], tef[:], 15.0, None, ALU.min, None, [tef.b], [tef.b])
                CP("dve", te_i[:], tef[:], [tef.b], [te_i.b])
                TS("dve", tef[:], tef[:], float(l * 16), 128.0, ALU.add, ALU.mult, [tef.b], [tef.b])
                TS("dve", tef[:], tef[:], pcol, 2.0, ALU.add, ALU.mult, [tef.b, cst.b], [tef.b])
                CP("dve", widx[:, :, 0], tef[:], [tef.b], [widx.b])
                TS("dve", tef[:], tef[:], 1.0, None, ALU.add, None, [tef.b], [tef.b])
                p.op("dve", lambda e: e.tensor_copy(out=widx[:, :, 1], in_=tef[:]), reads=[tef.b], joins=[widx.b])
                for i in range(NI):
                    for k in range(2):
                        p.op("pool", (lambda e, i=i, k=k: e.indirect_dma_start(
                            out=Hs[:, :], out_offset=bass.IndirectOffsetOnAxis(ap=slots_i[:, i, k:k + 1].bitcast(U32), axis=0),
                            in_=htok[:, i, :], in_offset=None)),
                            reads=[htok.b, slots_i.b, ZFB], joins=[HsB], dma=HsB)
                if debug:
                    d1 = nc.dram_tensor("dbg_slots", [128, NI, 2], I32, kind="ExternalOutput").ap()
                    DMA("sp", d1[:, :, :], slots_i[:], [slots_i.b], [], slots_i.b)
                    d2 = nc.dram_tensor("dbg_wgt", [128, NI, 2], F32, kind="ExternalOutput").ap()
                    DMA("sp", d2[:, :, :], wgt[:], [wgt.b], [], wgt.b)
                    d3 = nc.dram_tensor("dbg_te", [128, 32], I32, kind="ExternalOutput").ap()
                    DMA("sp", d3[:, :], te_i[:], [te_i.b], [], te_i.b)
                    d4 = nc.dram_tensor("dbg_mm", [128, NI, 16], F32, kind="ExternalOutput").ap()
                    DMA("sp", d4[:, :, :], Mm[:], [Mm.b], [], Mm.b)
                    d5 = nc.dram_tensor("dbg_rank", [128, NI, 16], F32, kind="ExternalOutput").ap()
                    DMA("sp", d5[:, :, :], rank[:], [rank.b], [], rank.b)
                    d6 = nc.dram_tensor("dbg_start", [128, 16], F32, kind="ExternalOutput").ap()
                    DMA("sp", d6[:, :], start[:], [start.b], [], start.b)
                p.barrier()
            if "B" not in MOE_DEBUG:
                return
            with ExitStack() as sbk:
                def mkB(tid, nth):
                    nb = 2 if nth == 1 else 1
                    wgs = Ring([sb(sbk, "swg%d" % i, [128, 8, 512], BF16) for i in range(2)])
                    wus = Ring([sb(sbk, "swu%d" % i, [128, 8, 512], BF16) for i in range(2)])
                    wds = Ring([sb(sbk, "swd%d" % i, [128, 4, 1024], BF16) for i in range(2)])
                    hrows = Ring([sb(sbk, "hrow%d" % i, [128, D], BF16) for i in range(4)])
                    hsTs = Ring([sb(sbk, "hsT%d" % i, [128, 8, T_S], BF16) for i in range(nb)])
                    acts = Ring([sb(sbk, "sact%d" % i, [128, 4, T_S], BF16) for i in range(nb)])
                    sgs = Ring([sb(sbk, "ssg%d" % i, [128, T_S], F32) for i in range(2)])
                    yrows = Ring([sb(sbk, "yrow%d" % i, [128, 512], F32) for i in range(4)])

                    def run():
                        for j in range(tid, NTL, nth):
                            wg, wu, wd = wgs.next(), wus.next(), wds.next()

                            for (dst_t, src5) in ((wg, w_gate), (wu, w_up), (wd, w_down)):
                                for hh in range(2):
                                    p.op("pool", (lambda e, dst_t=dst_t, src5=src5, j=j, hh=hh: e.indirect_dma_start(
                                        out=dst_t[:].rearrange("p (h k) n -> p h (k n)", h=2)[:, hh, :], out_offset=None,
                                        in_=src5.rearrange("l e p (h k) n -> (l e p h) (k n)", h=2),
                                        in_offset=bass.IndirectOffsetOnAxis(ap=widx[:, j, hh:hh + 1].bitcast(U32), axis=0))),
                                        reads=[widx.b], writes=[dst_t.b] if hh == 0 else [], joins=[] if hh == 0 else [dst_t.b], dma=dst_t.b)
                            hsT = hsTs.next()
                            for sub in range(4):
                                hr = hrows.next()
                                r0 = j * T_S + sub * 128
                                DMA("sp", hr[:], Hs[r0:r0 + 128, :], [HsB], [hr.b], hr.b)
                                tp = PA.next()
                                tpb = tp.t[:].bitcast(BF16)
                                for kc in range(8):
                                    p.op("pe", (lambda e, o=tpb[:, kc * 128:(kc + 1) * 128], a=hr[:, kc * 128:(kc + 1) * 128]: e.transpose(o, a, identb[:])),
                                         reads=[hr.b, identb.b], writes=[tp.b] if kc == 0 else [], joins=[] if kc == 0 else [tp.b])
                                eng = "act" if sub % 2 == 0 else "dve"
                                o_ = hsT[:, :, sub * 128:(sub + 1) * 128]
                                i_ = tpb[:, 0:1024].rearrange("p (k t) -> p k t", k=8)
                                if eng == "act":
                                    p.op("act", (lambda e, o_=o_, i_=i_: e.activation(out=o_, in_=i_, func=AF.Identity)),
                                         reads=[tp.b], writes=[hsT.b] if sub == 0 else [], joins=[] if sub == 0 else [hsT.b])
                                else:
                                    p.op("dve", (lambda e, o_=o_, i_=i_: e.tensor_copy(out=o_, in_=i_)),
                                         reads=[tp.b], writes=[hsT.b] if sub == 0 else [], joins=[] if sub == 0 else [hsT.b])
                            act = acts.next()
                            for fc in range(4):
                                gps = PA.next()
                                for kc in range(8):
                                    MM(gps[:], wg[:, kc, fc * 128:(fc + 1) * 128], hsT[:, kc, :], kc == 0, kc == 7, [wg.b, hsT.b], gps.b)
                                ups = PA.next()
                                for kc in range(8):
                                    MM(ups[:], wu[:, kc, fc * 128:(fc + 1) * 128], hsT[:, kc, :], kc == 0, kc == 7, [wu.b, hsT.b], ups.b)
                                sg = sgs.next()
                                ACT(sg[:], gps[:], AF.Silu, [gps.b], [sg.b])
                                p.op("dve", (lambda e, o=act[:, fc, :], a=sg[:], b=ups[:]: e.tensor_tensor(out=o, in0=a, in1=b, op=ALU.mult)),
                                     reads=[sg.b, ups.b], writes=[act.b] if fc == 0 else [], joins=[] if fc == 0 else [act.b])
                            for sub in range(4):
                                for dh in range(2):
                                    dps = (PB if dh == 0 else PC).next()
                                    for fc in range(4):
                                        MM(dps[:], act[:, fc, sub * 128:(sub + 1) * 128], wd[:, fc, dh * 512:(dh + 1) * 512], fc == 0, fc == 3, [wd.b, act.b], dps.b)
                                    yr = yrows.next()
                                    if dh == 0:
                                        ACT(yr[:], dps[:], AF.Identity, [dps.b], [yr.b])
                                    else:
                                        CP("dve", yr[:], dps[:], [dps.b], [yr.b])
                                    r0 = j * T_S + sub * 128
                                    DMA("pool", Ys[r0:r0 + 128, dh * 512:(dh + 1) * 512], yr[:], [yr.b], [], yr.b)
                    return run
                p.run_threads([mkB(0, NTHR), mkB(1, NTHR)] if NTHR == 2 else [mkB(0, 1)], head=HEAD_B)
                p.barrier()
            if "C" not in MOE_DEBUG:
                return
            with ExitStack() as sc_:
                def mk_thread(tid, nth):
                    g1s = Ring([sb(sc_, "g1_%d" % i, [128, D], F32) for i in range(2)])
                    g2s = Ring([sb(sc_, "g2_%d" % i, [128, D], F32) for i in range(2)])
                    dgs = Ring([sb(sc_, "dg_%d" % i, [128, 128], F32) for i in range(4)])
                    yT = sb(sc_, "yTc", [128, 8, TT], F32)
                    xt = sb(sc_, "xtC", [128, 8, TT], F32)
                    tmpc = Ring([sb(sc_, "tmpC%d" % i, [128, TT], F32) for i in range(2)])
                    lnr = {"rb": sb(sc_, "lnC_rb", [128, 8, TT], BF16), "sq": sb(sc_, "lnC_sq", [128, 8, TT], BF16), "sd": sb(sc_, "lnC_sd", [128, TT], F32), "mu": sb(sc_, "lnC_mu", [128, TT], F32)}

                    def run():
                        for t8 in range(tid, TOK // TT, nth):
                            s = (t8 * TT) // SEQ
                            tok0 = t8 * TT
                            load_x(src, tok0, xt)
                            for sub in range(4):
                                i = t8 * 4 + sub
                                g1, g2 = g1s.next(), g2s.next()
                                p.op("pool", (lambda e, g1=g1, i=i: e.indirect_dma_start(
                                    out=g1[:], out_offset=None, in_=Ys[:, :],
                                    in_offset=bass.IndirectOffsetOnAxis(ap=slots_i[:, i, 0:1].bitcast(U32), axis=0))),
                                    reads=[slots_i.b], writes=[g1.b], dma=g1.b)
                                p.op("pool", (lambda e, g2=g2, i=i: e.indirect_dma_start(
                                    out=g2[:], out_offset=None, in_=Ys[:, :],
                                    in_offset=bass.IndirectOffsetOnAxis(ap=slots_i[:, i, 1:2].bitcast(U32), axis=0))),
                                    reads=[slots_i.b], writes=[g2.b], dma=g2.b)
                                d1, d2 = dgs.next(), dgs.next()
                                TS("dve", d1[:], ident, wgt[:, i, 0:1], None, ALU.mult, None, [cst.b, wgt.b], [d1.b])
                                TS("dve", d2[:], ident, wgt[:, i, 1:2], None, ALU.mult, None, [cst.b, wgt.b], [d2.b])
                                for half in range(2):
                                    tp = PA.next()
                                    for q in range(4):
                                        dc = half * 4 + q
                                        p.op("pe", (lambda e, o=tp[:, q * 128:(q + 1) * 128], a=g1[:, dc * 128:(dc + 1) * 128], d1=d1: e.matmul(o, a, d1[:], start=True, stop=False)),
                                             reads=[g1.b, d1.b], writes=[tp.b] if q == 0 else [], joins=[] if q == 0 else [tp.b])
                                        p.op("pe", (lambda e, o=tp[:, q * 128:(q + 1) * 128], a=g2[:, dc * 128:(dc + 1) * 128], d2=d2: e.matmul(o, a, d2[:], start=False, stop=True)),
                                             reads=[g2.b, d2.b], joins=[tp.b])
                                    first = (sub == 0 and half == 0)
                                    p.op("act", (lambda e, o=yT[:, half * 4:(half + 1) * 4, sub * 128:(sub + 1) * 128], a=tp[:].rearrange("p (q t) -> p q t", q=4):
                                                 e.activation(out=o, in_=a, func=AF.Identity)),
                                         reads=[tp.b], writes=[yT.b] if first else [], joins=[] if first else [yT.b])
                            for dc in range(8):
                                tm = tmpc.next()
                                ACT(tm[:], yT[:, dc, :], AF.Identity, [yT.b, cond.b], [tm.b], scale=cv(l, 5, dc, s))
                                p.op("dve", (lambda e, dc=dc, tm=tm: e.scalar_tensor_tensor(out=xt[:, dc, :], in0=xt[:, dc, :], scalar=ALPHA, in1=tm[:],
                                                                                      op0=ALU.mult, op1=ALU.add)),
                                     reads=[xt.b, tm.b] if dc == 0 else [tm.b], writes=[xt.b] if dc == 0 else [], joins=[] if dc == 0 else [xt.b])
                            ln_fm(xt, 8, on1024, lnr)
                            ln_out_store(xt, 2, 3, l, dst, tok0)
                    return run
                p.run_threads([mk_thread(0, NTHR), mk_thread(1, NTHR)] if NTHR == 2 else [mk_thread(0, 1)], head=HEAD_C)
                p.barrier()

    SCALE = 128.0 ** -0.5

    def attn(src, dst, l):
        with ExitStack() as st:
            wout = sb(st, "wo_at", [128, 8, 1024], BF16)
            for j in range(2):
                p.op("pool", (lambda e, j=j: e.dma_start(out=wout[:, :, j * 512:(j + 1) * 512],
                                                          in_=od_w_out[:, j * 512:(j + 1) * 512].rearrange("(kc p) n -> p kc n", p=128))),
                     writes=[wout.b] if j == 0 else [], joins=[] if j == 0 else [wout.b], dma=wout.b)
            if "w" in ATTN_DEBUG:
                kT = sb(st, "kT", [128, 8, SEQ], BF16)
                qT = sb(st, "qT", [128, 8, SEQ], BF16)
            else:
                qT = sb(st, "qT", [128, 8, SEQ], BF16)
                kT = sb(st, "kT", [128, 8, SEQ], BF16)
            v1 = sb(st, "v1", [128, 16, 8, 132], BF16)
            oT = sb(st, "oT", [128, 8, SEQ], BF16)
            ksum = sb(st, "ksum", [128, 8, 8], F32)
            gate = sb(st, "gate", [128, 16, 8, 8], F32)
            selt = sb(st, "selt", [128, 16, 8, 8], F32)
            MSET("pool", v1[:].rearrange("p a b c -> p (a b c)"), 1.0, [v1.b])
            def do_seq(s):
                with ExitStack() as sa:
                    xt = sb(sa, "xta", [128, 8, TT], F32)
                    wr2 = Ring([sb(sa, "wqkv%d" % i, [128, 8, 512], BF16) for i in range(2)])
                    qf = Ring([sb(sa, "qf%d" % i, [128, TT], F32) for i in range(2)])
                    top8 = sb(sa, "top8", [128, 2, 8, 8], F32)
                    for t in range(4):
                        load_x(src, s * SEQ + t * TT, xt)
                        for dc in range(8):
                            first = (t == 0 and dc == 0)
                            p.op("act", (lambda e, o=oT[:, dc, t * TT:(t + 1) * TT], i=xt[:, dc, :], b=cv(l, 0, dc, s), sc=cv(l, 1, dc, s):
                                         e.activation(out=o, in_=i, func=AF.Identity, bias=b, scale=sc)),
                                 reads=[xt.b, cond.b], writes=[oT.b] if first else [], joins=[] if first else [oT.b])
                    for which, half in ((1, 0), (1, 1), (0, 0), (0, 1), (2, 0), (2, 1)):
                        if "kqv"[which] not in ATTN_DEBUG + "kqv" * ("A" not in ATTN_DEBUG or "x" not in ATTN_DEBUG):
                            continue
                        w = wr2.next()
                        c0 = which * 1024 + half * 512
                        if "m" in ATTN_DEBUG:
                            c0 = 1024 + half * 512
                        DMA("pool", w[:], od_w_qkv[:, c0:c0 + 512].rearrange("(kc p) n -> p kc n", p=128), [], [w.b], w.b)
                        for t in range(4):
                            if which in (0, 1):
                                for j in range(4):
                                    h = half * 4 + j
                                    ps = PA.next()
                                    for kc in range(8):
                                        MM(ps[:], w[:, kc, j * 128:(j + 1) * 128], oT[:, kc, t * TT:(t + 1) * TT], kc == 0, kc == 7, [w.b, oT.b], ps.b)
                                    if which == 1:
                                        p.op("act", (lambda e, o=kT[:, h, t * TT:(t + 1) * TT], i=ps[:]: e.activation(out=o, in_=i, func=AF.Identity)),
                                             reads=[ps.b], joins=[kT.b])
                                        p.op("dve", (lambda e, o=ksum[:, h, 2 * t:2 * t + 2], i=ps[:].rearrange("p (b k) -> p b k", b=2):
                                                     e.tensor_reduce(out=o, in_=i, axis=AX.X, op=ALU.add)),
                                             reads=[ps.b], joins=[ksum.b])
                                    else:
                                        q32 = qf.next()
                                        if "n" not in ATTN_DEBUG:
                                            ACT(q32[:], ps[:], AF.Identity, [ps.b], [q32.b])
                                        p.op("act", (lambda e, o=qT[:, h, t * TT:(t + 1) * TT], i=ps[:]: e.activation(out=o, in_=i, func=AF.Identity)),
                                             reads=[ps.b], joins=[qT.b])
                                        if "x" in ATTN_DEBUG and "g" not in ATTN_DEBUG:
                                            continue
                                        gp = PC.next()
                                        for sub in range(4):
                                            p.op("pe", (lambda e, gp=gp, sub=sub, q32=q32, h=h: e.matmul(gp[:, sub * 8:sub * 8 + 8], q32[:, sub * 128:(sub + 1) * 128],
                                                                                                   ksum[:, h, :], start=True, stop=True)),
                                                 reads=[q32.b, ksum.b], writes=[gp.b] if sub == 0 else [], joins=[] if sub == 0 else [gp.b])
                                        p.op("dve", (lambda e, gp=gp, h=h, t=t: e.tensor_copy(out=gate[:, t * 4:t * 4 + 4, h, :],
                                                                                         in_=gp[:, 0:32].rearrange("p (a n) -> p a n", a=4))),
                                             reads=[gp.b], joins=[gate.b])
                            else:
                                for sub in range(4):
                                    ps = PA.next()
                                    for kc in range(8):
                                        MM(ps[:], oT[:, kc, (t * 4 + sub) * 128:(t * 4 + sub + 1) * 128], w[:, kc, :], kc == 0, kc == 7, [w.b, oT.b], ps.b)
                                    eng = "act" if sub % 2 == 0 else "dve"
                                    if eng == "act":
                                        p.op("act", (lambda e, ps=ps, kt=t * 4 + sub, half=half: e.activation(out=v1[:, kt, half * 4:half * 4 + 4, 0:128],
                                                                                               in_=ps[:].rearrange("p (h d) -> p h d", h=4), func=AF.Identity)),
                                             reads=[ps.b], joins=[v1.b])
                                    else:
                                        p.op("dve", (lambda e, ps=ps, kt=t * 4 + sub, half=half: e.tensor_copy(out=v1[:, kt, half * 4:half * 4 + 4, 0:128],
                                                                                                in_=ps[:].rearrange("p (h d) -> p h d", h=4))),
                                             reads=[ps.b], joins=[v1.b])
                    for b in range(4, 8):
                        if "x" in ATTN_DEBUG and "s" not in ATTN_DEBUG:
                            continue
                        p.op("dve", (lambda e, b=b: e.memset(gate[:, 2 * b:2 * b + 2, :, b:8], NEG)), reads=[], joins=[gate.b]) if False else \
                            p.op("dve", (lambda e, b=b: e.memset(gate[:, 2 * b:2 * b + 2, :, b:8], NEG)), writes=[gate.b])
                        for qq in range(2):
                            for h in range(8):
                                first = (qq == 0 and h == 0)
                                p.op("dve", (lambda e, b=b, qq=qq, h=h: e.max(out=top8[:, qq, h, :], in_=gate[:, 2 * b + qq, h, :])),
                                     reads=[gate.b], writes=[top8.b] if first else [], joins=[] if first else [top8.b])
                        p.op("dve", (lambda e, b=b: e.tensor_tensor(out=selt[:, 2 * b:2 * b + 2, :, :], in0=gate[:, 2 * b:2 * b + 2, :, :],
                                                                     in1=top8[:, :, :, 2:3].to_broadcast([128, 2, 8, 8]), op=ALU.is_ge)),
                             reads=[gate.b, top8.b], writes=[selt.b])
                    p.barrier()
                if "B" not in ATTN_DEBUG:
                    return
                with ExitStack() as sbk:
                    def mkAt(tid, nth):
                        Es = Ring([sb(sbk, "E%d" % i, [128, 256], BF16) for i in range(6)])
                        accs = Ring([sb(sbk, "acc%d" % i, [128, 132], F32) for i in range(4)])
                        rden = Ring([sb(sbk, "rden%d" % i, [128, 1], F32) for i in range(4)])
                        o32 = Ring([sb(sbk, "o32_%d" % i, [128, 128], F32) for i in range(4)])

                        def run():
                            for h in range(tid, 8, nth):
                                for b in range(8):
                                    dense = b < 4
                                    order = [b] + list(range(b))
                                    accp = [accs.next(), accs.next()]
                                    ops_ = [None, None]
                                    for idx, n in enumerate(order):
                                        own = (n == b)
                                        newgrp = (idx == 0) or (not dense)
                                        lastgrp = (idx == len(order) - 1) or (not dense)
                                        if newgrp:
                                            ops_ = [PB.next(), PC.next()]
                                        Et = []
                                        for kt in range(2):
                                            sp_ = PA.next()
                                            MM(sp_[:, 0:256], kT[:, h, (2 * n + kt) * 128:(2 * n + kt + 1) * 128], qT[:, h, b * 256:(b + 1) * 256], True, True, [kT.b, qT.b], sp_.b)
                                            E = Es.next()
                                            ACT(E[:], sp_[:, 0:256], AF.Exp, [sp_.b], [E.b], scale=SCALE)
                                            if own:
                                                c0 = kt * 128
                                                p.op("dve", (lambda e, E=E, c0=c0: e.tensor_tensor(out=E[:, c0:c0 + 128], in0=E[:, c0:c0 + 128], in1=trib[:], op=ALU.mult)),
                                                     reads=[trib.b], writes=[E.b])
                                            Et.append(E)
                                        for qs in range(2):
                                            kts = [kt for kt in range(2) if not (own and kt == 1 and qs == 0)]
                                            for kt in kts:
                                                first = newgrp and (kt == kts[0])
                                                last = lastgrp and (kt == kts[-1])
                                                MM(ops_[qs][:, 0:129], Et[kt][:, qs * 128:(qs + 1) * 128], v1[:, 2 * n + kt, h, 0:129], first, last, [Et[kt].b, v1.b], ops_[qs].b)
                                            if lastgrp:
                                                acc = accp[qs]
                                                if dense or own:
                                                    ACT(acc[:, 0:129], ops_[qs][:, 0:129], AF.Identity, [ops_[qs].b], [acc.b])
                                                else:
                                                    STT(acc[:, 0:129], ops_[qs][:, 0:129], selt[:, 2 * b + qs, h, n:n + 1], acc[:, 0:129], ALU.mult, ALU.add,
                                                        [ops_[qs].b, selt.b], [acc.b])
                                    for qs in range(2):
                                        acc = accp[qs]
                                        rd = rden.next()
                                        RECIP(rd[:], acc[:, 128:129], [acc.b], [rd.b])
                                        o = o32.next()
                                        TS("dve", o[:], acc[:, 0:128], rd[:, 0:1], None, ALU.mult, None, [acc.b, rd.b], [o.b])
                                        tp = PA.next()
                                        p.op("pe", (lambda e, tp=tp, o=o: e.transpose(tp[:, 0:128], o[:], ident)), reads=[o.b, cst.b], writes=[tp.b])
                                        qt = 2 * b + qs
                                        p.op("act", (lambda e, tp=tp, h=h, qt=qt: e.activation(out=oT[:, h, qt * 128:(qt + 1) * 128], in_=tp[:, 0:128], func=AF.Identity)),
                                             reads=[tp.b], joins=[oT.b])
                        return run
                    p.run_threads([mkAt(0, NTHR), mkAt(1, NTHR)] if NTHR == 2 else [mkAt(0, 1)], head=HEAD_AT)
                    if debug and s == 0:
                        dbo = nc.dram_tensor("dbg_oT", [128, 8, SEQ], BF16, kind="ExternalOutput").ap()
                        DMA("sp", dbo[:, :, :], oT[:], [oT.b], [], oT.b)
                        dbq = nc.dram_tensor("dbg_qT", [128, 8, SEQ], BF16, kind="ExternalOutput").ap()
                        DMA("sp", dbq[:, :, :], qT[:], [qT.b], [], qT.b)
                        dbk = nc.dram_tensor("dbg_kT", [128, 8, SEQ], BF16, kind="ExternalOutput").ap()
                        DMA("sp", dbk[:, :, :], kT[:], [kT.b], [], kT.b)
                        dbv = nc.dram_tensor("dbg_v1", [128, 16, 8, 132], BF16, kind="ExternalOutput").ap()
                        DMA("sp", dbv[:, :, :, :], v1[:], [v1.b], [], v1.b)
                    p.barrier()
                if "C" not in ATTN_DEBUG:
                    return
                with ExitStack() as sc_:
                    xt = sb(sc_, "xtc", [128, 8, TT], F32)
                    tmp = Ring([sb(sc_, "tmpc%d" % i, [128, TT], F32) for i in range(2)])
                    lnr = {"rb": sb(sc_, "lnc_rb", [128, 8, TT], BF16), "sq": sb(sc_, "lnc_sq", [128, 8, TT], BF16), "sd": sb(sc_, "lnc_sd", [128, TT], F32), "mu": sb(sc_, "lnc_mu", [128, TT], F32)}
                    for t in range(4):
                        tok0 = s * SEQ + t * TT
                        load_x(src, tok0, xt)
                        for dc in range(8):
                            ps = PA.next()
                            for kc in range(8):
                                MM(ps[:], wout[:, kc, dc * 128:(dc + 1) * 128], oT[:, kc, t * TT:(t + 1) * TT], kc == 0, kc == 7, [wout.b, oT.b], ps.b)
                            tm = tmp.next()
                            ACT(tm[:], ps[:], AF.Identity, [ps.b, cond.b], [tm.b], scale=cv(l, 2, dc, s))
                            p.op("dve", (lambda e, dc=dc, tm=tm: e.scalar_tensor_tensor(out=xt[:, dc, :], in0=xt[:, dc, :], scalar=ALPHA, in1=tm[:],
                                                                                  op0=ALU.mult, op1=ALU.add)),
                                 reads=[xt.b, tm.b] if dc == 0 else [tm.b], writes=[xt.b] if dc == 0 else [], joins=[] if dc == 0 else [xt.b])
                        ln_fm(xt, 8, on1024, lnr)
                        ln_out_store(xt, 0, 1, l, dst, tok0)
                    p.barrier()

            for s_ in range(NSEQ):
                do_seq(s_)

    if only == "attn":
        attn(xT, yT, 1)
        stages = 0
    def moe_any(src, dst, l, zero_fill):
        if SPARSE:
            moe_sparse(src, dst, l, zero_fill)
        else:
            moe(src, dst, l)

    if only == "moe":
        moe_any(xT, yT, 0, True)
        stages = 0
    if stages >= 1:
        mixer0(xT, yT if stages == 1 else XA, 0)
    if stages >= 2:
        moe_any(XA, yT if stages == 2 else XB, 0, True)
    if stages >= 3:
        attn(XB, yT if stages == 3 else XA, 1)
    if stages >= 4:
        moe_any(XA, yT, 1, False)

    p.barrier()
    p.emit()
    return nc, es


def prep_inputs(inputs):
    f = lambda a: np.ascontiguousarray(np.asarray(a, dtype=np.float32))
    x = f(inputs["x"])
    c = f(inputs["c"])
    shared = {}
    shared["ada_w"] = f(inputs["ada_w"])
    shared["ada_bT"] = f(inputs["ada_b"].reshape(2, 48, 128).transpose(2, 0, 1))
    lnp = np.stack([inputs["ln_mix_g"], inputs["ln_mix_b"], inputs["ln_ffn_g"], inputs["ln_ffn_b"]], 1)
    shared["lnp"] = f(lnp.reshape(2, 4, 8, 128).transpose(3, 0, 1, 2))
    shared["ev_w_in"] = f(inputs["ev_w_in"][0])
    shared["ev_w_out"] = f(inputs["ev_w_out"][0])
    sg = np.stack([inputs["ev_sgu_ln_g"][0], inputs["ev_sgu_ln_b"][0]], 0)
    shared["sgp"] = f(sg.reshape(2, 4, 128).transpose(2, 0, 1))
    shared["wsT"] = f(inputs["ev_w_s"][0].transpose(2, 0, 1))
    shared["bs"] = f(inputs["ev_b_s"][0].reshape(1, 512))
    shared["wdw"] = f(inputs["ev_w_dw"][0].reshape(31, 4, 128).transpose(2, 1, 0))
    cv = np.stack([inputs["ev_b_dw"][0], inputs["ev_conv_ln_g"][0], inputs["ev_conv_ln_b"][0]], 0)
    shared["cvp"] = f(cv.reshape(3, 4, 128).transpose(2, 0, 1))
    shared["od_w_qkv"] = f(inputs["od_w_qkv"][0])
    shared["od_w_out"] = f(inputs["od_w_out"][0])
    wr = np.concatenate([inputs["moe_w_grp"], inputs["moe_w_er"].transpose(0, 2, 1, 3).reshape(2, D, 16)], -1)
    shared["wr"] = f(wr.reshape(2, 8, 128, 20).transpose(0, 2, 1, 3))
    shared["br"] = f(np.concatenate([inputs["moe_b_grp"], inputs["moe_b_er"].reshape(2, 16)], -1).reshape(2, 1, 20))
    shared["moe_w_gate"] = f(np.asarray(inputs["moe_w_gate"]).reshape(2, 16, 8, 128, 512).transpose(0, 1, 3, 2, 4))
    shared["moe_w_up"] = f(np.asarray(inputs["moe_w_up"]).reshape(2, 16, 8, 128, 512).transpose(0, 1, 3, 2, 4))
    shared["moe_w_down"] = f(np.asarray(inputs["moe_w_down"]).reshape(2, 16, 4, 128, D).transpose(0, 1, 3, 2, 4))
    cst = np.zeros((128, 2473), np.float32)
    cst[:, 2472] = np.arange(128, dtype=np.float32)
    cst[:, 2432:2440] = np.arange(8, dtype=np.float32)[None, :] * 512.0
    cst[:, 2440:2472] = np.arange(32, dtype=np.float32)[None, :] * 512.0
    cst[:, 0:128] = np.eye(128, dtype=np.float32)
    cst[:, 128:256] = np.triu(np.ones((128, 128), np.float32))
    cst[:, 256:384] = 1.0
    for e in range(16):
        cst[e, 384 + e * 128:384 + (e + 1) * 128] = 1.0
    shared["consts"] = cst
    maps = []
    for core in range(NCORES):
        m = dict(shared)
        xs = x[core * NSEQ:(core + 1) * NSEQ]
        m["xT"] = f(xs.reshape(TOK, 8, 128).transpose(1, 2, 0))
        m["cT"] = f(c[core * NSEQ:(core + 1) * NSEQ].reshape(NSEQ, 8, 128).transpose(2, 1, 0))
        maps.append(m)
    return maps


_CACHE = {}


def kernel(**inputs):
    maps = prep_inputs(inputs)
    if "nc" not in _CACHE:
        _CACHE["nc"] = build()
    nc, _ = _CACHE["nc"]
    res = run_bass_kernel_spmd(nc, maps, core_ids=list(range(NCORES)))
    out = np.empty((NCORES * NSEQ, SEQ, D), np.float32)
    for core in range(NCORES):
        yT = np.asarray(res.results[core]["yT"])
        out[core * NSEQ:(core + 1) * NSEQ] = yT.transpose(2, 0, 1).reshape(NSEQ, SEQ, D)
    return out
```

```python
import threading
import numpy as np
from contextlib import ExitStack
import concourse.bass as bass
import concourse.mybir as mybir
from concourse.bass_utils import run_bass_kernel_spmd

F32 = mybir.dt.float32
BF16 = mybir.dt.bfloat16
I32 = mybir.dt.int32
U32 = mybir.dt.uint32
AF = mybir.ActivationFunctionType
ALU = mybir.AluOpType
AX = mybir.AxisListType

NCORES = 8
D = 1024
SEQ = 2048
NSEQ = 2
TOK = NSEQ * SEQ
TT = 512
ALPHA = 4.0 ** 0.25
EPS = 1e-5
NEG = -1.0e30
ATTN_DEBUG = "ABC"
SPARSE = True
MOE_DEBUG = "ABC"
NTHR = 2
MIX_W = 512
NTHR_A = 2
NTHR_AT = 2
HEAD_M = 250
HEAD_A = 56
HEAD_B = 83
HEAD_C = 70
HEAD_AT = 300

ENGS = ("pe", "act", "dve", "pool", "sp")


class Buf:
    __slots__ = ("name", "writers", "readers", "dsem", "dcount", "excl", "dslot")

    def __init__(self, name):
        self.name = name
        self.writers = []
        self.readers = []
        self.dsem = None
        self.dcount = 0
        self.excl = False
        self.dslot = None


class Op:
    __slots__ = ("eng", "fn", "waits", "is_dma", "is_nop", "dbuf", "value", "needed", "eidx", "semval", "epoch")

    def __init__(self, eng, fn):
        self.eng = eng
        self.fn = fn
        self.waits = []
        self.is_dma = False
        self.is_nop = False
        self.dbuf = None
        self.value = 0
        self.needed = False
        self.eidx = 0
        self.semval = 0
        self.epoch = 0


class Turns:
    def __init__(self, n):
        self.n = n
        self.cur = 0
        self.alive = [True] * n
        self.cv = threading.Condition()
        self.local = threading.local()
        self.heads = [0] * n

    def _advance(self):
        for k in range(1, self.n + 1):
            c = (self.cur + k) % self.n
            if self.alive[c]:
                self.cur = c
                return

    def start(self, tid):
        self.local.tid = tid
        with self.cv:
            while self.cur != tid:
                self.cv.wait()

    def yield_turn(self):
        tid = self.local.tid
        if self.heads[tid] > 0:
            self.heads[tid] -= 1
            return
        with self.cv:
            self._advance()
            self.cv.notify_all()
            while self.cur != tid:
                self.cv.wait()

    def finish(self):
        tid = self.local.tid
        with self.cv:
            self.alive[tid] = False
            if any(self.alive):
                self._advance()
            self.cv.notify_all()


class Prog:
    def __init__(self, nc, es):
        self.nc = nc
        self.es = es
        self.ops = {e: [] for e in ENGS}
        self.clock = {e: {} for e in ENGS}
        self.esem = {}
        self.bufs = []
        self.same_engine_sync = True
        self.epoch = 0
        self.turns = None
        self.slots = []
        self.free_slots = []

    def buf(self, name):
        b = Buf(name)
        self.bufs.append(b)
        return b

    def _dep(self, op, d):
        E = op.eng
        if d.is_dma:
            key = ("d", id(d.dbuf))
            if self.clock[E].get(key, 0) >= d.value:
                return
            self.clock[E][key] = d.value
            op.waits.append(("d", d.dbuf, d.value))
        else:
            if d.is_nop:
                return
            if d.eng == E and (E == "pe" or not self.same_engine_sync):
                return
            key = ("e", d.eng)
            if self.clock[E].get(key, 0) >= d.eidx:
                return
            self.clock[E][key] = d.eidx
            d.needed = True
            op.waits.append(("e", d.eng, d))

    def op(self, eng, fn, reads=(), writes=(), joins=(), dma=None):
        o = Op(eng, fn)
        o.epoch = self.epoch
        lst = self.ops[eng]
        lst.append(o)
        o.eidx = len(lst)
        if dma is not None:
            o.is_dma = True
            if dma.dslot is None:
                if self.free_slots:
                    dma.dslot = self.free_slots.pop()
                else:
                    dma.dslot = Buf("slot%d" % len(self.slots))
                    self.slots.append(dma.dslot)
            sl = dma.dslot
            o.dbuf = sl
            sl.dcount += 16
            o.value = sl.dcount
        deps = []
        for b in reads:
            deps.extend(b.writers)
            if b.excl:
                deps.extend(r for r in b.readers if r.eng != eng)
        for b in writes:
            deps.extend(b.writers)
            deps.extend(b.readers)
        for b in joins:
            if b.readers:
                deps.extend(b.writers)
                deps.extend(b.readers)
        best = {}
        for d in deps:
            if d.is_dma:
                key = ("d", id(d.dbuf))
                v = d.value
            else:
                if d.is_nop:
                    continue
                key = ("e", d.eng)
                v = d.eidx
            if key not in best or best[key][0] < v:
                best[key] = (v, d)
        for key in best:
            self._dep(o, best[key][1])
        for b in reads:
            b.readers.append(o)
        for b in writes:
            b.writers = [o]
            b.readers = []
        for b in joins:
            if b.readers:
                b.writers = [o]
                b.readers = []
            else:
                b.writers.append(o)
        if self.turns is not None:
            self.turns.yield_turn()
        return o

    def run_threads(self, fns, head=0):
        if len(fns) == 1:
            fns[0]()
            return
        turns = Turns(len(fns))
        turns.heads = [head * (len(fns) - 1 - k) for k in range(len(fns))]
        self.turns = turns
        errs = []

        def wrap(tid, fn):
            turns.start(tid)
            try:
                fn()
            except BaseException as ex:
                errs.append(ex)
            finally:
                turns.finish()
        ths = [threading.Thread(target=wrap, args=(i, f)) for i, f in enumerate(fns)]
        for t in ths:
            t.start()
        for t in ths:
            t.join()
        self.turns = None
        if errs:
            raise errs[0]

    def barrier(self):
        lasts = {}
        for e in ENGS:
            for o in reversed(self.ops[e]):
                if not o.is_dma and not o.is_nop:
                    lasts[e] = o
                    break
        dm = [(b, b.dcount) for b in self.slots if b.dcount > 0]
        for e in ENGS:
            o = Op(e, lambda eng: eng.nop())
            o.is_nop = True
            self.ops[e].append(o)
            o.eidx = len(self.ops[e])
            for e2, l in lasts.items():
                if e2 == e:
                    continue
                key = ("e", e2)
                if self.clock[e].get(key, 0) >= l.eidx:
                    continue
                self.clock[e][key] = l.eidx
                l.needed = True
                o.waits.append(("e", e2, l))
            for b, v in dm:
                key = ("d", id(b))
                if self.clock[e].get(key, 0) >= v:
                    continue
                self.clock[e][key] = v
                o.waits.append(("d", b, v))
        for b in self.bufs:
            b.writers = []
            b.readers = []
            if b.dslot is not None:
                self.free_slots.append(b.dslot)
                b.dslot = None

    def emit(self):
        nc, es = self.nc, self.es
        for e in ENGS:
            eps_ = sorted(set(o.epoch for o in self.ops[e] if o.needed))
            for ep in eps_:
                self.esem[(e, ep)] = es.enter_context(nc.semaphore("es_%s_%d" % (e, ep)))
        n = 0
        for e in ENGS:
            for o in self.ops[e]:
                if o.is_dma and o.dbuf.dsem is None:
                    o.dbuf.dsem = es.enter_context(nc.semaphore("ds%d" % n))
                    n += 1
        self.ndsem = n
        for e in ENGS:
            c = {}
            for o in self.ops[e]:
                if o.needed:
                    c[o.epoch] = c.get(o.epoch, 0) + 1
                    o.semval = c[o.epoch]
        block = es.enter_context(nc.Block())

        def run(ename):
            def body(eng):
                for o in self.ops[ename]:
                    for (k, a, b) in o.waits:
                        if k == "d":
                            eng.wait_ge(a.dsem, b)
                        else:
                            eng.wait_ge(self.esem[(a, b.epoch)], b.semval)
                    ins = o.fn(eng)
                    if o.is_dma:
                        ins.then_inc(o.dbuf.dsem, 16)
                    elif o.needed:
                        ins.then_inc(self.esem[(ename, o.epoch)], 1)
            return body

        block.tensor(run("pe"))
        block.scalar(run("act"))
        block.vector(run("dve"))
        block.gpsimd(run("pool"))
        block.sync(run("sp"))


class TB:
    def __init__(self, p, t, name):
        self.t = t
        self.b = p.buf(name)

    def __getitem__(self, k):
        return self.t[k]


class Ring:
    def __init__(self, items):
        self.items = items
        self.i = 0

    def next(self):
        r = self.items[self.i % len(self.items)]
        self.i += 1
        return r


def build(stages=4, debug=False, only=None):
    nc = bass.Bass("TRN2", target_bir_lowering=False)

    def din(name, shape):
        return nc.dram_tensor(name, list(shape), F32, kind="ExternalInput").ap()

    xT = din("xT", [8, 128, TOK])
    cT = din("cT", [128, 8, NSEQ])
    ada_w = din("ada_w", [2, D, 6 * D])
    ada_bT = din("ada_bT", [128, 2, 48])
    lnp = din("lnp", [128, 2, 4, 8])
    ev_w_in = din("ev_w_in", [D, 2048])
    ev_w_out = din("ev_w_out", [D, D])
    sgp_d = din("sgp", [128, 2, 4])
    wsT_d = din("wsT", [128, 4, 128])
    bs_d = din("bs", [1, 512])
    wdw_d = din("wdw", [128, 4, 31])
    cvp_d = din("cvp", [128, 3, 4])
    od_w_qkv = din("od_w_qkv", [D, 3 * D])
    od_w_out = din("od_w_out", [D, D])
    wr_d = din("wr", [2, 128, 8, 20])
    br_d = din("br", [2, 1, 20])
    w_gate = din("moe_w_gate", [2, 16, 128, 8, 512])
    w_up = din("moe_w_up", [2, 16, 128, 8, 512])
    w_down = din("moe_w_down", [2, 16, 128, 4, D])
    consts_d = din("consts", [128, 2473])

    yT = nc.dram_tensor("yT", [8, 128, TOK], F32, kind="ExternalOutput").ap()
    XA = nc.dram_tensor("XA", [8, 128, TOK], F32, kind="Internal").ap()
    XB = nc.dram_tensor("XB", [8, 128, TOK], F32, kind="Internal").ap()
    dbg = {}

    es = ExitStack()
    p = Prog(nc, es)

    uniq = [0]

    def sb(st, name, shape, dt):
        uniq[0] += 1
        name = "%s_u%d" % (name, uniq[0])
        return TB(p, st.enter_context(nc.sbuf_tensor(name, list(shape), dt)), name)

    def MM(out, lhsT, rhs, first, last, reads, wb):
        p.op("pe", lambda e: e.matmul(out, lhsT, rhs, start=first, stop=last), reads=reads,
             writes=[wb] if first else [], joins=[] if first else [wb])

    def MMg(out, lhsT, rhs, reads, wb, newgroup):
        p.op("pe", lambda e: e.matmul(out, lhsT, rhs, start=True, stop=True), reads=reads,
             writes=[wb] if newgroup else [], joins=[] if newgroup else [wb])

    def ACT(out, in_, func, reads, writes, bias=None, scale=None):
        kw = {}
        if bias is not None:
            kw["bias"] = bias
        if scale is not None:
            kw["scale"] = scale
        p.op("act", lambda e: e.activation(out=out, in_=in_, func=func, **kw), reads=reads, writes=writes)

    def TTo(eng, out, in0, in1, op, reads, writes):
        p.op(eng, lambda e: e.tensor_tensor(out=out, in0=in0, in1=in1, op=op), reads=reads, writes=writes)

    def TS(eng, out, in0, s1, s2, op0, op1, reads, writes):
        if op1 is None:
            p.op(eng, lambda e: e.tensor_scalar(out=out, in0=in0, scalar1=s1, scalar2=None, op0=op0),
                 reads=reads, writes=writes)
        else:
            p.op(eng, lambda e: e.tensor_scalar(out=out, in0=in0, scalar1=s1, scalar2=s2, op0=op0, op1=op1),
                 reads=reads, writes=writes)

    def STT(out, in0, scalar, in1, op0, op1, reads, writes):
        p.op("dve", lambda e: e.scalar_tensor_tensor(out=out, in0=in0, scalar=scalar, in1=in1, op0=op0, op1=op1),
             reads=reads, writes=writes)

    def CP(eng, out, in_, reads, writes):
        p.op(eng, lambda e: e.tensor_copy(out=out, in_=in_), reads=reads, writes=writes)

    def RED(out, in_, op, reads, writes):
        p.op("dve", lambda e: e.tensor_reduce(out=out, in_=in_, axis=AX.X, op=op), reads=reads, writes=writes)

    def RECIP(out, in_, reads, writes):
        p.op("dve", lambda e: e.reciprocal(out=out, in_=in_), reads=reads, writes=writes)

    def MSET(eng, ap, val, writes):
        p.op(eng, lambda e: e.memset(ap, val), writes=writes)

    def DMA(q, out, in_, reads, writes, dbuf):
        p.op(q, lambda e: e.dma_start(out=out, in_=in_), reads=reads, writes=writes, dma=dbuf)

    G = ExitStack()
    es.enter_context(G)
    psb = [TB(p, G.enter_context(nc.psum_tensor("ps%d" % i, [128, 512], F32)), "ps%d" % i) for i in range(8)]
    for t_ in psb:
        t_.b.excl = True
    class TRing:
        def __init__(self, items):
            self.full = Ring(items)
            h = len(items) // 2
            self.sub = [Ring(items[:h]), Ring(items[h:])]

        def next(self):
            if p.turns is not None:
                return self.sub[p.turns.local.tid % 2].next()
            return self.full.next()
    PA = TRing(psb[0:4])
    PB = TRing(psb[4:6])
    PC = TRing(psb[6:8])

    cst = sb(G, "cst", [128, 2473], F32)
    DMA("sp", cst[:], consts_d[:, :], [], [cst.b], cst.b)
    ident = cst[:, 0:128]
    tri = cst[:, 128:256]
    ones = cst[:, 256:384]
    sel = cst[0:16, 384:2432]
    thr8 = cst[:, 2432:2440]
    jt32 = cst[:, 2440:2472]
    pcol = cst[:, 2472:2473]

    identb = sb(G, "identb", [128, 128], BF16)
    trib = sb(G, "trib", [128, 128], BF16)
    onesb = sb(G, "onesb", [128, 128], BF16)
    on1024 = sb(G, "on1024", [128, 128], BF16)
    on512 = sb(G, "on512", [128, 128], BF16)
    CP("dve", identb[:], ident, [cst.b], [identb.b])
    CP("dve", trib[:], tri, [cst.b], [trib.b])
    CP("dve", onesb[:], ones, [cst.b], [onesb.b])
    TS("dve", on1024[:], ones, 1.0 / 1024, None, ALU.mult, None, [cst.b], [on1024.b])
    TS("dve", on512[:], ones, 1.0 / 512, None, ALU.mult, None, [cst.b], [on512.b])
    ustrb = sb(G, "ustrb", [128, 128], BF16)
    TTo("dve", ustrb[:], tri, ident, ALU.subtract, [cst.b], [ustrb.b])
    epst = sb(G, "epst", [128, 1], F32)
    MSET("dve", epst[:], EPS, [epst.b])

    lnp_sb = sb(G, "lnp_sb", [128, 2, 4, 8], F32)
    DMA("sp", lnp_sb[:], lnp[:, :, :, :], [], [lnp_sb.b], lnp_sb.b)
    cond = sb(G, "cond", [128, 2, 48, NSEQ], F32)

    with ExitStack() as st:
        cT_sb = sb(st, "cT_sb", [128, 8, NSEQ], F32)
        csil = sb(st, "csil", [128, 8, NSEQ], BF16)
        adab = sb(st, "adab", [128, 2, 48], F32)
        DMA("sp", cT_sb[:], cT[:, :, :], [], [cT_sb.b], cT_sb.b)
        DMA("sp", adab[:], ada_bT[:, :, :], [], [adab.b], adab.b)
        ACT(csil[:], cT_sb[:], AF.Silu, [cT_sb.b], [csil.b])
        wb2 = [sb(st, "adaw%d" % i, [128, 8, 1024], BF16) for i in range(2)]
        k = 0
        for l in range(2):
            for s6 in range(6):
                w = wb2[k % 2]
                k += 1
                DMA("pool", w[:], ada_w[l, :, s6 * 1024:(s6 + 1) * 1024].rearrange("(kc p) n -> p kc n", p=128),
                    [], [w.b], w.b)
                ps = PC.next()
                for dc in range(8):
                    for kc in range(8):
                        first = (dc == 0 and kc == 0)
                        p.op("pe", (lambda e, o=ps[:, dc * 2:dc * 2 + 2], a=w[:, kc, dc * 128:(dc + 1) * 128],
                                    b=csil[:, kc, :], f=(kc == 0), la=(kc == 7): e.matmul(o, a, b, start=f, stop=la)),
                             reads=[w.b, csil.b], writes=[ps.b] if first else [], joins=[] if first else [ps.b])
                add1 = 1.0 if s6 in (1, 2, 4, 5) else 0.0
                for s in range(NSEQ):
                    STT(cond[:, l, s6 * 8:(s6 + 1) * 8, s], ps[:, s:16:2], add1, adab[:, l, s6 * 8:(s6 + 1) * 8],
                        ALU.add, ALU.add, [ps.b, adab.b], [cond.b])
        p.barrier()

    def cv(l, split, dc, s):
        return cond[:, l, split * 8 + dc, s:s + 1]

    def load_x(src, tok0, xt, W=TT):
        DMA("sp", xt[:], src[:, :, tok0:tok0 + W].rearrange("kc p t -> p kc t"), [], [xt.b], xt.b)

    def modulate(xt, hT, l, split_sh, split_sc, s):
        for dc in range(8):
            ACT(hT[:, dc, :], xt[:, dc, :], AF.Identity, [xt.b, cond.b], [hT.b] if dc == 0 else [],
                bias=cv(l, split_sh, dc, s), scale=cv(l, split_sc, dc, s)) if dc == 0 else \
                p.op("act", (lambda e, o=hT[:, dc, :], i=xt[:, dc, :], b=cv(l, split_sh, dc, s), sc=cv(l, split_sc, dc, s):
                             e.activation(out=o, in_=i, func=AF.Identity, bias=b, scale=sc)),
                     reads=[xt.b, cond.b], joins=[hT.b])

    def ln_fm(r, nch, onb, lnT, W=TT):
        rb, sd, mu = lnT["rb"], lnT["sd"], lnT["mu"]
        if "sqf" in lnT:
            sqf, sqb = lnT["sqf"], lnT["sqb"]
        else:
            sqf, sqb = (lambda c, t_=lnT["sq"]: t_[:, c, :]), [lnT["sq"].b]
        for c in range(nch):
            p.op("act", (lambda e, o=sqf(c), i=r[:, c, :]: e.activation(out=o, in_=i, func=AF.Square)),
                 reads=[r.b], writes=sqb if c == 0 else [], joins=[] if c == 0 else sqb)
            p.op("dve", (lambda e, o=rb[:, c, :], i=r[:, c, :]: e.tensor_copy(out=o, in_=i)),
                 reads=[r.b], writes=[rb.b] if c == 0 else [], joins=[] if c == 0 else [rb.b])
        mps = PB.next()
        for c in range(nch):
            MM(mps[:, 0:W], onb[:], rb[:, c, :], c == 0, c == nch - 1, [onb.b, rb.b], mps.b)
        vps = PC.next()
        for c in range(nch):
            MM(vps[:, 0:W], onb[:], sqf(c), c == 0, c == nch - 1, [onb.b] + sqb, vps.b)
        ACT(mu[:], mps[:, 0:W], AF.Identity, [mps.b], [mu.b])
        STT(sd[:], mu[:], -1.0, mu[:], ALU.mult, ALU.mult, [mu.b], [sd.b])
        STT(sd[:], vps[:, 0:W], EPS, sd[:], ALU.add, ALU.add, [vps.b, sd.b], [sd.b])
        ACT(sd[:], sd[:], AF.Sqrt, [sd.b], [sd.b])
        RECIP(vps[:, 0:W], sd[:], [sd.b], [vps.b])
        for c in range(nch):
            p.op("dve", (lambda e, o=r[:, c, :], a=r[:, c, :], b=mps[:, 0:W]: e.tensor_tensor(out=o, in0=a, in1=b, op=ALU.subtract)),
                 reads=[mps.b, r.b] if c == 0 else [mps.b], writes=[r.b] if c == 0 else [], joins=[] if c == 0 else [r.b])
        for c in range(nch):
            p.op("dve", (lambda e, o=r[:, c, :], a=r[:, c, :], b=vps[:, 0:W]: e.tensor_tensor(out=o, in0=a, in1=b, op=ALU.mult)),
                 reads=[vps.b, r.b] if c == 0 else [vps.b], writes=[r.b] if c == 0 else [], joins=[] if c == 0 else [r.b])

    def ln_out_store(r, gi, bi, l, dst, tok0, W=TT):
        xo = r
        for dc in range(8):
            p.op("act", (lambda e, o=xo[:, dc, :], i=r[:, dc, :], sc=lnp_sb[:, l, gi, dc:dc + 1], b=lnp_sb[:, l, bi, dc:dc + 1]:
                         e.activation(out=o, in_=i, func=AF.Identity, bias=b, scale=sc)),
                 reads=[r.b, lnp_sb.b] if dc == 0 else [lnp_sb.b], writes=[xo.b] if dc == 0 else [], joins=[] if dc == 0 else [xo.b])
        DMA("act", dst[:, :, tok0:tok0 + W].rearrange("kc p t -> p kc t"), xo[:], [xo.b], [], xo.b)

    def mixer0(src, dst, l):
        with ExitStack() as st:
            win = sb(st, "win", [128, 8, 2048], BF16)
            wout = sb(st, "wout", [128, 8, 1024], BF16)
            for j in range(4):
                p.op("pool", (lambda e, j=j: e.dma_start(out=win[:, :, j * 512:(j + 1) * 512],
                                                          in_=ev_w_in[:, j * 512:(j + 1) * 512].rearrange("(kc p) n -> p kc n", p=128))),
                     writes=[win.b] if j == 0 else [], joins=[] if j == 0 else [win.b], dma=win.b)
            for j in range(2):
                p.op("pool", (lambda e, j=j: e.dma_start(out=wout[:, :, j * 512:(j + 1) * 512],
                                                          in_=ev_w_out[:, j * 512:(j + 1) * 512].rearrange("(kc p) n -> p kc n", p=128))),
                     writes=[wout.b] if j == 0 else [], joins=[] if j == 0 else [wout.b], dma=wout.b)
            sgp = sb(st, "sgp_sb", [128, 2, 4], F32)
            DMA("sp", sgp[:], sgp_d[:, :, :], [], [sgp.b], sgp.b)
            cvp = sb(st, "cvp_sb", [128, 3, 4], F32)
            DMA("sp", cvp[:], cvp_d[:, :, :], [], [cvp.b], cvp.b)
            wsTm = sb(st, "wsTm", [128, 4, 128], BF16)
            C4 = sb(st, "C4", [128, 4, 128], F32)
            diag = sb(st, "diag", [128, 4, 31, 128], BF16)
            stmp = ExitStack()
            wsT = sb(stmp, "wsT_sb", [128, 4, 128], F32)
            DMA("sp", wsT[:], wsT_d[:, :, :], [], [wsT.b], wsT.b)
            bsB = sb(stmp, "bsB", [128, 4, 128], F32)
            DMA("sp", bsB[:].rearrange("p g q -> p (g q)"), bs_d[0:1, :].to_broadcast([128, 512]), [], [bsB.b], bsB.b)
            wdw = sb(stmp, "wdw_sb", [128, 4, 31], F32)
            DMA("sp", wdw[:], wdw_d[:, :, :], [], [wdw.b], wdw.b)
            for g in range(4):
                p.op("dve", (lambda e, g=g: e.tensor_tensor(out=wsTm[:, g, :], in0=wsT[:, g, :], in1=tri, op=ALU.mult)),
                     reads=[wsT.b, cst.b], writes=[wsTm.b] if g == 0 else [], joins=[] if g == 0 else [wsTm.b])
            rps = PC.next()
            for g in range(4):
                p.op("pe", (lambda e, g=g: e.matmul(rps[:, g * 128:(g + 1) * 128], onesb[:], wsTm[:, g, :], start=True, stop=True)),
                     reads=[onesb.b, wsTm.b], writes=[rps.b] if g == 0 else [], joins=[] if g == 0 else [rps.b])
            for g in range(4):
                p.op("dve", (lambda e, g=g: e.scalar_tensor_tensor(out=C4[:, g, :], in0=rps[:, g * 128:(g + 1) * 128],
                                                                    scalar=sgp[:, 1, g:g + 1], in1=bsB[:, g, :],
                                                                    op0=ALU.mult, op1=ALU.add)),
                     reads=[rps.b, sgp.b, bsB.b], writes=[C4.b] if g == 0 else [], joins=[] if g == 0 else [C4.b])
            for c in range(4):
                for k in range(31):
                    p.op("dve", (lambda e, c=c, k=k: e.tensor_scalar(out=diag[:, c, k, :], in0=ident, scalar1=wdw[:, c, k:k + 1],
                                                                      scalar2=None, op0=ALU.mult)),
                         reads=[cst.b, wdw.b], writes=[diag.b] if (c == 0 and k == 0) else [],
                         joins=[] if (c == 0 and k == 0) else [diag.b])

            p.barrier()
            stmp.close()
            WM = MIX_W
            NSB = WM // 128

            def mkM(seqs):
                GL = sb(st, "GL", [128, 4, 30 + SEQ], BF16).t
                GLh = p.buf("GLh")
                GLb = [p.buf("GLb%d" % i) for i in range(SEQ // WM)]
                MSET("pool", GL[:, :, 0:30], 0.0, [GLh])
                xt = sb(st, "xt", [128, 8, WM], F32)
                hT = sb(st, "hT", [128, 8, WM], BF16)
                uT = sb(st, "uT", [128, 4, WM], F32)
                vg = Ring([sb(st, "vg%d" % i, [128, TT], F32) for i in range(1)])
                st6 = sb(st, "st6", [128, 6], F32)
                mv = sb(st, "mv", [128, 2], F32)
                rs = sb(st, "rs", [128, 1], F32)
                vn = sb(st, "vn", [128, NSB, TT], BF16)
                t1 = sb(st, "t1", [128, WM], F32)
                yab = sb(st, "yab", [128, 8, WM], BF16)

                class _V:
                    def __init__(self, lo, name):
                        self.lo = lo
                        self.b = p.buf(name)

                    def __getitem__(self, k):
                        a, c, d_ = k
                        return yab[a, self.lo + c, d_]
                ya = _V(0, "ya")
                yb = _V(4, "yb")
                sg = Ring([sb(st, "sg%d" % i, [128, WM], F32) for i in range(2)])
                yc = sb(st, "yc", [128, 4, WM], F32)
                tmp = Ring([sb(st, "tmp%d" % i, [128, WM], F32) for i in range(2)])
                _rb = sb(st, "lnr_rb", [128, 8, WM], BF16)
                _sd = sb(st, "lnr_sd", [128, WM], F32)
                lnr = {"rb": _rb, "sd": _sd, "mu": t1, "sqf": (lambda c: yab[:, c, :]), "sqb": [ya.b, yb.b]}
                lnc = {"rb": _rb, "sd": _sd, "mu": t1, "sqf": (lambda c: yab[:, 4 + c, :]), "sqb": [yb.b]}

                def run():
                  for s in seqs:
                    for t in range(SEQ // WM):
                        tok0 = s * SEQ + t * WM
                        load_x(src, tok0, xt, WM)
                        modulate(xt, hT, l, 0, 1, s)
                        for fo in range(4):
                            ps = PA.next()
                            for kc in range(8):
                                MM(ps[:, 0:WM], win[:, kc, fo * 128:(fo + 1) * 128], hT[:, kc, :], kc == 0, kc == 7, [win.b, hT.b], ps.b)
                            p.op("act", (lambda e, o=uT[:, fo, :], i=ps[:, 0:WM]: e.activation(out=o, in_=i, func=AF.Gelu)),
                                 reads=[ps.b], writes=[uT.b] if fo == 0 else [], joins=[] if fo == 0 else [uT.b])
                        for sub in range(NSB):
                            ps = PA.next()
                            for kc in range(8):
                                MM(ps[:], hT[:, kc, sub * 128:(sub + 1) * 128], win[:, kc, 512:1024], kc == 0, kc == 7, [win.b, hT.b], ps.b)
                            v = vg.next()
                            ACT(v[:], ps[:], AF.Gelu, [ps.b], [v.b])
                            p.op("dve", (lambda e, v=v: e.bn_stats(out=st6[:], in_=v[:])), reads=[v.b], writes=[st6.b])
                            p.op("dve", lambda e: e.bn_aggr(out=mv[:], in_=st6[:]), reads=[st6.b], writes=[mv.b])
                            ACT(rs[:], mv[:, 1:2], AF.Sqrt, [mv.b, epst.b], [rs.b], bias=epst[:, 0:1])
                            RECIP(rs[:], rs[:], [rs.b], [rs.b])
                            p.op("dve", (lambda e, v=v, sub=sub: e.tensor_scalar(out=vn[:, sub, :], in0=v[:], scalar1=mv[:, 0:1], scalar2=rs[:, 0:1],
                                                                                  op0=ALU.subtract, op1=ALU.mult)),
                                 reads=[v.b, mv.b, rs.b], writes=[vn.b] if sub == 0 else [], joins=[] if sub == 0 else [vn.b])
                        for g in range(4):
                            ps = PA.next()
                            for sub in range(NSB):
                                p.op("pe", (lambda e, ps=ps, g=g, sub=sub: e.matmul(ps[:, sub * 128:(sub + 1) * 128], vn[:, sub, g * 128:(g + 1) * 128],
                                                                                     wsTm[:, g, :], start=True, stop=True)),
                                     reads=[vn.b, wsTm.b], writes=[ps.b] if sub == 0 else [], joins=[] if sub == 0 else [ps.b])
                            STT(t1[:].rearrange("p (c q) -> p c q", c=NSB), ps[:, 0:WM].rearrange("p (c q) -> p c q", c=NSB), sgp[:, 0, g:g + 1],
                                C4[:, g:g + 1, :].to_broadcast([128, NSB, 128]), ALU.mult, ALU.add, [ps.b, sgp.b, C4.b], [t1.b])
                            p.op("dve", (lambda e, g=g: e.tensor_tensor(out=ya[:, g, :], in0=t1[:], in1=uT[:, g, :], op=ALU.mult)),
                                 reads=[t1.b, uT.b], writes=[ya.b] if g == 0 else [], joins=[] if g == 0 else [ya.b])
                        for fo in range(4):
                            pa = PA.next()
                            for kc in range(8):
                                MM(pa[:, 0:WM], win[:, kc, 1024 + fo * 128:1024 + (fo + 1) * 128], hT[:, kc, :], kc == 0, kc == 7, [win.b, hT.b], pa.b)
                            pg = PA.next()
                            for kc in range(8):
                                MM(pg[:, 0:WM], win[:, kc, 1536 + fo * 128:1536 + (fo + 1) * 128], hT[:, kc, :], kc == 0, kc == 7, [win.b, hT.b], pg.b)
                            sgt = sg.next()
                            ACT(sgt[:], pg[:, 0:WM], AF.Sigmoid, [pg.b], [sgt.b])
                            p.op("dve", (lambda e, fo=fo, pa=pa, sgt=sgt, t=t: e.tensor_tensor(out=GL[:, fo, 30 + t * WM:30 + (t + 1) * WM], in0=pa[:, 0:WM], in1=sgt[:], op=ALU.mult)),
                                 reads=[pa.b, sgt.b], writes=[GLb[t]] if fo == 0 else [], joins=[] if fo == 0 else [GLb[t]])
                        glreads = [diag.b, GLb[t], GLh] + ([GLb[t - 1]] if t > 0 else [])
                        for c in range(4):
                            ps = PA.next()
                            for k in range(31):
                                MM(ps[:, 0:WM], diag[:, c, k, :], GL[:, c, t * WM + k:t * WM + k + WM], k == 0, k == 30, glreads, ps.b)
                            p.op("act", (lambda e, c=c, ps=ps: e.activation(out=yc[:, c, :], in_=ps[:, 0:WM], func=AF.Identity, bias=cvp[:, 0, c:c + 1])),
                                 reads=[ps.b, cvp.b], writes=[yc.b] if c == 0 else [], joins=[] if c == 0 else [yc.b])
                        ln_fm(yc, 4, on512, lnc, WM)
                        for c in range(4):
                            p.op("act", (lambda e, c=c: e.activation(out=yb[:, c, :], in_=yc[:, c, :], func=AF.Silu,
                                                                      bias=cvp[:, 2, c:c + 1], scale=cvp[:, 1, c:c + 1])),
                                 reads=[yc.b, cvp.b], writes=[yb.b] if c == 0 else [], joins=[] if c == 0 else [yb.b])
                        for dc in range(8):
                            ps = PA.next()
                            for kc in range(8):
                                src_y = ya if kc < 4 else yb
                                MM(ps[:, 0:WM], wout[:, kc, dc * 128:(dc + 1) * 128], src_y[:, kc % 4, :], kc == 0, kc == 7, [wout.b, ya.b, yb.b], ps.b)
                            tm = tmp.next()
                            ACT(tm[:], ps[:, 0:WM], AF.Identity, [ps.b, cond.b], [tm.b], scale=cv(l, 2, dc, s))
                            p.op("dve", (lambda e, dc=dc, tm=tm: e.scalar_tensor_tensor(out=xt[:, dc, :], in0=xt[:, dc, :], scalar=ALPHA, in1=tm[:],
                                                                                  op0=ALU.mult, op1=ALU.add)),
                                 reads=[xt.b, tm.b] if dc == 0 else [tm.b], writes=[xt.b] if dc == 0 else [], joins=[] if dc == 0 else [xt.b])
                        ln_fm(xt, 8, on1024, lnr, WM)
                        ln_out_store(xt, 0, 1, l, dst, tok0, WM)
                return run
            p.run_threads([mkM([0]), mkM([1])] if MIX_W == 256 else [mkM([0, 1])], head=HEAD_M)
            p.barrier()

    ST = 1024
    NTI = ST // 128

    def moe(src, dst, l):
        with ExitStack() as st:
            wr_sb = sb(st, "wr_sb", [128, 8, 20], F32)
            DMA("sp", wr_sb[:], wr_d[l, :, :, :], [], [wr_sb.b], wr_sb.b)
            brB = sb(st, "brB", [128, 20], F32)
            DMA("sp", brB[:], br_d[l, 0:1, :].to_broadcast([128, 20]), [], [brB.b], brB.b)
            hT = sb(st, "hTm", [128, 8, ST], BF16)
            yacc_t = sb(st, "yacc", [128, 2, 8, TT], F32).t
            yb_ = [[p.buf("yacc%d_%d" % (a, b)) for b in range(8)] for a in range(2)]
            wgs = Ring([sb(st, "wg%d" % i, [128, 8, 512], BF16) for i in range(2)])
            wus = Ring([sb(st, "wu%d" % i, [128, 8, 512], BF16) for i in range(2)])
            wds = Ring([sb(st, "wd%d" % i, [128, 4, 1024], BF16) for i in range(2)])
            xt = sb(st, "xtm", [128, 8, TT], F32)
            hf = sb(st, "hf", [128, 8, 128], F32)
            acts = Ring([sb(st, "act%d" % i, [128, 4, TT], BF16) for i in range(2)])
            sgs = Ring([sb(st, "sgm%d" % i, [128, TT], F32) for i in range(2)])
            t1s = Ring([sb(st, "t1m%d" % i, [128, TT], F32) for i in range(2)])
            lnr = {"rb": sb(st, "lnm_rb", [128, 8, TT], BF16), "sq": sb(st, "lnm_sq", [128, 8, TT], BF16), "sd": sb(st, "lnm_sd", [128, TT], F32), "mu": sb(st, "lnm_mu", [128, TT], F32)}
            L = sb(st, "Lrt", [128, NTI, 20], F32)
            cwT = sb(st, "cwT", [16, ST], F32)

            def rt(name, shape):
                return sb(st, "rt_" + name, shape, F32)
            gmax = rt("gmax", [128, NTI]); gsel = rt("gsel", [128, NTI, 4]); gd = rt("gd", [128, NTI, 4])
            gsum = rt("gsum", [128, NTI]); gw = rt("gw", [128, NTI]); tmp4 = rt("tmp4", [128, NTI, 4, 4])
            ig = rt("ig", [128, NTI, 4]); m1 = rt("m1", [128, NTI]); oh1 = rt("oh1", [128, NTI, 4])
            ig2 = rt("ig2", [128, NTI, 4]); m2 = rt("m2", [128, NTI]); oh2 = rt("oh2", [128, NTI, 4])
            dd = rt("dd", [128, NTI]); w1 = rt("w1", [128, NTI]); w2 = rt("w2", [128, NTI])
            a1 = rt("a1", [128, NTI, 4]); a2 = rt("a2", [128, NTI, 4]); cw = rt("cw", [128, NTI, 4, 4])

            def bc3(t):
                return t[:].unsqueeze(2).to_broadcast([128, NTI, 4])

            for sti in range(TOK // ST):
                s = (sti * ST) // SEQ
                base = sti * ST
                for tt in range(ST // TT):
                    load_x(src, base + tt * TT, xt)
                    for dc in range(8):
                        p.op("act", (lambda e, o=hT[:, dc, tt * TT:(tt + 1) * TT], i=xt[:, dc, :], b=cv(l, 3, dc, s), sc=cv(l, 4, dc, s):
                                     e.activation(out=o, in_=i, func=AF.Identity, bias=b, scale=sc)),
                             reads=[xt.b, cond.b], writes=[hT.b] if (dc == 0 and tt == 0) else [],
                             joins=[] if (dc == 0 and tt == 0) else [hT.b])
                    for sub in range(4):
                        for dc in range(8):
                            p.op("dve", (lambda e, o=hf[:, dc, :], i=xt[:, dc, sub * 128:(sub + 1) * 128], b=cv(l, 3, dc, s), sc=cv(l, 4, dc, s):
                                         e.tensor_scalar(out=o, in0=i, scalar1=sc, scalar2=b, op0=ALU.mult, op1=ALU.add)),
                                 reads=[xt.b, cond.b], writes=[hf.b] if dc == 0 else [], joins=[] if dc == 0 else [hf.b])
                        lps = PC.next()
                        for kc in range(8):
                            MM(lps[:, 0:20], hf[:, kc, :], wr_sb[:, kc, :], kc == 0, kc == 7, [hf.b, wr_sb.b], lps.b)
                        i16 = tt * 4 + sub
                        p.op("dve", (lambda e, o=L[:, i16, :], a=lps[:, 0:20]: e.tensor_tensor(out=o, in0=a, in1=brB[:], op=ALU.add)),
                             reads=[lps.b, brB.b], writes=[L.b] if i16 == 0 else [], joins=[] if i16 == 0 else [L.b])
                Lg = L[:, :, 0:4]
                Le = L[:, :, 4:20].rearrange("p t (g j) -> p t g j", g=4)
                RED(gmax[:], Lg, ALU.max, [L.b], [gmax.b])
                TTo("dve", gsel[:], Lg, bc3(gmax), ALU.is_equal, [L.b, gmax.b], [gsel.b])
                TTo("dve", gd[:], Lg, bc3(gmax), ALU.subtract, [L.b, gmax.b], [gd.b])
                ACT(gd[:], gd[:], AF.Exp, [gd.b], [gd.b])
                RED(gsum[:], gd[:], ALU.add, [gd.b], [gsum.b])
                RECIP(gw[:], gsum[:], [gsum.b], [gw.b])
                TTo("dve", tmp4[:], Le, gsel[:].unsqueeze(3).to_broadcast([128, NTI, 4, 4]), ALU.mult, [L.b, gsel.b], [tmp4.b])
                RED(ig[:], tmp4[:].rearrange("p t g j -> p t j g"), ALU.add, [tmp4.b], [ig.b])
                RED(m1[:], ig[:], ALU.max, [ig.b], [m1.b])
                TTo("dve", oh1[:], ig[:], bc3(m1), ALU.is_equal, [ig.b, m1.b], [oh1.b])
                STT(ig2[:], oh1[:], NEG, ig[:], ALU.mult, ALU.add, [oh1.b, ig.b], [ig2.b])
                RED(m2[:], ig2[:], ALU.max, [ig2.b], [m2.b])
                TTo("dve", oh2[:], ig2[:], bc3(m2), ALU.is_equal, [ig2.b, m2.b], [oh2.b])
                TTo("dve", dd[:], m2[:], m1[:], ALU.subtract, [m1.b, m2.b], [dd.b])
                ACT(dd[:], dd[:], AF.Exp, [dd.b], [dd.b])
                TS("dve", w1[:], dd[:], 1.0, None, ALU.add, None, [dd.b], [w1.b])
                RECIP(w1[:], w1[:], [w1.b], [w1.b])
                TTo("dve", w2[:], dd[:], w1[:], ALU.mult, [dd.b, w1.b], [w2.b])
                TTo("dve", w1[:], w1[:], gw[:], ALU.mult, [w1.b, gw.b], [w1.b])
                TTo("dve", w2[:], w2[:], gw[:], ALU.mult, [w2.b, gw.b], [w2.b])
                TTo("dve", a1[:], oh1[:], bc3(w1), ALU.mult, [oh1.b, w1.b], [a1.b])
                TTo("dve", a2[:], oh2[:], bc3(w2), ALU.mult, [oh2.b, w2.b], [a2.b])
                TTo("dve", a1[:], a1[:], a2[:], ALU.add, [a1.b, a2.b], [a1.b])
                TTo("dve", cw[:], gsel[:].unsqueeze(3).to_broadcast([128, NTI, 4, 4]),
                    a1[:].unsqueeze(2).to_broadcast([128, NTI, 4, 4]), ALU.mult, [gsel.b, a1.b], [cw.b])
                for i in range(NTI):
                    tp = PC.next()
                    p.op("pe", (lambda e, tp=tp, i=i: e.transpose(tp[0:16, 0:128], cw[:, i, :, :].rearrange("p g j -> p (g j)"), ident)),
                         reads=[cw.b, cst.b], writes=[tp.b])
                    p.op("act", (lambda e, tp=tp, i=i: e.activation(out=cwT[0:16, i * 128:(i + 1) * 128], in_=tp[0:16, 0:128], func=AF.Identity)),
                         reads=[tp.b], writes=[cwT.b] if i == 0 else [], joins=[] if i == 0 else [cwT.b])
                for ex in range(16):
                    wg, wu, wd = wgs.next(), wus.next(), wds.next()
                    DMA("pool", wg[:], w_gate[l, ex], [], [wg.b], wg.b)
                    DMA("pool", wu[:], w_up[l, ex], [], [wu.b], wu.b)
                    DMA("pool", wd[:], w_down[l, ex], [], [wd.b], wd.b)
                    for tt in range(ST // TT):
                        cps = PC.next()
                        MM(cps[:], sel[:, ex * 128:(ex + 1) * 128], cwT[0:16, tt * TT:(tt + 1) * TT], True, True, [cst.b, cwT.b], cps.b)
                        act = acts.next()
                        for fc in range(4):
                            gps = PA.next()
                            for kc in range(8):
                                MM(gps[:], wg[:, kc, fc * 128:(fc + 1) * 128], hT[:, kc, tt * TT:(tt + 1) * TT], kc == 0, kc == 7, [wg.b, hT.b], gps.b)
                            ups = PA.next()
                            for kc in range(8):
                                MM(ups[:], wu[:, kc, fc * 128:(fc + 1) * 128], hT[:, kc, tt * TT:(tt + 1) * TT], kc == 0, kc == 7, [wu.b, hT.b], ups.b)
                            sg = sgs.next()
                            t1 = t1s.next()
                            ACT(sg[:], gps[:], AF.Silu, [gps.b], [sg.b])
                            TTo("dve", t1[:], sg[:], ups[:], ALU.mult, [sg.b, ups.b], [t1.b])
                            p.op("dve", (lambda e, o=act[:, fc, :], a=t1[:], b=cps[:]: e.tensor_tensor(out=o, in0=a, in1=b, op=ALU.mult)),
                                 reads=[t1.b, cps.b], writes=[act.b] if fc == 0 else [], joins=[] if fc == 0 else [act.b])
                        for dc in range(8):
                            dps = PB.next()
                            for fc in range(4):
                                MM(dps[:], wd[:, fc, dc * 128:(dc + 1) * 128], act[:, fc, :], fc == 0, fc == 3, [wd.b, act.b], dps.b)
                            if ex == 0:
                                p.op("act", (lambda e, o=yacc_t[:, tt, dc, :], i=dps[:]: e.activation(out=o, in_=i, func=AF.Identity)),
                                     reads=[dps.b], writes=[yb_[tt][dc]])
                            else:
                                p.op("dve", (lambda e, o=yacc_t[:, tt, dc, :], i=dps[:]: e.tensor_tensor(out=o, in0=o, in1=i, op=ALU.add)),
                                     reads=[dps.b], writes=[yb_[tt][dc]])
                for tt in range(ST // TT):
                    tok0 = base + tt * TT
                    load_x(src, tok0, xt)
                    for dc in range(8):
                        tm = t1s.next()
                        ACT(tm[:], yacc_t[:, tt, dc, :], AF.Identity, [yb_[tt][dc], cond.b], [tm.b], scale=cv(l, 5, dc, s))
                        p.op("dve", (lambda e, dc=dc, tm=tm: e.scalar_tensor_tensor(out=xt[:, dc, :], in0=xt[:, dc, :], scalar=ALPHA, in1=tm[:],
                                                                              op0=ALU.mult, op1=ALU.add)),
                             reads=[xt.b, tm.b] if dc == 0 else [tm.b], writes=[xt.b] if dc == 0 else [], joins=[] if dc == 0 else [xt.b])
                    ln_fm(xt, 8, on1024, lnr)
                    ln_out_store(xt, 2, 3, l, dst, tok0)
            p.barrier()

    T_S = 512
    NTL = 31
    S_ROWS = NTL * T_S
    Hs = nc.dram_tensor("Hs", [S_ROWS, D], BF16, kind="Internal").ap()
    Ys = nc.dram_tensor("Ys", [S_ROWS, D], F32, kind="Internal").ap()

    dynreg = {}

    def moe_sparse(src, dst, l, zero_fill):
        NI = TOK // 128
        with ExitStack() as st:
            slots_i = sb(st, "slots_i", [128, NI, 2], I32)
            wgt = sb(st, "wgt", [128, NI, 2], F32)
            te_i = sb(st, "te_i", [128, 32], I32)
            widx = sb(st, "widx", [128, 32, 2], I32)
            HsB = p.buf("HsB")
            ZFB = p.buf("ZFB")
            with ExitStack() as sa:
                wr_sb = sb(sa, "wr_sb", [128, 8, 20], F32)
                DMA("sp", wr_sb[:], wr_d[l, :, :, :], [], [wr_sb.b], wr_sb.b)
                brB = sb(sa, "brB", [128, 20], F32)
                DMA("sp", brB[:], br_d[l, 0:1, :].to_broadcast([128, 20]), [], [brB.b], brB.b)
                htok = sb(sa, "htok", [128, NI, D], BF16)
                L = sb(sa, "LA", [128, NI, 20], F32)
                if zero_fill:
                    zt = sb(sa, "zt", [128, 4, D], BF16)
                    MSET("pool", zt[:].rearrange("p a n -> p (a n)"), 0.0, [zt.b])
                    for a in range(S_ROWS // 512):
                        p.op("sp", (lambda e, a=a: e.dma_start(out=Hs[a * 512:(a + 1) * 512, :].rearrange("(a p) n -> p a n", p=128), in_=zt[:])),
                             reads=[zt.b], joins=[ZFB], dma=ZFB)
                def mkA(tid, nth):
                    xt = sb(sa, "xtA", [128, 8, TT], F32)
                    hT = sb(sa, "hTA", [128, 8, TT], BF16)
                    hf = sb(sa, "hfA", [128, 8, 128], F32)

                    def run():
                        for t8 in range(tid, TOK // TT, nth):
                            s = (t8 * TT) // SEQ
                            load_x(src, t8 * TT, xt)
                            for dc in range(8):
                                p.op("act", (lambda e, o=hT[:, dc, :], i=xt[:, dc, :], b=cv(l, 3, dc, s), sc=cv(l, 4, dc, s):
                                             e.activation(out=o, in_=i, func=AF.Identity, bias=b, scale=sc)),
                                     reads=[xt.b, cond.b], writes=[hT.b] if dc == 0 else [], joins=[] if dc == 0 else [hT.b])
                            for sub in range(4):
                                i = t8 * 4 + sub
                                for dc in range(8):
                                    p.op("dve", (lambda e, o=hf[:, dc, :], i_=xt[:, dc, sub * 128:(sub + 1) * 128], b=cv(l, 3, dc, s), sc=cv(l, 4, dc, s):
                                                 e.tensor_scalar(out=o, in0=i_, scalar1=sc, scalar2=b, op0=ALU.mult, op1=ALU.add)),
                                         reads=[xt.b, cond.b], writes=[hf.b] if dc == 0 else [], joins=[] if dc == 0 else [hf.b])
                                lps = PC.next()
                                for kc in range(8):
                                    MM(lps[:, 0:20], hf[:, kc, :], wr_sb[:, kc, :], kc == 0, kc == 7, [hf.b, wr_sb.b], lps.b)
                                p.op("dve", (lambda e, o=L[:, i, :], a=lps[:, 0:20]: e.tensor_tensor(out=o, in0=a, in1=brB[:], op=ALU.add)),
                                     reads=[lps.b, brB.b], joins=[L.b])
                                tp = PA.next()
                                tpb = tp.t[:].bitcast(BF16)
                                for kc in range(8):
                                    p.op("pe", (lambda e, o=tpb[:, kc * 128:(kc + 1) * 128], a=hT[:, kc, sub * 128:(sub + 1) * 128]: e.transpose(o, a, identb[:])),
                                         reads=[hT.b, identb.b], writes=[tp.b] if kc == 0 else [], joins=[] if kc == 0 else [tp.b])
                                p.op("act", (lambda e, o=htok[:, i, :], a=tpb[:, 0:1024]: e.activation(out=o, in_=a, func=AF.Identity)),
                                     reads=[tp.b], joins=[htok.b])

                    return run
                p.run_threads([mkA(k, NTHR_A) for k in range(NTHR_A)], head=HEAD_A)

                def rt(name, shape, dt=F32):
                    return sb(sa, "rs_" + name, shape, dt)
                gmax = rt("gmax", [128, NI]); gsel = rt("gsel", [128, NI, 4]); gd = rt("gd", [128, NI, 4])
                gsum = rt("gsum", [128, NI]); gw = rt("gw", [128, NI]); tmp4 = rt("tmp4", [128, NI, 4, 4])
                ig = rt("ig", [128, NI, 4]); m1_ = rt("m1", [128, NI]); oh1 = rt("oh1", [128, NI, 4])
                ig2 = rt("ig2", [128, NI, 4]); m2_ = rt("m2", [128, NI]); oh2 = rt("oh2", [128, NI, 4])
                dd = rt("dd", [128, NI]); w1 = rt("w1", [128, NI]); w2 = rt("w2", [128, NI])
                M1 = rt("M1", [128, NI, 4, 4]); M2 = rt("M2", [128, NI, 4, 4]); Mm = rt("Mm", [128, NI, 16])
                mb = rt("mb", [128, NI, 16], BF16); Mp = rt("Mp", [128, NI + 1, 16], BF16)
                rank = rt("rank", [128, NI, 16]); cnt = rt("cnt", [128, 16]); cmp8 = rt("cmp8", [128, 16, 8])
                ncap = rt("ncap", [128, 16]); cap = rt("cap", [128, 16]); sa_ = rt("sa", [128, 16]); sb_ = rt("sb", [128, 16])
                start = rt("start", [128, 16]); pos = rt("pos", [128, NI, 16]); tmpp = rt("tmpp", [128, NI, 16])
                slots_f = rt("slots_f", [128, NI, 2]); cmpj = rt("cmpj", [128, 32, 16]); tef = rt("tef", [128, 32])

                def bc3(t):
                    return t[:].unsqueeze(2).to_broadcast([128, NI, 4])
                Lg = L[:, :, 0:4]
                Le = L[:, :, 4:20].rearrange("p t (g j) -> p t g j", g=4)
                RED(gmax[:], Lg, ALU.max, [L.b], [gmax.b])
                TTo("dve", gsel[:], Lg, bc3(gmax), ALU.is_equal, [L.b, gmax.b], [gsel.b])
                TTo("dve", gd[:], Lg, bc3(gmax), ALU.subtract, [L.b, gmax.b], [gd.b])
                ACT(gd[:], gd[:], AF.Exp, [gd.b], [gd.b])
                RED(gsum[:], gd[:], ALU.add, [gd.b], [gsum.b])
                RECIP(gw[:], gsum[:], [gsum.b], [gw.b])
                TTo("dve", tmp4[:], Le, gsel[:].unsqueeze(3).to_broadcast([128, NI, 4, 4]), ALU.mult, [L.b, gsel.b], [tmp4.b])
                RED(ig[:], tmp4[:].rearrange("p t g j -> p t j g"), ALU.add, [tmp4.b], [ig.b])
                RED(m1_[:], ig[:], ALU.max, [ig.b], [m1_.b])
                TTo("dve", oh1[:], ig[:], bc3(m1_), ALU.is_equal, [ig.b, m1_.b], [oh1.b])
                STT(ig2[:], oh1[:], NEG, ig[:], ALU.mult, ALU.add, [oh1.b, ig.b], [ig2.b])
                RED(m2_[:], ig2[:], ALU.max, [ig2.b], [m2_.b])
                TTo("dve", oh2[:], ig2[:], bc3(m2_), ALU.is_equal, [ig2.b, m2_.b], [oh2.b])
                TTo("dve", dd[:], m2_[:], m1_[:], ALU.subtract, [m1_.b, m2_.b], [dd.b])
                ACT(dd[:], dd[:], AF.Exp, [dd.b], [dd.b])
                TS("dve", w1[:], dd[:], 1.0, None, ALU.add, None, [dd.b], [w1.b])
                RECIP(w1[:], w1[:], [w1.b], [w1.b])
                TTo("dve", w2[:], dd[:], w1[:], ALU.mult, [dd.b, w1.b], [w2.b])
                TTo("dve", wgt[:, :, 0], w1[:], gw[:], ALU.mult, [w1.b, gw.b], [wgt.b])
                p.op("dve", lambda e: e.tensor_tensor(out=wgt[:, :, 1], in0=w2[:], in1=gw[:], op=ALU.mult), reads=[w2.b, gw.b], joins=[wgt.b])
                TTo("dve", M1[:], gsel[:].unsqueeze(3).to_broadcast([128, NI, 4, 4]), oh1[:].unsqueeze(2).to_broadcast([128, NI, 4, 4]), ALU.mult,
                    [gsel.b, oh1.b], [M1.b])
                TTo("dve", M2[:], gsel[:].unsqueeze(3).to_broadcast([128, NI, 4, 4]), oh2[:].unsqueeze(2).to_broadcast([128, NI, 4, 4]), ALU.mult,
                    [gsel.b, oh2.b], [M2.b])
                M1v = M1[:].rearrange("p t g j -> p t (g j)")
                M2v = M2[:].rearrange("p t g j -> p t (g j)")
                TTo("dve", Mm[:], M1v, M2v, ALU.add, [M1.b, M2.b], [Mm.b])
                CP("dve", mb[:], Mm[:], [Mm.b], [mb.b])
                MSET("dve", Mp[:, 0, :], 0.0, [Mp.b])
                for i in range(NI):
                    p.op("dve", (lambda e, i=i: e.tensor_tensor(out=Mp[:, i + 1, :], in0=Mp[:, i, :], in1=mb[:, i, :], op=ALU.add)),
                         reads=[mb.b], writes=[Mp.b])
                rps = PB.next()
                for i in range(NI):
                    p.op("pe", (lambda e, i=i: e.matmul(rps[:, i * 16:(i + 1) * 16], ustrb[:], mb[:, i, :], start=True, stop=False)),
                         reads=[ustrb.b, mb.b], writes=[rps.b] if i == 0 else [], joins=[] if i == 0 else [rps.b])
                    p.op("pe", (lambda e, i=i: e.matmul(rps[:, i * 16:(i + 1) * 16], onesb[:], Mp[:, i, :], start=False, stop=True)),
                         reads=[onesb.b, Mp.b], joins=[rps.b])
                ACT(rank[:].rearrange("p t e -> p (t e)"), rps[:], AF.Identity, [rps.b], [rank.b])
                cps_ = PC.next()
                MM(cps_[:, 0:16], onesb[:], Mp[:, NI, :], True, True, [onesb.b, Mp.b], cps_.b)
                ACT(cnt[:], cps_[:, 0:16], AF.Identity, [cps_.b], [cnt.b])
                TTo("dve", cmp8[:], cnt[:].unsqueeze(2).to_broadcast([128, 16, 8]), thr8.unsqueeze(1).to_broadcast([128, 16, 8]), ALU.is_gt,
                    [cnt.b, cst.b], [cmp8.b])
                RED(ncap[:], cmp8[:], ALU.add, [cmp8.b], [ncap.b])
                TS("dve", cap[:], ncap[:], float(T_S), None, ALU.mult, None, [ncap.b], [cap.b])
                srcb, dstb = cap, sa_
                for sh in (1, 2, 4, 8):
                    p.op("dve", (lambda e, a=srcb, d_=dstb, sh=sh: e.tensor_copy(out=d_[:, 0:sh], in_=a[:, 0:sh])), reads=[srcb.b], writes=[dstb.b])
                    p.op("dve", (lambda e, a=srcb, d_=dstb, sh=sh: e.tensor_tensor(out=d_[:, sh:16], in0=a[:, sh:16], in1=a[:, 0:16 - sh], op=ALU.add)),
                         reads=[srcb.b], joins=[dstb.b])
                    srcb, dstb = dstb, (sb_ if dstb is sa_ else sa_)
                incl = srcb
                TTo("dve", start[:], incl[:], cap[:], ALU.subtract, [incl.b, cap.b], [start.b])
                TTo("dve", pos[:], rank[:], start[:].unsqueeze(1).to_broadcast([128, NI, 16]), ALU.add, [rank.b, start.b], [pos.b])
                TTo("dve", tmpp[:], M1v, pos[:], ALU.mult, [M1.b, pos.b], [tmpp.b])
                RED(slots_f[:, :, 0], tmpp[:], ALU.add, [tmpp.b], [slots_f.b])
                TTo("dve", tmpp[:], M2v, pos[:], ALU.mult, [M2.b, pos.b], [tmpp.b])
                p.op("dve", lambda e: e.tensor_reduce(out=slots_f[:, :, 1], in_=tmpp[:], axis=AX.X, op=ALU.add), reads=[tmpp.b], joins=[slots_f.b])
                CP("dve", slots_i[:], slots_f[:], [slots_f.b], [slots_i.b])
                TTo("dve", cmpj[:], incl[:].unsqueeze(1).to_broadcast([128, 32, 16]), jt32.unsqueeze(2).to_broadcast([128, 32, 16]), ALU.is_le,
                    [incl.b, cst.b], [cmpj.b])
                RED(tef[:], cmpj[:], ALU.add, [cmpj.b], [tef.b])
                TS("dve", tef[:], tef[:], 15.0, None, ALU.min, None, [tef.b], [tef.b])
                CP("dve", te_i[:], tef[:], [tef.b], [te_i.b])
                TS("dve", tef[:], tef[:], float(l * 16), 128.0, ALU.add, ALU.mult, [tef.b], [tef.b])
                TS("dve", tef[:], tef[:], pcol, 2.0, ALU.add, ALU.mult, [tef.b, cst.b], [tef.b])
                CP("dve", widx[:, :, 0], tef[:], [tef.b], [widx.b])
                TS("dve", tef[:], tef[:], 1.0, None, ALU.add, None, [tef.b], [tef.b])
                p.op("dve", lambda e: e.tensor_copy(out=widx[:, :, 1], in_=tef[:]), reads=[tef.b], joins=[widx.b])
                for i in range(NI):
                    for k in range(2):
                        p.op("pool", (lambda e, i=i, k=k: e.indirect_dma_start(
                            out=Hs[:, :], out_offset=bass.IndirectOffsetOnAxis(ap=slots_i[:, i, k:k + 1].bitcast(U32), axis=0),
                            in_=htok[:, i, :], in_offset=None)),
                            reads=[htok.b, slots_i.b, ZFB], joins=[HsB], dma=HsB)
                if debug:
                    d1 = nc.dram_tensor("dbg_slots", [128, NI, 2], I32, kind="ExternalOutput").ap()
                    DMA("sp", d1[:, :, :], slots_i[:], [slots_i.b], [], slots_i.b)
                    d2 = nc.dram_tensor("dbg_wgt", [128, NI, 2], F32, kind="ExternalOutput").ap()
                    DMA("sp", d2[:, :, :], wgt[:], [wgt.b], [], wgt.b)
                    d3 = nc.dram_tensor("dbg_te", [128, 32], I32, kind="ExternalOutput").ap()
                    DMA("sp", d3[:, :], te_i[:], [te_i.b], [], te_i.b)
                    d4 = nc.dram_tensor("dbg_mm", [128, NI, 16], F32, kind="ExternalOutput").ap()
                    DMA("sp", d4[:, :, :], Mm[:], [Mm.b], [], Mm.b)
                    d5 = nc.dram_tensor("dbg_rank", [128, NI, 16], F32, kind="ExternalOutput").ap()
                    DMA("sp", d5[:, :, :], rank[:], [rank.b], [], rank.b)
                    d6 = nc.dram_tensor("dbg_start", [128, 16], F32, kind="ExternalOutput").ap()
                    DMA("sp", d6[:, :], start[:], [start.b], [], start.b)
                p.barrier()
            if "B" not in MOE_DEBUG:
                return
            with ExitStack() as sbk:
                def mkB(tid, nth):
                    nb = 2 if nth == 1 else 1
                    wgs = Ring([sb(sbk, "swg%d" % i, [128, 8, 512], BF16) for i in range(2)])
                    wus = Ring([sb(sbk, "swu%d" % i, [128, 8, 512], BF16) for i in range(2)])
                    wds = Ring([sb(sbk, "swd%d" % i, [128, 4, 1024], BF16) for i in range(2)])
                    hrows = Ring([sb(sbk, "hrow%d" % i, [128, D], BF16) for i in range(4)])
                    hsTs = Ring([sb(sbk, "hsT%d" % i, [128, 8, T_S], BF16) for i in range(nb)])
                    acts = Ring([sb(sbk, "sact%d" % i, [128, 4, T_S], BF16) for i in range(nb)])
                    sgs = Ring([sb(sbk, "ssg%d" % i, [128, T_S], F32) for i in range(2)])
                    yrows = Ring([sb(sbk, "yrow%d" % i, [128, 512], F32) for i in range(4)])

                    def run():
                        for j in range(tid, NTL, nth):
                            wg, wu, wd = wgs.next(), wus.next(), wds.next()

                            for (dst_t, src5) in ((wg, w_gate), (wu, w_up), (wd, w_down)):
                                for hh in range(2):
                                    p.op("pool", (lambda e, dst_t=dst_t, src5=src5, j=j, hh=hh: e.indirect_dma_start(
                                        out=dst_t[:].rearrange("p (h k) n -> p h (k n)", h=2)[:, hh, :], out_offset=None,
                                        in_=src5.rearrange("l e p (h k) n -> (l e p h) (k n)", h=2),
                                        in_offset=bass.IndirectOffsetOnAxis(ap=widx[:, j, hh:hh + 1].bitcast(U32), axis=0))),
                                        reads=[widx.b], writes=[dst_t.b] if hh == 0 else [], joins=[] if hh == 0 else [dst_t.b], dma=dst_t.b)
                            hsT = hsTs.next()
                            for sub in range(4):
                                hr = hrows.next()
                                r0 = j * T_S + sub * 128
                                DMA("sp", hr[:], Hs[r0:r0 + 128, :], [HsB], [hr.b], hr.b)
                                tp = PA.next()
                                tpb = tp.t[:].bitcast(BF16)
                                for kc in range(8):
                                    p.op("pe", (lambda e, o=tpb[:, kc * 128:(kc + 1) * 128], a=hr[:, kc * 128:(kc + 1) * 128]: e.transpose(o, a, identb[:])),
                                         reads=[hr.b, identb.b], writes=[tp.b] if kc == 0 else [], joins=[] if kc == 0 else [tp.b])
                                eng = "act" if sub % 2 == 0 else "dve"
                                o_ = hsT[:, :, sub * 128:(sub + 1) * 128]
                                i_ = tpb[:, 0:1024].rearrange("p (k t) -> p k t", k=8)
                                if eng == "act":
                                    p.op("act", (lambda e, o_=o_, i_=i_: e.activation(out=o_, in_=i_, func=AF.Identity)),
                                         reads=[tp.b], writes=[hsT.b] if sub == 0 else [], joins=[] if sub == 0 else [hsT.b])
                                else:
                                    p.op("dve", (lambda e, o_=o_, i_=i_: e.tensor_copy(out=o_, in_=i_)),
                                         reads=[tp.b], writes=[hsT.b] if sub == 0 else [], joins=[] if sub == 0 else [hsT.b])
                            act = acts.next()
                            for fc in range(4):
                                gps = PA.next()
                                for kc in range(8):
                                    MM(gps[:], wg[:, kc, fc * 128:(fc + 1) * 128], hsT[:, kc, :], kc == 0, kc == 7, [wg.b, hsT.b], gps.b)
                                ups = PA.next()
                                for kc in range(8):
                                    MM(ups[:], wu[:, kc, fc * 128:(fc + 1) * 128], hsT[:, kc, :], kc == 0, kc == 7, [wu.b, hsT.b], ups.b)
                                sg = sgs.next()
                                ACT(sg[:], gps[:], AF.Silu, [gps.b], [sg.b])
                                p.op("dve", (lambda e, o=act[:, fc, :], a=sg[:], b=ups[:]: e.tensor_tensor(out=o, in0=a, in1=b, op=ALU.mult)),
                                     reads=[sg.b, ups.b], writes=[act.b] if fc == 0 else [], joins=[] if fc == 0 else [act.b])
                            for sub in range(4):
                                for dh in range(2):
                                    dps = (PB if dh == 0 else PC).next()
                                    for fc in range(4):
                                        MM(dps[:], act[:, fc, sub * 128:(sub + 1) * 128], wd[:, fc, dh * 512:(dh + 1) * 512], fc == 0, fc == 3, [wd.b, act.b], dps.b)
                                    yr = yrows.next()
                                    if dh == 0:
                                        ACT(yr[:], dps[:], AF.Identity, [dps.b], [yr.b])
                                    else:
                                        CP("dve", yr[:], dps[:], [dps.b], [yr.b])
                                    r0 = j * T_S + sub * 128
                                    DMA("pool", Ys[r0:r0 + 128, dh * 512:(dh + 1) * 512], yr[:], [yr.b], [], yr.b)
                    return run
                p.run_threads([mkB(0, NTHR), mkB(1, NTHR)] if NTHR == 2 else [mkB(0, 1)], head=HEAD_B)
                p.barrier()
            if "C" not in MOE_DEBUG:
                return
            with ExitStack() as sc_:
                def mk_thread(tid, nth):
                    g1s = Ring([sb(sc_, "g1_%d" % i, [128, D], F32) for i in range(2)])
                    g2s = Ring([sb(sc_, "g2_%d" % i, [128, D], F32) for i in range(2)])
                    dgs = Ring([sb(sc_, "dg_%d" % i, [128, 128], F32) for i in range(4)])
                    yT = sb(sc_, "yTc", [128, 8, TT], F32)
                    xt = sb(sc_, "xtC", [128, 8, TT], F32)
                    tmpc = Ring([sb(sc_, "tmpC%d" % i, [128, TT], F32) for i in range(2)])
                    lnr = {"rb": sb(sc_, "lnC_rb", [128, 8, TT], BF16), "sq": sb(sc_, "lnC_sq", [128, 8, TT], BF16), "sd": sb(sc_, "lnC_sd", [128, TT], F32), "mu": sb(sc_, "lnC_mu", [128, TT], F32)}

                    def run():
                        for t8 in range(tid, TOK // TT, nth):
                            s = (t8 * TT) // SEQ
                            tok0 = t8 * TT
                            load_x(src, tok0, xt)
                            for sub in range(4):
                                i = t8 * 4 + sub
                                g1, g2 = g1s.next(), g2s.next()
                                p.op("pool", (lambda e, g1=g1, i=i: e.indirect_dma_start(
                                    out=g1[:], out_offset=None, in_=Ys[:, :],
                                    in_offset=bass.IndirectOffsetOnAxis(ap=slots_i[:, i, 0:1].bitcast(U32), axis=0))),
                                    reads=[slots_i.b], writes=[g1.b], dma=g1.b)
                                p.op("pool", (lambda e, g2=g2, i=i: e.indirect_dma_start(
                                    out=g2[:], out_offset=None, in_=Ys[:, :],
                                    in_offset=bass.IndirectOffsetOnAxis(ap=slots_i[:, i, 1:2].bitcast(U32), axis=0))),
                                    reads=[slots_i.b], writes=[g2.b], dma=g2.b)
                                d1, d2 = dgs.next(), dgs.next()
                                TS("dve", d1[:], ident, wgt[:, i, 0:1], None, ALU.mult, None, [cst.b, wgt.b], [d1.b])
                                TS("dve", d2[:], ident, wgt[:, i, 1:2], None, ALU.mult, None, [cst.b, wgt.b], [d2.b])
                                for half in range(2):
                                    tp = PA.next()
                                    for q in range(4):
                                        dc = half * 4 + q
                                        p.op("pe", (lambda e, o=tp[:, q * 128:(q + 1) * 128], a=g1[:, dc * 128:(dc + 1) * 128], d1=d1: e.matmul(o, a, d1[:], start=True, stop=False)),
                                             reads=[g1.b, d1.b], writes=[tp.b] if q == 0 else [], joins=[] if q == 0 else [tp.b])
                                        p.op("pe", (lambda e, o=tp[:, q * 128:(q + 1) * 128], a=g2[:, dc * 128:(dc + 1) * 128], d2=d2: e.matmul(o, a, d2[:], start=False, stop=True)),
                                             reads=[g2.b, d2.b], joins=[tp.b])
                                    first = (sub == 0 and half == 0)
                                    p.op("act", (lambda e, o=yT[:, half * 4:(half + 1) * 4, sub * 128:(sub + 1) * 128], a=tp[:].rearrange("p (q t) -> p q t", q=4):
                                                 e.activation(out=o, in_=a, func=AF.Identity)),
                                         reads=[tp.b], writes=[yT.b] if first else [], joins=[] if first else [yT.b])
                            for dc in range(8):
                                tm = tmpc.next()
                                ACT(tm[:], yT[:, dc, :], AF.Identity, [yT.b, cond.b], [tm.b], scale=cv(l, 5, dc, s))
                                p.op("dve", (lambda e, dc=dc, tm=tm: e.scalar_tensor_tensor(out=xt[:, dc, :], in0=xt[:, dc, :], scalar=ALPHA, in1=tm[:],
                                                                                      op0=ALU.mult, op1=ALU.add)),
                                     reads=[xt.b, tm.b] if dc == 0 else [tm.b], writes=[xt.b] if dc == 0 else [], joins=[] if dc == 0 else [xt.b])
                            ln_fm(xt, 8, on1024, lnr)
                            ln_out_store(xt, 2, 3, l, dst, tok0)
                    return run
                p.run_threads([mk_thread(0, NTHR), mk_thread(1, NTHR)] if NTHR == 2 else [mk_thread(0, 1)], head=HEAD_C)
                p.barrier()

    SCALE = 128.0 ** -0.5

    def attn(src, dst, l):
        with ExitStack() as st:
            wout = sb(st, "wo_at", [128, 8, 1024], BF16)
            for j in range(2):
                p.op("pool", (lambda e, j=j: e.dma_start(out=wout[:, :, j * 512:(j + 1) * 512],
                                                          in_=od_w_out[:, j * 512:(j + 1) * 512].rearrange("(kc p) n -> p kc n", p=128))),
                     writes=[wout.b] if j == 0 else [], joins=[] if j == 0 else [wout.b], dma=wout.b)
            if "w" in ATTN_DEBUG:
                kT = sb(st, "kT", [128, 8, SEQ], BF16)
                qT = sb(st, "qT", [128, 8, SEQ], BF16)
            else:
                qT = sb(st, "qT", [128, 8, SEQ], BF16)
                kT = sb(st, "kT", [128, 8, SEQ], BF16)
            v1 = sb(st, "v1", [128, 16, 8, 132], BF16)
            oT = sb(st, "oT", [128, 8, SEQ], BF16)
            ksum = sb(st, "ksum", [128, 8, 8], F32)
            gate = sb(st, "gate", [128, 16, 8, 8], F32)
            selt = sb(st, "selt", [128, 16, 8, 8], F32)
            MSET("pool", v1[:].rearrange("p a b c -> p (a b c)"), 1.0, [v1.b])
            def do_seq(s):
                with ExitStack() as sa:
                    xt = sb(sa, "xta", [128, 8, TT], F32)
                    wr2 = Ring([sb(sa, "wqkv%d" % i, [128, 8, 512], BF16) for i in range(2)])
                    qf = Ring([sb(sa, "qf%d" % i, [128, TT], F32) for i in range(2)])
                    top8 = sb(sa, "top8", [128, 2, 8, 8], F32)
                    for t in range(4):
                        load_x(src, s * SEQ + t * TT, xt)
                        for dc in range(8):
                            first = (t == 0 and dc == 0)
                            p.op("act", (lambda e, o=oT[:, dc, t * TT:(t + 1) * TT], i=xt[:, dc, :], b=cv(l, 0, dc, s), sc=cv(l, 1, dc, s):
                                         e.activation(out=o, in_=i, func=AF.Identity, bias=b, scale=sc)),
                                 reads=[xt.b, cond.b], writes=[oT.b] if first else [], joins=[] if first else [oT.b])
                    for which, half in ((1, 0), (1, 1), (0, 0), (0, 1), (2, 0), (2, 1)):
                        if "kqv"[which] not in ATTN_DEBUG + "kqv" * ("A" not in ATTN_DEBUG or "x" not in ATTN_DEBUG):
                            continue
                        w = wr2.next()
                        c0 = which * 1024 + half * 512
                        if "m" in ATTN_DEBUG:
                            c0 = 1024 + half * 512
                        DMA("pool", w[:], od_w_qkv[:, c0:c0 + 512].rearrange("(kc p) n -> p kc n", p=128), [], [w.b], w.b)
                        for t in range(4):
                            if which in (0, 1):
                                for j in range(4):
                                    h = half * 4 + j
                                    ps = PA.next()
                                    for kc in range(8):
                                        MM(ps[:], w[:, kc, j * 128:(j + 1) * 128], oT[:, kc, t * TT:(t + 1) * TT], kc == 0, kc == 7, [w.b, oT.b], ps.b)
                                    if which == 1:
                                        p.op("act", (lambda e, o=kT[:, h, t * TT:(t + 1) * TT], i=ps[:]: e.activation(out=o, in_=i, func=AF.Identity)),
                                             reads=[ps.b], joins=[kT.b])
                                        p.op("dve", (lambda e, o=ksum[:, h, 2 * t:2 * t + 2], i=ps[:].rearrange("p (b k) -> p b k", b=2):
                                                     e.tensor_reduce(out=o, in_=i, axis=AX.X, op=ALU.add)),
                                             reads=[ps.b], joins=[ksum.b])
                                    else:
                                        q32 = qf.next()
                                        if "n" not in ATTN_DEBUG:
                                            ACT(q32[:], ps[:], AF.Identity, [ps.b], [q32.b])
                                        p.op("act", (lambda e, o=qT[:, h, t * TT:(t + 1) * TT], i=ps[:]: e.activation(out=o, in_=i, func=AF.Identity)),
                                             reads=[ps.b], joins=[qT.b])
                                        if "x" in ATTN_DEBUG and "g" not in ATTN_DEBUG:
                                            continue
                                        gp = PC.next()
                                        for sub in range(4):
                                            p.op("pe", (lambda e, gp=gp, sub=sub, q32=q32, h=h: e.matmul(gp[:, sub * 8:sub * 8 + 8], q32[:, sub * 128:(sub + 1) * 128],
                                                                                                   ksum[:, h, :], start=True, stop=True)),
                                                 reads=[q32.b, ksum.b], writes=[gp.b] if sub == 0 else [], joins=[] if sub == 0 else [gp.b])
                                        p.op("dve", (lambda e, gp=gp, h=h, t=t: e.tensor_copy(out=gate[:, t * 4:t * 4 + 4, h, :],
                                                                                         in_=gp[:, 0:32].rearrange("p (a n) -> p a n", a=4))),
                                             reads=[gp.b], joins=[gate.b])
                            else:
                                for sub in range(4):
                                    ps = PA.next()
                                    for kc in range(8):
                                        MM(ps[:], oT[:, kc, (t * 4 + sub) * 128:(t * 4 + sub + 1) * 128], w[:, kc, :], kc == 0, kc == 7, [w.b, oT.b], ps.b)
                                    eng = "act" if sub % 2 == 0 else "dve"
                                    if eng == "act":
                                        p.op("act", (lambda e, ps=ps, kt=t * 4 + sub, half=half: e.activation(out=v1[:, kt, half * 4:half * 4 + 4, 0:128],
                                                                                               in_=ps[:].rearrange("p (h d) -> p h d", h=4), func=AF.Identity)),
                                             reads=[ps.b], joins=[v1.b])
                                    else:
                                        p.op("dve", (lambda e, ps=ps, kt=t * 4 + sub, half=half: e.tensor_copy(out=v1[:, kt, half * 4:half * 4 + 4, 0:128],
                                                                                                in_=ps[:].rearrange("p (h d) -> p h d", h=4))),
                                             reads=[ps.b], joins=[v1.b])
                    for b in range(4, 8):
                        if "x" in ATTN_DEBUG and "s" not in ATTN_DEBUG:
                            continue
                        p.op("dve", (lambda e, b=b: e.memset(gate[:, 2 * b:2 * b + 2, :, b:8], NEG)), reads=[], joins=[gate.b]) if False else \
                            p.op("dve", (lambda e, b=b: e.memset(gate[:, 2 * b:2 * b + 2, :, b:8], NEG)), writes=[gate.b])
                        for qq in range(2):
                            for h in range(8):
                                first = (qq == 0 and h == 0)
                                p.op("dve", (lambda e, b=b, qq=qq, h=h: e.max(out=top8[:, qq, h, :], in_=gate[:, 2 * b + qq, h, :])),
                                     reads=[gate.b], writes=[top8.b] if first else [], joins=[] if first else [top8.b])
                        p.op("dve", (lambda e, b=b: e.tensor_tensor(out=selt[:, 2 * b:2 * b + 2, :, :], in0=gate[:, 2 * b:2 * b + 2, :, :],
                                                                     in1=top8[:, :, :, 2:3].to_broadcast([128, 2, 8, 8]), op=ALU.is_ge)),
                             reads=[gate.b, top8.b], writes=[selt.b])
                    p.barrier()
                if "B" not in ATTN_DEBUG:
                    return
                with ExitStack() as sbk:
                    def mkAt(tid, nth):
                        Es = Ring([sb(sbk, "E%d" % i, [128, 256], BF16) for i in range(6)])
                        accs = Ring([sb(sbk, "acc%d" % i, [128, 132], F32) for i in range(4)])
                        rden = Ring([sb(sbk, "rden%d" % i, [128, 1], F32) for i in range(4)])
                        o32 = Ring([sb(sbk, "o32_%d" % i, [128, 128], F32) for i in range(4)])

                        def run():
                            for h in range(tid, 8, nth):
                                for b in range(8):
                                    dense = b < 4
                                    order = [b] + list(range(b))
                                    accp = [accs.next(), accs.next()]
                                    ops_ = [None, None]
                                    for idx, n in enumerate(order):
                                        own = (n == b)
                                        newgrp = (idx == 0) or (not dense)
                                        lastgrp = (idx == len(order) - 1) or (not dense)
                                        if newgrp:
                                            ops_ = [PB.next(), PC.next()]
                                        Et = []
                                        for kt in range(2):
                                            sp_ = PA.next()
                                            MM(sp_[:, 0:256], kT[:, h, (2 * n + kt) * 128:(2 * n + kt + 1) * 128], qT[:, h, b * 256:(b + 1) * 256], True, True, [kT.b, qT.b], sp_.b)
                                            E = Es.next()
                                            ACT(E[:], sp_[:, 0:256], AF.Exp, [sp_.b], [E.b], scale=SCALE)
                                            if own:
                                                c0 = kt * 128
                                                p.op("dve", (lambda e, E=E, c0=c0: e.tensor_tensor(out=E[:, c0:c0 + 128], in0=E[:, c0:c0 + 128], in1=trib[:], op=ALU.mult)),
                                                     reads=[trib.b], writes=[E.b])
                                            Et.append(E)
                                        for qs in range(2):
                                            kts = [kt for kt in range(2) if not (own and kt == 1 and qs == 0)]
                                            for kt in kts:
                                                first = newgrp and (kt == kts[0])
                                                last = lastgrp and (kt == kts[-1])
                                                MM(ops_[qs][:, 0:129], Et[kt][:, qs * 128:(qs + 1) * 128], v1[:, 2 * n + kt, h, 0:129], first, last, [Et[kt].b, v1.b], ops_[qs].b)
                                            if lastgrp:
                                                acc = accp[qs]
                                                if dense or own:
                                                    ACT(acc[:, 0:129], ops_[qs][:, 0:129], AF.Identity, [ops_[qs].b], [acc.b])
                                                else:
                                                    STT(acc[:, 0:129], ops_[qs][:, 0:129], selt[:, 2 * b + qs, h, n:n + 1], acc[:, 0:129], ALU.mult, ALU.add,
                                                        [ops_[qs].b, selt.b], [acc.b])
                                    for qs in range(2):
                                        acc = accp[qs]
                                        rd = rden.next()
                                        RECIP(rd[:], acc[:, 128:129], [acc.b], [rd.b])
                                        o = o32.next()
                                        TS("dve", o[:], acc[:, 0:128], rd[:, 0:1], None, ALU.mult, None, [acc.b, rd.b], [o.b])
                                        tp = PA.next()
                                        p.op("pe", (lambda e, tp=tp, o=o: e.transpose(tp[:, 0:128], o[:], ident)), reads=[o.b, cst.b], writes=[tp.b])
                                        qt = 2 * b + qs
                                        p.op("act", (lambda e, tp=tp, h=h, qt=qt: e.activation(out=oT[:, h, qt * 128:(qt + 1) * 128], in_=tp[:, 0:128], func=AF.Identity)),
                                             reads=[tp.b], joins=[oT.b])
                        return run
                    p.run_threads([mkAt(k, NTHR_AT) for k in range(NTHR_AT)], head=HEAD_AT)
                    if debug and s == 0:
                        dbo = nc.dram_tensor("dbg_oT", [128, 8, SEQ], BF16, kind="ExternalOutput").ap()
                        DMA("sp", dbo[:, :, :], oT[:], [oT.b], [], oT.b)
                        dbq = nc.dram_tensor("dbg_qT", [128, 8, SEQ], BF16, kind="ExternalOutput").ap()
                        DMA("sp", dbq[:, :, :], qT[:], [qT.b], [], qT.b)
                        dbk = nc.dram_tensor("dbg_kT", [128, 8, SEQ], BF16, kind="ExternalOutput").ap()
                        DMA("sp", dbk[:, :, :], kT[:], [kT.b], [], kT.b)
                        dbv = nc.dram_tensor("dbg_v1", [128, 16, 8, 132], BF16, kind="ExternalOutput").ap()
                        DMA("sp", dbv[:, :, :, :], v1[:], [v1.b], [], v1.b)
                    p.barrier()
                if "C" not in ATTN_DEBUG:
                    return
                with ExitStack() as sc_:
                    xt = sb(sc_, "xtc", [128, 8, TT], F32)
                    tmp = Ring([sb(sc_, "tmpc%d" % i, [128, TT], F32) for i in range(2)])
                    lnr = {"rb": sb(sc_, "lnc_rb", [128, 8, TT], BF16), "sq": sb(sc_, "lnc_sq", [128, 8, TT], BF16), "sd": sb(sc_, "lnc_sd", [128, TT], F32), "mu": sb(sc_, "lnc_mu", [128, TT], F32)}
                    for t in range(4):
                        tok0 = s * SEQ + t * TT
                        load_x(src, tok0, xt)
                        for dc in range(8):
                            ps = PA.next()
                            for kc in range(8):
                                MM(ps[:], wout[:, kc, dc * 128:(dc + 1) * 128], oT[:, kc, t * TT:(t + 1) * TT], kc == 0, kc == 7, [wout.b, oT.b], ps.b)
                            tm = tmp.next()
                            ACT(tm[:], ps[:], AF.Identity, [ps.b, cond.b], [tm.b], scale=cv(l, 2, dc, s))
                            p.op("dve", (lambda e, dc=dc, tm=tm: e.scalar_tensor_tensor(out=xt[:, dc, :], in0=xt[:, dc, :], scalar=ALPHA, in1=tm[:],
                                                                                  op0=ALU.mult, op1=ALU.add)),
                                 reads=[xt.b, tm.b] if dc == 0 else [tm.b], writes=[xt.b] if dc == 0 else [], joins=[] if dc == 0 else [xt.b])
                        ln_fm(xt, 8, on1024, lnr)
                        ln_out_store(xt, 0, 1, l, dst, tok0)
                    p.barrier()

            for s_ in range(NSEQ):
                do_seq(s_)

    if only == "attn":
        attn(xT, yT, 1)
        stages = 0
    def moe_any(src, dst, l, zero_fill):
        if SPARSE:
            moe_sparse(src, dst, l, zero_fill)
        else:
            moe(src, dst, l)

    if only == "moe":
        moe_any(xT, yT, 0, True)
        stages = 0
    if stages >= 1:
        mixer0(xT, yT if stages == 1 else XA, 0)
    if stages >= 2:
        moe_any(XA, yT if stages == 2 else XB, 0, True)
    if stages >= 3:
        attn(XB, yT if stages == 3 else XA, 1)
    if stages >= 4:
        moe_any(XA, yT, 1, False)

    p.barrier()
    p.emit()
    return nc, es


def prep_inputs(inputs):
    f = lambda a: np.ascontiguousarray(np.asarray(a, dtype=np.float32))
    x = f(inputs["x"])
    c = f(inputs["c"])
    shared = {}
    shared["ada_w"] = f(inputs["ada_w"])
    shared["ada_bT"] = f(inputs["ada_b"].reshape(2, 48, 128).transpose(2, 0, 1))
    lnp = np.stack([inputs["ln_mix_g"], inputs["ln_mix_b"], inputs["ln_ffn_g"], inputs["ln_ffn_b"]], 1)
    shared["lnp"] = f(lnp.reshape(2, 4, 8, 128).transpose(3, 0, 1, 2))
    shared["ev_w_in"] = f(inputs["ev_w_in"][0])
    shared["ev_w_out"] = f(inputs["ev_w_out"][0])
    sg = np.stack([inputs["ev_sgu_ln_g"][0], inputs["ev_sgu_ln_b"][0]], 0)
    shared["sgp"] = f(sg.reshape(2, 4, 128).transpose(2, 0, 1))
    shared["wsT"] = f(inputs["ev_w_s"][0].transpose(2, 0, 1))
    shared["bs"] = f(inputs["ev_b_s"][0].reshape(1, 512))
    shared["wdw"] = f(inputs["ev_w_dw"][0].reshape(31, 4, 128).transpose(2, 1, 0))
    cv = np.stack([inputs["ev_b_dw"][0], inputs["ev_conv_ln_g"][0], inputs["ev_conv_ln_b"][0]], 0)
    shared["cvp"] = f(cv.reshape(3, 4, 128).transpose(2, 0, 1))
    shared["od_w_qkv"] = f(inputs["od_w_qkv"][0])
    shared["od_w_out"] = f(inputs["od_w_out"][0])
    wr = np.concatenate([inputs["moe_w_grp"], inputs["moe_w_er"].transpose(0, 2, 1, 3).reshape(2, D, 16)], -1)
    shared["wr"] = f(wr.reshape(2, 8, 128, 20).transpose(0, 2, 1, 3))
    shared["br"] = f(np.concatenate([inputs["moe_b_grp"], inputs["moe_b_er"].reshape(2, 16)], -1).reshape(2, 1, 20))
    shared["moe_w_gate"] = f(np.asarray(inputs["moe_w_gate"]).reshape(2, 16, 8, 128, 512).transpose(0, 1, 3, 2, 4))
    shared["moe_w_up"] = f(np.asarray(inputs["moe_w_up"]).reshape(2, 16, 8, 128, 512).transpose(0, 1, 3, 2, 4))
    shared["moe_w_down"] = f(np.asarray(inputs["moe_w_down"]).reshape(2, 16, 4, 128, D).transpose(0, 1, 3, 2, 4))
    cst = np.zeros((128, 2473), np.float32)
    cst[:, 2472] = np.arange(128, dtype=np.float32)
    cst[:, 2432:2440] = np.arange(8, dtype=np.float32)[None, :] * 512.0
    cst[:, 2440:2472] = np.arange(32, dtype=np.float32)[None, :] * 512.0
    cst[:, 0:128] = np.eye(128, dtype=np.float32)
    cst[:, 128:256] = np.triu(np.ones((128, 128), np.float32))
    cst[:, 256:384] = 1.0
    for e in range(16):
        cst[e, 384 + e * 128:384 + (e + 1) * 128] = 1.0
    shared["consts"] = cst
    maps = []
    for core in range(NCORES):
        m = dict(shared)
        xs = x[core * NSEQ:(core + 1) * NSEQ]
        m["xT"] = f(xs.reshape(TOK, 8, 128).transpose(1, 2, 0))
        m["cT"] = f(c[core * NSEQ:(core + 1) * NSEQ].reshape(NSEQ, 8, 128).transpose(2, 1, 0))
        maps.append(m)
    return maps


_CACHE = {}


def kernel(**inputs):
    maps = prep_inputs(inputs)
    if "nc" not in _CACHE:
        _CACHE["nc"] = build()
    nc, _ = _CACHE["nc"]
    res = run_bass_kernel_spmd(nc, maps, core_ids=list(range(NCORES)))
    out = np.empty((NCORES * NSEQ, SEQ, D), np.float32)
    for core in range(NCORES):
        yT = np.asarray(res.results[core]["yT"])
        out[core * NSEQ:(core + 1) * NSEQ] = yT.transpose(2, 0, 1).reshape(NSEQ, SEQ, D)
    return out
```
